# Optimizing a Trainium2 kernel written in Bass

```python
import jax, jax.numpy as jnp
from jax import lax
import numpy as np

D_MODEL = 2048
BATCH = 2
SEQ = 4096
DEPTH = 4

SB_HEADS = 8
SB_HEAD_DIM = 128
SB_WIDTH = SB_HEADS * SB_HEAD_DIM
MLA_HEADS = 8
MLA_NOPE_DIM = 128
MLA_ROPE_DIM = 64
MLA_V_DIM = 128
MLA_QK_DIM = MLA_NOPE_DIM + MLA_ROPE_DIM
MLA_WIDTH = MLA_HEADS * MLA_V_DIM
Q_LORA_RANK = 512
KV_LORA_RANK = 512
ROPE_THETA = 10000.0
IN_COLS = 3 * SB_WIDTH + Q_LORA_RANK + KV_LORA_RANK + MLA_ROPE_DIM + 2 * D_MODEL
D_FF = ((8 * D_MODEL // 3 + 255) // 256) * 256
N_EXPERTS = 8
TOP_K = 2
N_DENSE = (DEPTH + 1) // 2
N_MOE = DEPTH // 2
Q_BLOCK = 128
NORM_EPS = 1e-6
N_MOD = 6

kernel_name = "hybrid_stickbreak_mla_moe_adaln"


def _rms_norm(x, g):
    xf = x.astype(jnp.float32)
    y = xf * lax.rsqrt(jnp.mean(xf * xf, axis=-1, keepdims=True) + NORM_EPS)
    return (y * g.astype(jnp.float32)).astype(x.dtype)


def _rope_tables(positions, dtype):
    inv_freq = 1.0 / (ROPE_THETA ** (jnp.arange(0, MLA_ROPE_DIM, 2, dtype=jnp.float32) / MLA_ROPE_DIM))
    ang = positions.astype(jnp.float32)[..., None] * inv_freq
    return jnp.cos(ang).astype(dtype), jnp.sin(ang).astype(dtype)


def _apply_rope(x, cos, sin):
    half = x.shape[-1] // 2
    x1, x2 = x[..., :half], x[..., half:]
    return jnp.concatenate([x1 * cos - x2 * sin, x2 * cos + x1 * sin], axis=-1)


def _query_block_sweep(q, block_fn):
    B, H, S, d = q.shape
    nb = S // Q_BLOCK
    qb = q.reshape(B, H, nb, Q_BLOCK, d).transpose(2, 0, 1, 3, 4)
    starts = jnp.arange(nb, dtype=jnp.int32) * Q_BLOCK
    out = lax.map(block_fn, (qb, starts))
    return out.transpose(1, 2, 0, 3, 4).reshape(B, H, S, out.shape[-1])


def _stick_breaking_attention(q, k, v):
    S = k.shape[2]
    scale = q.shape[-1] ** -0.5
    key_idx = jnp.arange(S, dtype=jnp.int32)

    def block(args):
        qb, start = args
        t_idx = start + jnp.arange(Q_BLOCK, dtype=jnp.int32)
        z = jnp.einsum('bhqd,bhkd->bhqk', qb, k, preferred_element_type=jnp.float32) * scale
        causal = key_idx[None, :] < t_idx[:, None]
        sp = jnp.where(causal, jax.nn.softplus(z), 0.0)
        suffix = lax.cumsum(sp, axis=sp.ndim - 1, reverse=True) - sp
        log_a = z - sp - suffix
        a = jnp.exp(jnp.where(causal, log_a, -jnp.inf))
        return jnp.einsum('bhqk,bhkd->bhqd', a.astype(v.dtype), v)

    return _query_block_sweep(q, block)


def _causal_softmax_attention(q, k, v):
    S = k.shape[2]
    scale = q.shape[-1] ** -0.5
    key_idx = jnp.arange(S, dtype=jnp.int32)

    def block(args):
        qb, start = args
        t_idx = start + jnp.arange(Q_BLOCK, dtype=jnp.int32)
        s = jnp.einsum('bhqd,bhkd->bhqk', qb, k, preferred_element_type=jnp.float32) * scale
        s = jnp.where(key_idx[None, :] <= t_idx[:, None], s, -jnp.inf)
        p = jax.nn.softmax(s, axis=-1)
        return jnp.einsum('bhqk,bhkd->bhqd', p.astype(v.dtype), v)

    return _query_block_sweep(q, block)


def _hybrid_mixer(h, cos, sin, w_in, q_norm_g, kv_norm_g, w_uq, w_ukv, w_sb_up, w_mla_up, w_o):
    B, S, _ = h.shape
    proj = h @ w_in
    sizes = [SB_WIDTH, SB_WIDTH, SB_WIDTH, Q_LORA_RANK, KV_LORA_RANK, MLA_ROPE_DIM, D_MODEL]
    offs, o = [], 0
    for sz in sizes:
        o += sz
        offs.append(o)
    q_sb, k_sb, v_sb, c_q, c_kv, k_pe, g_sb, g_mla = jnp.split(proj, offs, axis=-1)

    def heads(t, n):
        return t.reshape(B, S, n, -1).transpose(0, 2, 1, 3)

    o_sb = _stick_breaking_attention(heads(q_sb, SB_HEADS), heads(k_sb, SB_HEADS), heads(v_sb, SB_HEADS))
    o_sb = o_sb.transpose(0, 2, 1, 3).reshape(B, S, SB_WIDTH)

    q = (_rms_norm(c_q, q_norm_g) @ w_uq).reshape(B, S, MLA_HEADS, MLA_QK_DIM)
    q_nope, q_pe = q[..., :MLA_NOPE_DIM], q[..., MLA_NOPE_DIM:]
    q_pe = _apply_rope(q_pe, cos[:, :, None, :], sin[:, :, None, :])
    kv = (_rms_norm(c_kv, kv_norm_g) @ w_ukv).reshape(B, S, MLA_HEADS, MLA_NOPE_DIM + MLA_V_DIM)
    k_nope, v_mla = kv[..., :MLA_NOPE_DIM], kv[..., MLA_NOPE_DIM:]
    k_pe = _apply_rope(k_pe, cos, sin)
    q_full = jnp.concatenate([q_nope, q_pe], axis=-1)
    k_full = jnp.concatenate(
        [k_nope, jnp.broadcast_to(k_pe[:, :, None, :], (B, S, MLA_HEADS, MLA_ROPE_DIM))], axis=-1)
    o_mla = _causal_softmax_attention(q_full.transpose(0, 2, 1, 3), k_full.transpose(0, 2, 1, 3),
                                      v_mla.transpose(0, 2, 1, 3))
    o_mla = o_mla.transpose(0, 2, 1, 3).reshape(B, S, MLA_WIDTH)

    y = jax.nn.sigmoid(g_sb) * (o_sb @ w_sb_up) + jax.nn.sigmoid(g_mla) * (o_mla @ w_mla_up)
    return y @ w_o


def _swiglu(h, wg, wu, wd):
    return (jax.nn.silu(h @ wg) * (h @ wu)) @ wd


def _moe_swiglu(h, w_router, wg, wu, wd):
    logits = jnp.einsum('bsd,de->bse', h, w_router, preferred_element_type=jnp.float32)
    top_v, top_i = lax.top_k(logits, TOP_K)
    top_w = jax.nn.softmax(top_v, axis=-1)
    gates = jnp.sum(jax.nn.one_hot(top_i, N_EXPERTS, dtype=jnp.float32) * top_w[..., None],
                    axis=-2).astype(h.dtype)
    y = jnp.zeros_like(h)
    for e in range(N_EXPERTS):
        y = y + gates[..., e:e + 1] * _swiglu(h, wg[e], wu[e], wd[e])
    return y


def _normal(k, shape, fan_in, mult=1.0):
    return jax.random.normal(k, shape, jnp.float32) * (mult * fan_in ** -0.5)


def setup_inputs(seed: int = 0) -> dict:
    key = jax.random.key(seed)
    ks = jax.random.split(key, 24)
    D = D_MODEL
    gain = lambda k, shape: 1.0 + 0.02 * jax.random.normal(k, shape, jnp.float32)
    positions = (jnp.arange(SEQ, dtype=jnp.int32)[None, :]
                 + jax.random.randint(ks[2], (BATCH, 1), 0, 1024, dtype=jnp.int32))
    return {
        "x": jax.random.normal(ks[0], (BATCH, SEQ, D), jnp.float32),
        "c": jax.random.normal(ks[1], (BATCH, D), jnp.float32),
        "positions": positions,
        "w_ada": _normal(ks[3], (DEPTH, D, N_MOD * D), D, 0.5),
        "b_ada": 0.02 * jax.random.normal(ks[4], (DEPTH, N_MOD * D), jnp.float32),
        "norm_mix_g": gain(ks[5], (DEPTH, D)),
        "norm_ffn_g": gain(ks[6], (DEPTH, D)),
        "w_in": _normal(ks[7], (DEPTH, D, IN_COLS), D),
        "q_norm_g": gain(ks[8], (DEPTH, Q_LORA_RANK)),
        "kv_norm_g": gain(ks[9], (DEPTH, KV_LORA_RANK)),
        "w_uq": _normal(ks[10], (DEPTH, Q_LORA_RANK, MLA_HEADS * MLA_QK_DIM), Q_LORA_RANK),
        "w_ukv": _normal(ks[11], (DEPTH, KV_LORA_RANK, MLA_HEADS * (MLA_NOPE_DIM + MLA_V_DIM)), KV_LORA_RANK),
        "w_sb_up": _normal(ks[12], (DEPTH, SB_WIDTH, D), SB_WIDTH),
        "w_mla_up": _normal(ks[13], (DEPTH, MLA_WIDTH, D), MLA_WIDTH),
        "w_o": _normal(ks[14], (DEPTH, D, D), D),
        "w_ffn_gate": _normal(ks[15], (N_DENSE, D, D_FF), D),
        "w_ffn_up": _normal(ks[16], (N_DENSE, D, D_FF), D),
        "w_ffn_down": _normal(ks[17], (N_DENSE, D_FF, D), D_FF),
        "w_router": _normal(ks[18], (N_MOE, D, N_EXPERTS), D),
        "w_exp_gate": _normal(ks[19], (N_MOE, N_EXPERTS, D, D_FF), D),
        "w_exp_up": _normal(ks[20], (N_MOE, N_EXPERTS, D, D_FF), D),
        "w_exp_down": _normal(ks[21], (N_MOE, N_EXPERTS, D_FF, D), D_FF),
        "final_norm_g": gain(ks[22], (D,)),
    }


def reference(x, c, positions, w_ada, b_ada, norm_mix_g, norm_ffn_g, w_in, q_norm_g, kv_norm_g,
              w_uq, w_ukv, w_sb_up, w_mla_up, w_o, w_ffn_gate, w_ffn_up, w_ffn_down,
              w_router, w_exp_gate, w_exp_up, w_exp_down, final_norm_g):
    cos, sin = _rope_tables(positions, x.dtype)
    c_act = jax.nn.silu(c)
    for l in range(DEPTH):
        mod = c_act @ w_ada[l] + b_ada[l]
        sh1, sc1, g1, sh2, sc2, g2 = [m[:, None, :] for m in jnp.split(mod, N_MOD, axis=-1)]
        h = _rms_norm(x, norm_mix_g[l]) * (1.0 + sc1) + sh1
        x = x + g1 * _hybrid_mixer(h, cos, sin, w_in[l], q_norm_g[l], kv_norm_g[l], w_uq[l], w_ukv[l],
                                   w_sb_up[l], w_mla_up[l], w_o[l])
        h = _rms_norm(x, norm_ffn_g[l]) * (1.0 + sc2) + sh2
        j = l // 2
        if l % 2 == 0:
            f = _swiglu(h, w_ffn_gate[j], w_ffn_up[j], w_ffn_down[j])
        else:
            f = _moe_swiglu(h, w_router[j], w_exp_gate[j], w_exp_up[j], w_exp_down[j])
        x = x + g2 * f
    return _rms_norm(x, final_norm_g)
```

```python
import contextlib
import math
import numpy as np
import ml_dtypes
import concourse.bass as bass
import concourse.mybir as mybir
from concourse.bass_utils import run_bass_kernel_spmd

F32 = mybir.dt.float32
BF16 = mybir.dt.bfloat16
I32 = mybir.dt.int32
AF = mybir.ActivationFunctionType
ALU = mybir.AluOpType

ENGS = ["pe", "act", "dve", "pool", "sp"]

D = 2048
KC = 16
SEQ = 4096
NB = 2
DEPTH = 4
TL = 1024
NTC = 2
TCW = 512
NH = 8
DFF = 5632
NFC = 44
GROUPS = [12, 12, 10, 10]
GMAX = 12
NE = 8
IN_COLS = 8256
OFF_Q, OFF_K, OFF_V, OFF_CQ, OFF_CKV, OFF_KPE, OFF_GSB, OFF_GMLA = 0, 1024, 2048, 3072, 3584, 4096, 4160, 6208
X_K, X_V, X_KN, X_VM, X_KPE = 0, 8192, 16384, 24576, 32768
XW = 33792
EPS = 1e-6
SB_SCALE = 128 ** -0.5
MLA_SCALE = 192 ** -0.5
SLOT_EL = 4096
NSLOT = 5


class Buf:
    _n = 0

    def __init__(self, name, ap=None, phase=False):
        Buf._n += 1
        self.id = Buf._n
        self.name = name
        self.ap = ap
        self.phase = phase
        self.dsem = None
        self.dcnt = 0
        self.rsem = None
        self.rcnt = 0


class Prog:
    def __init__(self, nc, arena_kb=206):
        self.nc = nc
        self.eng = {"pe": nc.tensor, "act": nc.scalar, "dve": nc.vector,
                    "pool": nc.gpsimd, "sp": nc.sync}
        self.q = {e: [] for e in ENGS}
        self.cnt = {e: 0 for e in ENGS}
        self.known = {e: {} for e in ENGS}
        self.stack = contextlib.ExitStack()
        self.esem = {e: self.stack.enter_context(nc.semaphore("s_" + e)) for e in ENGS}
        self.state = {}
        self.nsem = 0
        self.ninstr = 0
        self.open_dma = []
        self.arena_words = arena_kb * 256
        self.arena = self.stack.enter_context(nc.sbuf_tensor("arena", [128, self.arena_words], F32))
        self.top = 0
        self.peak = 0
        self.sem_pool = {}
        self.ccsem = None
        self.cccnt = 0
        self.live = []
        self.free_sems = []
        self.pool_pending = []

    def alloc(self, name, shape, dtype, phase=True):
        esz = 2 if dtype == BF16 else 4
        nparts = shape[0]
        nel = 1
        for s in shape[1:]:
            nel *= s
        nbytes = (nel * esz + 31) // 32 * 32
        off = self.top
        self.top += nbytes
        self.peak = max(self.peak, self.top)
        assert self.top <= self.arena_words * 4, f"SBUF arena overflow at {name}: {self.top}"
        v = self.arena[0:nparts, off // 4:(off + nbytes) // 4]
        if esz == 2:
            v = v.bitcast(BF16)
        elif dtype != F32:
            v = v.bitcast(dtype)
        v = v[:, 0:nel]
        if len(shape) == 3:
            v = v.rearrange("p (a b) -> p a b", a=shape[1])
        elif len(shape) == 4:
            v = v.rearrange("p (a b c) -> p a b c", a=shape[1], b=shape[2])
        b = Buf(name, v, phase=phase)
        self.live.append((off, b))
        return b

    def mark(self):
        return self.top

    def release(self, mark):
        self.barrier()
        self.top = mark
        keep = []
        for (off, b) in self.live:
            if off >= mark:
                if b.dsem is not None:
                    self.free_sems.append((b.dsem, b.dcnt))
                    b.dsem = None
                if b.rsem is not None:
                    self.free_sems.append((b.rsem, b.rcnt))
                    b.rsem = None
            else:
                keep.append((off, b))
        self.live = keep

    def sem_for(self, buf, kind):
        if self.free_sems:
            sem, cnt = self.free_sems.pop()
        else:
            sem, cnt = self.new_sem(kind), 0
        if kind == "d":
            buf.dsem, buf.dcnt = sem, cnt
        else:
            buf.rsem, buf.rcnt = sem, cnt

    def collective_allgather(self, in_buf, in_ap, out_buf, out_ap, groups):
        e = "pool"
        reads = [(in_buf, None)]
        writes = [(out_buf, None)]
        self._flush_pool_pending()
        self._emit_waits(e, self._collect(reads, writes))
        if self.ccsem is None:
            self.ccsem = self.new_sem("cc")
        self.cccnt += 1
        sem = self.ccsem
        self.ninstr += 1
        self.q[e].append(lambda eng=self.eng[e], sem=sem, i=in_ap, o=out_ap, groups=groups:
                         eng.collective_compute("AllGather", ALU.bypass, replica_groups=groups, ins=[i], outs=[o]).then_inc(sem, 1))
        ref = ("sem", sem, ("cc",), self.cccnt)
        self._record(ref, reads, writes)
        return ref

    def _flush_pool_pending(self):
        if self.pool_pending:
            refs = self.pool_pending
            self.pool_pending = []
            self._emit_waits("pool", refs)

    def psum(self, name):
        t = self.stack.enter_context(self.nc.psum_tensor(name, [128, 512], F32))
        return Buf(name, t)

    def new_sem(self, name):
        self.nsem += 1
        return self.stack.enter_context(self.nc.semaphore(f"{name}_{self.nsem}"))

    def _conf(self, b, key):
        d = self.state.get(b.id)
        if not d:
            return []
        if key is None:
            return list(d.values())
        out = []
        s = d.get(key)
        if s is not None:
            out.append(s)
        s = d.get(None)
        if s is not None:
            out.append(s)
        return out

    def _collect(self, reads, writes):
        refs = []
        for (b, key) in reads:
            for s in self._conf(b, key):
                if s[0] is not None:
                    refs.append(s[0])
        for (b, key) in writes:
            for s in self._conf(b, key):
                if s[0] is not None:
                    refs.append(s[0])
                refs.extend(s[1].values())
        return refs

    @staticmethod
    def _rk(ref):
        return ("eng", ref[1]) if ref[0] == "eng" else ("sem", ref[2])

    @staticmethod
    def _rv(ref):
        return ref[2] if ref[0] == "eng" else ref[3]

    def _record(self, ref, reads, writes):
        rk = self._rk(ref)
        for (b, key) in reads:
            d = self.state.setdefault(b.id, {})
            s = d.get(key)
            if s is None:
                s = [None, {}]
                d[key] = s
            old = s[1].get(rk)
            if old is None or self._rv(old) < self._rv(ref):
                s[1][rk] = ref
        for (b, key) in writes:
            d = self.state.setdefault(b.id, {})
            if key is None:
                d.clear()
            d[key] = [ref, {}]

    def _emit_waits(self, e, refs):
        need = {}
        for r in refs:
            if r[0] == "eng" and r[1] == e and e == "pe":
                continue
            k = self._rk(r)
            v = self._rv(r)
            sem = self.esem[r[1]] if r[0] == "eng" else r[1]
            if k not in need or need[k][1] < v:
                need[k] = (sem, v)
        kn = self.known[e]
        for k, (sem, v) in need.items():
            if kn.get(k, 0) >= v:
                continue
            kn[k] = v
            self.q[e].append(lambda eng=self.eng[e], sem=sem, v=v: eng.wait_ge(sem, v))

    @staticmethod
    def _norm(lst):
        out = []
        for x in lst or []:
            out.append((x, None) if isinstance(x, Buf) else x)
        return out

    def op(self, e, fn, reads=None, writes=None, inc=True):
        reads = self._norm(reads)
        writes = self._norm(writes)
        if e == "pool":
            self._flush_pool_pending()
        self._emit_waits(e, self._collect(reads, writes))
        self.ninstr += 1
        if inc:
            self.cnt[e] += 1
            idx = self.cnt[e]
            self.q[e].append(lambda eng=self.eng[e], fn=fn, sem=self.esem[e]: fn(eng).then_inc(sem, 1))
        else:
            idx = self.cnt[e] + 1
            self.q[e].append(lambda eng=self.eng[e], fn=fn: fn(eng))
        self._record(("eng", e, idx), reads, writes)

    def dma(self, e, out_buf, out_ap, in_buf, in_ap, out_key=None, in_key=None, **kw):
        reads = [(in_buf, in_key)] if in_buf is not None else []
        writes = [(out_buf, out_key)] if out_buf is not None else []
        if e == "pool" and ((out_buf is not None and out_buf.phase) or (in_buf is not None and in_buf.phase)):
            self._flush_pool_pending()
        self._emit_waits(e, self._collect(reads, writes))
        self.ninstr += 1
        tb = out_buf if (out_buf is not None and out_buf.ap is not None) else (in_buf if in_buf is not None else out_buf)
        if tb is out_buf:
            if tb.dsem is None:
                self.sem_for(tb, "d")
            tb.dcnt += 16
            ref = ("sem", tb.dsem, ("d", tb.id), tb.dcnt)
            sem = tb.dsem
        else:
            if tb.rsem is None:
                self.sem_for(tb, "r")
            tb.rcnt += 16
            ref = ("sem", tb.rsem, ("r", tb.id), tb.rcnt)
            sem = tb.rsem
        self.q[e].append(lambda eng=self.eng[e], o=out_ap, i=in_ap, sem=sem, kw=kw:
                         eng.dma_start(out=o, in_=i, **kw).then_inc(sem, 16))
        self._record(ref, reads, writes)
        if (out_buf is not None and out_buf.phase) or (in_buf is not None and in_buf.phase):
            self.open_dma.append(ref)
        return ref

    def dma_multi(self, e, out_buf, parts, **kw):
        writes = [(out_buf, None)]
        if e == "pool" and out_buf.phase:
            self._flush_pool_pending()
        self._emit_waits(e, self._collect([], writes))
        if out_buf.dsem is None:
            self.sem_for(out_buf, "d")
        sem = out_buf.dsem
        for (o, i) in parts:
            self.ninstr += 1
            out_buf.dcnt += 16
            self.q[e].append(lambda eng=self.eng[e], o=o, i=i, sem=sem, kw=kw:
                             eng.dma_start(out=o, in_=i, **kw).then_inc(sem, 16))
        ref = ("sem", sem, ("d", out_buf.id), out_buf.dcnt)
        self._record(ref, [], writes)
        if out_buf.phase:
            self.open_dma.append(ref)
        return ref

    def barrier(self, engines=ENGS, lazy_pool=True):
        for e in engines:
            refs = [("eng", o, self.cnt[o]) for o in ENGS if o != e and self.cnt[o] > 0]
            refs += self.open_dma
            if e == "pool" and lazy_pool:
                self.pool_pending = self.pool_pending + refs
                continue
            self._emit_waits(e, refs)
        self.open_dma = []

    def finish(self, final_refs):
        self._emit_waits("sp", list(final_refs))
        self.barrier(["sp"])

    def emit(self):
        with self.nc.Block() as block:
            @block.tensor
            def _(eng):
                for f in self.q["pe"]:
                    f()

            @block.scalar
            def _(eng):
                for f in self.q["act"]:
                    f()

            @block.vector
            def _(eng):
                for f in self.q["dve"]:
                    f()

            @block.gpsimd
            def _(eng):
                for f in self.q["pool"]:
                    f()

            @block.sync
            def _(eng):
                for f in self.q["sp"]:
                    f()
        self.stack.close()


class KVX:
    def __init__(self, aps, bufs, pw):
        self.aps, self.bufs, self.pw = aps, bufs, pw

    def loc(self, x0, w, rows=None):
        i, off = x0 // self.pw, x0 % self.pw
        assert off + w <= self.pw
        ap = self.aps[i]
        ap = ap[:, off:off + w] if rows is None else ap[rows[0]:rows[1], off:off + w]
        return self.bufs[i], ap

    def full(self, x0, w, rows=None):
        i, off = x0 // self.pw, x0 % self.pw
        assert off + w <= self.pw
        ap = self.aps[i].rearrange("(r p) x -> p r x", p=128)
        ap = ap[:, :, off:off + w] if rows is None else ap[rows[0]:rows[1], :, off:off + w]
        return self.bufs[i], ap


class K:
    def __init__(self, nc):
        self.nc = nc
        P = self.P = Prog(nc)
        self.ps = [P.psum(f"ps{i}") for i in range(8)]
        self.xT = P.alloc("xT", [128, KC, TL], F32, phase=False)
        self.slots = [P.alloc(f"slot{i}", [128, SLOT_EL], BF16, phase=False) for i in range(NSLOT)]
        self.slot_i = 0
        self.ones = P.alloc("ones", [128, 128], BF16, phase=False)
        self.tincl = P.alloc("tincl", [128, 128], BF16, phase=False)
        self.kposc = P.alloc("kposc", [128, 32], F32, phase=False)
        self.qposb = P.alloc("qposb", [128, TL], F32, phase=False)
        self.cc = P.alloc("cc", [64, TL], F32, phase=False)
        self.ss = P.alloc("ss", [64, TL], F32, phase=False)
        self.modT = P.alloc("modT", [128, 96], F32, phase=False)
        self.lc = P.alloc("lc", [128, 32], F32, phase=False)
        self.cact = P.alloc("cact", [128, KC], BF16, phase=False)
        self.one1 = P.alloc("one1", [1, 8], F32, phase=False)
        self.rr = 0

    def mm(self, out, lhsT, rhs, start, stop, reads, writes, inc=None):
        if inc is None:
            inc = stop
        self.P.op("pe", lambda e: e.matmul(out, lhsT, rhs, start=start, stop=stop),
                  reads=reads, writes=writes, inc=inc)

    def wslot(self):
        s = self.slots[self.slot_i % NSLOT]
        self.slot_i += 1
        return s

    def wload(self, parts):
        P = self.P
        s = self.wslot()
        for (view, src) in parts(s):
            P.dma("pool", s, view, None, src)
        return s

    def wtile(self, src, a, b):
        s = self.wslot()
        v = s.ap[:, 0:a * b].rearrange("p (a b) -> p a b", a=a)
        self.P.dma("pool", s, v, None, src)
        return s, v

    def evac(self, fn_act, fn_dve, reads, writes):
        self.rr += 1
        if self.rr % 2 == 0:
            self.P.op("act", fn_act, reads=reads, writes=writes)
        else:
            self.P.op("dve", fn_dve, reads=reads, writes=writes)

    def emit_setup(self, io):
        P = self.P
        nc = self.nc
        ones, tincl = self.ones, self.tincl
        P.op("dve", lambda e: e.memset(ones.ap[:], 1.0), writes=[ones])
        P.op("pool", lambda e: e.affine_select(tincl.ap[:], ones.ap[:], [[-1, 128]], ALU.is_ge, 0.0,
                                               base=0, channel_multiplier=1), reads=[ones], writes=[tincl])
        m0 = P.mark()
        ti = P.alloc("ti", [128, 32], I32)
        P.op("dve", lambda e: e.memset(self.one1.ap[:], 1.0), writes=[self.one1])
        P.op("pool", lambda e: e.iota(ti.ap[:], [[128, 32]], base=0, channel_multiplier=1), writes=[ti])
        P.op("dve", lambda e: e.tensor_copy(self.kposc.ap[:], ti.ap[:]), reads=[ti], writes=[self.kposc])
        qi = P.alloc("qi", [128, TL], I32)
        P.dma("sp", qi, qi.ap[:], None, io["qpos"])
        P.op("dve", lambda e: e.tensor_copy(self.qposb.ap[:], qi.ap[:]), reads=[qi], writes=[self.qposb])
        pi = P.alloc("pi", [64, TL], I32)
        P.dma("sp", pi, pi.ap[:], None, io["pos"])
        posf = P.alloc("posf", [64, TL], F32)
        P.op("dve", lambda e: e.tensor_copy(posf.ap[:], pi.ap[:]), reads=[pi], writes=[posf])
        fi = P.alloc("fi", [64, 2], I32)
        ff = P.alloc("ff", [64, 4], F32)
        P.op("pool", lambda e: e.iota(fi.ap[0:32, 0:1], [[0, 1]], base=0, channel_multiplier=1), writes=[fi])
        P.op("pool", lambda e: e.iota(fi.ap[32:64, 0:1], [[0, 1]], base=0, channel_multiplier=1), writes=[fi])
        P.op("dve", lambda e: e.tensor_copy(ff.ap[:, 0:1], fi.ap[:, 0:1]), reads=[fi], writes=[ff])
        P.op("act", lambda e: e.activation(ff.ap[:, 1:2], ff.ap[:, 0:1], AF.Exp, scale=-math.log(10000.0) / 32.0),
             reads=[ff], writes=[ff])
        P.op("dve", lambda e: e.memset(ff.ap[0:32, 2:3], -1.0), reads=[ff], writes=[ff])
        P.op("dve", lambda e: e.memset(ff.ap[32:64, 2:3], 1.0), reads=[ff], writes=[ff])
        ang = P.alloc("ang", [64, TL], F32)
        P.op("dve", lambda e: e.tensor_scalar(ang.ap[:], posf.ap[:], ff.ap[:, 1:2], None, ALU.mult),
             reads=[posf, ff], writes=[ang])
        kf = P.alloc("kf", [64, TL], F32)
        ki = P.alloc("ki", [64, TL], I32)
        r = P.alloc("r", [64, TL], F32)
        g = P.alloc("g", [64, TL], F32)
        TWO_PI = 2.0 * math.pi
        C1 = 6.28125
        C2 = TWO_PI - C1

        def reduce_sin(dst, shift):
            P.op("dve", lambda e: e.tensor_scalar(kf.ap[:], ang.ap[:], shift, 1.0 / TWO_PI, ALU.add, ALU.mult),
                 reads=[ang], writes=[kf])
            P.op("dve", lambda e: e.tensor_copy(ki.ap[:], kf.ap[:]), reads=[kf], writes=[ki])
            P.op("dve", lambda e: e.tensor_copy(kf.ap[:], ki.ap[:]), reads=[ki], writes=[kf])
            P.op("dve", lambda e: e.scalar_tensor_tensor(r.ap[:], kf.ap[:], -C1, ang.ap[:], ALU.mult, ALU.add),
                 reads=[kf, ang], writes=[r])
            P.op("dve", lambda e: e.scalar_tensor_tensor(r.ap[:], kf.ap[:], -C2, r.ap[:], ALU.mult, ALU.add),
                 reads=[kf, r], writes=[r])
            if shift != 0.0:
                P.op("dve", lambda e: e.tensor_scalar(r.ap[:], r.ap[:], shift, None, ALU.add), reads=[r], writes=[r])
            P.op("dve", lambda e: e.tensor_scalar(g.ap[:], r.ap[:], math.pi, -TWO_PI, ALU.is_gt, ALU.mult),
                 reads=[r], writes=[g])
            P.op("dve", lambda e: e.tensor_tensor(r.ap[:], r.ap[:], g.ap[:], ALU.add), reads=[r, g], writes=[r])
            P.op("dve", lambda e: e.tensor_scalar(g.ap[:], r.ap[:], -math.pi, TWO_PI, ALU.is_lt, ALU.mult),
                 reads=[r], writes=[g])
            P.op("dve", lambda e: e.tensor_tensor(r.ap[:], r.ap[:], g.ap[:], ALU.add), reads=[r, g], writes=[r])
            P.op("dve", lambda e: e.tensor_scalar(r.ap[:], r.ap[:], 3.1415925, -3.1415925, ALU.min, ALU.max),
                 reads=[r], writes=[r])
            P.op("act", lambda e: e.activation(dst, r.ap[:], AF.Sin), reads=[r], writes=[self.cc, self.ss])

        reduce_sin(self.cc.ap[:], math.pi / 2.0)
        reduce_sin(self.ss.ap[:], 0.0)
        P.op("dve", lambda e: e.tensor_scalar(self.ss.ap[:], self.ss.ap[:], ff.ap[:, 2:3], None, ALU.mult),
             reads=[self.ss, ff], writes=[self.ss])
        ct = P.alloc("ct", [128, KC], F32)
        P.dma("sp", ct, ct.ap[:], None, io["cT"])
        P.op("act", lambda e: e.activation(self.cact.ap[:], ct.ap[:], AF.Silu), reads=[ct], writes=[self.cact])
        P.release(m0)

    def emit_mod(self, io):
        P = self.P
        m0 = P.mark()
        wada = io["w_ada"].rearrange("(kc p) n -> p kc n", p=128)
        rowt = [P.alloc(f"modrow{i}", [1, 512], F32) for i in range(2)]
        bT = P.alloc("bT", [128, 96], F32)
        P.dma("sp", bT, bT.ap[:], None, io["b_adaT"])
        gm = P.alloc("gm", [128, 32], F32)
        P.dma_multi("sp", gm, [(gm.ap[:, 0:16], io["g_mixT"]), (gm.ap[:, 16:32], io["g_ffnT"])])
        mps = self.ps[7]
        for blk in range(24):
            pb = self.ps[blk % 2]
            for half in range(2):
                s, wv = self.wtile(wada[:, :, blk * 512 + half * 256: blk * 512 + half * 256 + 256], KC, 256)
                for kc in range(KC):
                    self.mm(pb.ap[0:1, half * 256:(half + 1) * 256], self.cact.ap[:, kc:kc + 1], wv[:, kc, :],
                            kc == 0, kc == KC - 1, reads=[s, self.cact], writes=[(pb, half)])
            row = rowt[blk % 2]
            P.op("act", lambda e, row=row, pb=pb: e.copy(row.ap[:], pb.ap[0:1, :]), reads=[pb], writes=[row])
            for s4 in range(4):
                j = blk * 4 + s4
                self.mm(mps.ap[:, j:j + 1], row.ap[0:1, s4 * 128:(s4 + 1) * 128], self.one1.ap[0:1, 0:1],
                        True, True, reads=[row, self.one1], writes=[(mps, j)], inc=True)
        P.op("dve", lambda e: e.tensor_tensor(self.modT.ap[:], mps.ap[:, 0:96], bT.ap[:], ALU.add),
             reads=[mps, bT], writes=[self.modT])
        P.op("dve", lambda e: e.scalar_tensor_tensor(self.lc.ap[:, 0:16], self.modT.ap[:, 16:32], 1.0, gm.ap[:, 0:16],
                                                     ALU.add, ALU.mult), reads=[self.modT, gm], writes=[self.lc])
        P.op("dve", lambda e: e.scalar_tensor_tensor(self.lc.ap[:, 16:32], self.modT.ap[:, 64:80], 1.0, gm.ap[:, 16:32],
                                                     ALU.add, ALU.mult), reads=[self.modT, gm, self.lc], writes=[self.lc])
        P.release(m0)

    def emit_mod_shard(self, io, modloc_buf, modloc):
        P = self.P
        m0 = P.mark()
        rowt = [P.alloc(f"modrow{i}", [1, 512], F32) for i in range(2)]
        n = 0
        for l in range(DEPTH):
            wada = io["w_ada_sh"][l].rearrange("(kc p) n -> p kc n", p=128)
            for blk in range(6):
                pb = self.ps[n % 2]
                for half in range(2):
                    c0 = blk * 512 + half * 256
                    s, wv = self.wtile(wada[:, :, c0:c0 + 256], KC, 256)
                    for kc in range(KC):
                        self.mm(pb.ap[0:1, half * 256:(half + 1) * 256], self.cact.ap[:, kc:kc + 1], wv[:, kc, :],
                                kc == 0, kc == KC - 1, reads=[s, self.cact], writes=[(pb, half)])
                row = rowt[n % 2]
                P.op("act", lambda e, row=row, pb=pb: e.copy(row.ap[:], pb.ap[0:1, :]), reads=[pb], writes=[row])
                P.dma("sp", modloc_buf, modloc[0:1, l * 3072 + blk * 512: l * 3072 + blk * 512 + 512], row, row.ap[:], out_key=(l, blk))
                n += 1
        P.release(m0)

    def emit_mod_load(self, io, l, modfull_buf, modfull):
        P = self.P
        m0 = P.mark()
        bT = P.alloc("bT", [128, 96], F32)
        P.dma("sp", bT, bT.ap[:], None, io["b_adaT"][l])
        gm = P.alloc("gm", [128, 32], F32)
        P.dma_multi("sp", gm, [(gm.ap[:, 0:16], io["g_mixT"][l]), (gm.ap[:, 16:32], io["g_ffnT"][l])])
        mrow = P.alloc("mrow", [1, 4 * 3072], F32)
        P._emit_waits("sp", P._collect([(modfull_buf, None)], []))
        P.dma_multi("sp", mrow, [(mrow.ap[0:1, r * 3072:(r + 1) * 3072], modfull[r:r + 1, l * 3072:(l + 1) * 3072]) for r in range(4)])
        mraw = self.ps[7]
        for j in range(96):
            self.mm(mraw.ap[:, j:j + 1], mrow.ap[0:1, j * 128:(j + 1) * 128], self.one1.ap[0:1, 0:1],
                    True, True, reads=[mrow, self.one1], writes=[(mraw, j)], inc=True)
        P.op("dve", lambda e: e.tensor_tensor(self.modT.ap[:], mraw.ap[:, 0:96], bT.ap[:], ALU.add), reads=[mraw, bT], writes=[self.modT])
        P.op("dve", lambda e: e.scalar_tensor_tensor(self.lc.ap[:, 0:16], self.modT.ap[:, 16:32], 1.0, gm.ap[:, 0:16],
                                                     ALU.add, ALU.mult), reads=[self.modT, gm], writes=[self.lc])
        P.op("dve", lambda e: e.scalar_tensor_tensor(self.lc.ap[:, 16:32], self.modT.ap[:, 64:80], 1.0, gm.ap[:, 16:32],
                                                     ALU.add, ALU.mult), reads=[self.modT, gm, self.lc], writes=[self.lc])
        P.release(m0)

    def emit_norm(self, tc, hT, hoff, gcol, shcol, gbufs=(), hook=None):
        P = self.P
        t0 = tc * TCW
        m0 = P.mark()
        sq = [P.alloc(f"sq{i}", [128, TCW], BF16) for i in range(3)]
        rstd = P.alloc("rstd", [128, TCW], F32)
        tmp = [P.alloc(f"ntmp{i}", [128, TCW], F32) for i in range(2)]
        h32 = [P.alloc(f"h32_{i}", [128, TCW], F32) for i in range(3)] if hook is not None else None
        pb = self.ps[6]
        gb = list(gbufs)
        for kc in range(KC):
            s = sq[kc % 3]
            P.op("act", lambda e, s=s, kc=kc: e.activation(s.ap[:], self.xT.ap[:, kc, t0:t0 + TCW], AF.Square),
                 reads=[(self.xT, (kc, tc))], writes=[s])
            self.mm(pb.ap[:], self.ones.ap[:], s.ap[:], kc == 0, kc == KC - 1, reads=[s, self.ones], writes=[pb], inc=True)
        P.op("act", lambda e: e.activation(rstd.ap[:], pb.ap[:], AF.Ln, bias=EPS, scale=1.0 / D), reads=[pb], writes=[rstd])
        P.op("act", lambda e: e.activation(rstd.ap[:], rstd.ap[:], AF.Exp, scale=-0.5), reads=[rstd], writes=[rstd])
        for kc in range(KC):
            t = tmp[kc % 2]
            dst = hT.ap[:, kc, hoff:hoff + TCW]
            if shcol is None:
                P.op("dve", lambda e, kc=kc, dst=dst: e.scalar_tensor_tensor(dst, self.xT.ap[:, kc, t0:t0 + TCW], gcol(kc), rstd.ap[:],
                                                                            ALU.mult, ALU.mult),
                     reads=[(self.xT, (kc, tc)), rstd] + gb, writes=[(hT, (kc, tc))])
                continue
            P.op("dve", lambda e, kc=kc, t=t: e.scalar_tensor_tensor(t.ap[:], self.xT.ap[:, kc, t0:t0 + TCW], gcol(kc), rstd.ap[:],
                                                                      ALU.mult, ALU.mult),
                 reads=[(self.xT, (kc, tc)), rstd] + gb, writes=[t])
            if hook is not None:
                hb = h32[kc % 3]
                P.op("act", lambda e, kc=kc, t=t, hb=hb: e.activation(hb.ap[:], t.ap[:], AF.Identity, bias=shcol(kc), scale=1.0),
                     reads=[t] + gb, writes=[hb])
                P.op("pool", lambda e, dst=dst, hb=hb: e.tensor_copy(dst, hb.ap[:]), reads=[hb], writes=[(hT, (kc, tc))])
                hook(kc, hb)
            else:
                P.op("act", lambda e, kc=kc, t=t, dst=dst: e.activation(dst, t.ap[:], AF.Identity, bias=shcol(kc), scale=1.0),
                     reads=[t] + gb, writes=[(hT, (kc, tc))])
        P.release(m0)

    def emit_norm512(self, raw, rbase, out, ooff, gT, goff):
        P = self.P
        m0 = P.mark()
        sq = [P.alloc(f"sqb{i}", [128, TCW], BF16) for i in range(2)]
        rstd = P.alloc("rstdb", [128, TCW], F32)
        pb = self.ps[6]
        for kc in range(4):
            s = sq[kc % 2]
            P.op("act", lambda e, s=s, kc=kc: e.activation(s.ap[:], raw.ap[:, rbase + kc, :], AF.Square), reads=[(raw, rbase + kc)], writes=[s])
            self.mm(pb.ap[:], self.ones.ap[:], s.ap[:], kc == 0, kc == 3, reads=[s, self.ones], writes=[pb], inc=True)
        P.op("act", lambda e: e.activation(rstd.ap[:], pb.ap[:], AF.Ln, bias=EPS, scale=1.0 / 512.0), reads=[pb], writes=[rstd])
        P.op("act", lambda e: e.activation(rstd.ap[:], rstd.ap[:], AF.Exp, scale=-0.5), reads=[rstd], writes=[rstd])
        for kc in range(4):
            P.op("dve", lambda e, kc=kc: e.scalar_tensor_tensor(out.ap[:, kc, ooff:ooff + TCW], raw.ap[:, rbase + kc, :],
                                                                gT.ap[:, goff + kc:goff + kc + 1], rstd.ap[:], ALU.mult, ALU.mult),
                 reads=[(raw, rbase + kc), rstd, gT], writes=[(out, (kc, ooff))])
        P.release(m0)

    def emit_rope(self, p_n, p_sw, t0, dst_buf, dst_ap, tmpa, tmpb):
        P = self.P
        P.op("dve", lambda e: e.tensor_tensor(tmpa.ap[:], p_n.ap[0:64, :], self.cc.ap[:, t0:t0 + TCW], ALU.mult),
             reads=[p_n, self.cc], writes=[tmpa])
        P.op("dve", lambda e: e.tensor_tensor(tmpb.ap[:], p_sw.ap[0:64, :], self.ss.ap[:, t0:t0 + TCW], ALU.mult),
             reads=[p_sw, self.ss], writes=[tmpb])
        P.op("pool", lambda e: e.tensor_tensor(dst_ap, tmpa.ap[:], tmpb.ap[:], ALU.add), reads=[tmpa, tmpb], writes=[dst_buf])

    def emit_A(self, io, tc, qsb, cqn, kvx):
        P = self.P
        t0 = tc * TCW
        gq = P.alloc("gq", [128, 8], F32)
        P.dma_multi("sp", gq, [(gq.ap[:, 0:4], io["q_gT"]), (gq.ap[:, 4:8], io["kv_gT"])])
        hT = P.alloc("hT", [128, KC, TCW], BF16)
        self.emit_norm(tc, hT, 0, lambda kc: self.lc.ap[:, kc:kc + 1], lambda kc: self.modT.ap[:, kc:kc + 1],
                       gbufs=[self.lc, self.modT])
        win = io["w_in"].rearrange("(kc p) n -> p kc n", p=128)
        craw = P.alloc("craw", [128, 8, TCW], F32)
        kst = [P.alloc(f"kst{i}", [128, TCW], BF16) for i in range(2)]
        nst = 0
        for c0 in list(range(0, 2048, 256)) + list(range(OFF_CQ, OFF_KPE, 256)):
            s, wv = self.wtile(win[:, :, c0:c0 + 256], KC, 256)
            for sub in range(2):
                col = c0 + sub * 128
                pb = self.ps[(col // 128) % 4]
                for kc in range(KC):
                    self.mm(pb.ap[:], wv[:, kc, sub * 128:(sub + 1) * 128], hT.ap[:, kc, :], kc == 0, kc == KC - 1,
                            reads=[s, (hT, (kc, tc))], writes=[pb])
                if col < OFF_K:
                    h = col // 128
                    self.evac(lambda e, h=h, pb=pb: e.copy(qsb.ap[:, h, t0:t0 + TCW], pb.ap[:]),
                              lambda e, h=h, pb=pb: e.tensor_copy(qsb.ap[:, h, t0:t0 + TCW], pb.ap[:]),
                              reads=[pb], writes=[(qsb, (h, tc))])
                elif col < OFF_V:
                    h = (col - OFF_K) // 128
                    st = kst[nst % 2]
                    nst += 1
                    self.evac(lambda e, st=st, pb=pb: e.copy(st.ap[:], pb.ap[:]),
                              lambda e, st=st, pb=pb: e.tensor_copy(st.ap[:], pb.ap[:]), reads=[pb], writes=[st])
                    kb_, kap_ = kvx.loc(X_K + h * 1024 + t0, TCW)
                    P.dma("sp", kb_, kap_, st, st.ap[:], out_key=("k", h, tc))
                else:
                    ci = (col - OFF_CQ) // 128
                    self.evac(lambda e, ci=ci, pb=pb: e.copy(craw.ap[:, ci, :], pb.ap[:]),
                              lambda e, ci=ci, pb=pb: e.tensor_copy(craw.ap[:, ci, :], pb.ap[:]),
                              reads=[pb], writes=[(craw, ci)])
        s = self.wslot()
        wv = s.ap[:, 0:KC * 128].rearrange("p (a b) -> p a b", a=KC)
        P.dma_multi("pool", s, [(wv[:, :, 0:64], win[:, :, OFF_KPE:OFF_KPE + 64]),
                                (wv[:, :, 64:96], win[:, :, OFF_KPE + 32:OFF_KPE + 64]),
                                (wv[:, :, 96:128], win[:, :, OFF_KPE:OFF_KPE + 32])])
        pn, psw = self.ps[4], self.ps[5]
        for kc in range(KC):
            self.mm(pn.ap[0:64, :], wv[:, kc, 0:64], hT.ap[:, kc, :], kc == 0, kc == KC - 1, reads=[s, (hT, (kc, tc))], writes=[pn])
        for kc in range(KC):
            self.mm(psw.ap[0:64, :], wv[:, kc, 64:128], hT.ap[:, kc, :], kc == 0, kc == KC - 1, reads=[s, (hT, (kc, tc))], writes=[psw])
        ra = P.alloc("ra", [64, TCW], F32)
        rb = P.alloc("rb", [64, TCW], F32)
        kpst = P.alloc("kpst", [64, TCW], BF16)
        self.emit_rope(pn, psw, t0, kpst, kpst.ap[:], ra, rb)
        kb_, kap_ = kvx.loc(X_KPE + t0, TCW, rows=(0, 64))
        P.dma("sp", kb_, kap_, kpst, kpst.ap[:], out_key=("kpe", tc))
        kb_, kap_ = kvx.loc(X_KPE + t0, TCW, rows=(64, 128))
        P.dma("sp", kb_, kap_, kpst, kpst.ap[:], out_key=("kpe2", tc))
        vst = [P.alloc(f"vst{i}", [128, 2, 4, 128], BF16) for i in range(2)]
        for wi in range(4):
            s, wv = self.wtile(win[:, :, OFF_V + wi * 256:OFF_V + wi * 256 + 256], KC, 256)
            st = vst[wi % 2]
            for mm_ in range(4):
                pb = self.ps[mm_ % 4]
                for kc in range(KC):
                    self.mm(pb.ap[:, 0:256], hT.ap[:, kc, mm_ * 128:(mm_ + 1) * 128], wv[:, kc, :], kc == 0, kc == KC - 1,
                            reads=[s, (hT, (kc, tc))], writes=[pb])
                self.evac(lambda e, st=st, pb=pb, mm_=mm_: e.copy(st.ap[:, :, mm_, :], pb.ap[:, 0:256].rearrange("p (a b) -> p a b", a=2)),
                          lambda e, st=st, pb=pb, mm_=mm_: e.tensor_copy(st.ap[:, :, mm_, :], pb.ap[:, 0:256].rearrange("p (a b) -> p a b", a=2)),
                          reads=[pb], writes=[(st, mm_)])
            kb_, kap_ = kvx.loc(X_V + (2 * wi) * 1024, 2048)
            dst = kap_.rearrange("p (a b) -> p a b", a=2)[:, :, t0:t0 + TCW]
            P.dma("sp", kb_, dst, st, st.ap[:].rearrange("p a m d -> p a (m d)"), out_key=("v", wi, tc))
        ckvn = P.alloc("ckvn", [128, 4, TCW], BF16)
        self.emit_norm512(craw, 0, cqn, t0, gq, 0)
        self.emit_norm512(craw, 4, ckvn, 0, gq, 4)
        wukv = io["w_ukv"].rearrange("(kc p) n -> p kc n", p=128)
        vmst = [P.alloc(f"vmst{i}", [128, 4, 128], BF16) for i in range(2)]
        for h in range(NH):
            s, wv = self.wtile(wukv[:, :, h * 256:(h + 1) * 256], 4, 256)
            pb = self.ps[h % 2]
            for kc in range(4):
                self.mm(pb.ap[:], wv[:, kc, 0:128], ckvn.ap[:, kc, :], kc == 0, kc == 3, reads=[s, ckvn], writes=[pb])
            st = kst[nst % 2]
            nst += 1
            self.evac(lambda e, st=st, pb=pb: e.copy(st.ap[:], pb.ap[:]),
                      lambda e, st=st, pb=pb: e.tensor_copy(st.ap[:], pb.ap[:]), reads=[pb], writes=[st])
            kb_, kap_ = kvx.loc(X_KN + h * 1024 + t0, TCW)
            P.dma("sp", kb_, kap_, st, st.ap[:], out_key=("kn", h, tc))
            pv = self.ps[2 + h % 2]
            for mm_ in range(4):
                for kc in range(4):
                    self.mm(pv.ap[:, mm_ * 128:(mm_ + 1) * 128], ckvn.ap[:, kc, mm_ * 128:(mm_ + 1) * 128], wv[:, kc, 128:256],
                            kc == 0, kc == 3, reads=[s, ckvn], writes=[(pv, mm_)], inc=(kc == 3))
            vs = vmst[h % 2]
            self.evac(lambda e, vs=vs, pv=pv: e.copy(vs.ap[:].rearrange("p a b -> p (a b)"), pv.ap[:]),
                      lambda e, vs=vs, pv=pv: e.tensor_copy(vs.ap[:].rearrange("p a b -> p (a b)"), pv.ap[:]),
                      reads=[pv], writes=[vs])
            kb_, kap_ = kvx.loc(X_VM + h * 1024 + t0, TCW)
            P.dma("sp", kb_, kap_, vs, vs.ap[:].rearrange("p a b -> p (a b)"), out_key=("vm", h, tc))

    def emit_attn(self, io, tc, qsb, cqn, kvx, osb, omla):
        P = self.P
        t0 = tc * TCW
        nchunk = tc + 1
        nblk = 16 * nchunk
        kbuf = [P.alloc(f"kbuf{i}", [128, 4, 512], BF16) for i in range(2)]
        vbuf = [P.alloc(f"vbuf{i}", [128, 4, 512], BF16) for i in range(2)]
        qpos = self.qposb.ap[:, t0:t0 + TCW]
        nld = [0]

        def load_chunk(xk, xv, h, ch):
            kb, vb = kbuf[nld[0] % 2], vbuf[nld[0] % 2]
            nld[0] += 1
            c0 = h * 1024 + ch * 512
            fb_, fap_ = kvx.full(xk + c0, 512)
            P.dma("sp", kb, kb.ap[:], fb_, fap_)
            fb_, fap_ = kvx.full(xv + c0, 512)
            P.dma("sp", vb, vb.ap[:], fb_, fap_)
            return kb, vb

        m1 = P.mark()
        ebuf = [P.alloc(f"ebuf{i}", [128, TCW], F32) for i in range(3)]
        spb = [P.alloc(f"spb{i}", [128, TCW], BF16) for i in range(3)]
        wbuf = [P.alloc(f"wbuf{i}", [128, TCW], F32) for i in range(2)]
        abuf3 = [P.alloc(f"abuf3_{i}", [128, TCW], BF16) for i in range(3)]
        cacc = [P.alloc(f"cacc{i}", [128, TCW], BF16) for i in range(2)]
        for h in range(NH):
            ops_ = self.ps[4 + h % 2]
            q = qsb.ap[:, h, t0:t0 + TCW]
            units = []
            for ch in range(nchunk - 1, -1, -1):
                for ml in range(3, -1, -1):
                    for r in range(3, -1, -1):
                        units.append((ch, ml, r, 4 * (4 * ch + ml) + r))
            chunk_bufs = {}

            def s1(n):
                ch, ml, r, i = units[n]
                if ch not in chunk_bufs:
                    chunk_bufs[ch] = load_chunk(X_K, X_V, h, ch)
                kb, vb = chunk_bufs[ch]
                zps = self.ps[n % 2]
                eb, sp_ = ebuf[n % 3], spb[n % 3]
                ks = slice(ml * 128, (ml + 1) * 128)
                self.mm(zps.ap[:], kb.ap[:, r, ks], q, True, True, reads=[kb, (qsb, (h, tc))], writes=[zps])
                P.op("act", lambda e: e.activation(eb.ap[:], zps.ap[:], AF.Exp, scale=SB_SCALE), reads=[zps], writes=[eb])
                if i >= 16 * tc:
                    P.op("dve", lambda e: e.scalar_tensor_tensor(eb.ap[:], qpos, self.kposc.ap[:, i:i + 1], eb.ap[:], ALU.is_gt, ALU.mult),
                         reads=[eb, self.qposb, self.kposc], writes=[eb])
                P.op("act", lambda e: e.activation(sp_.ap[:], eb.ap[:], AF.Ln, bias=1.0, scale=1.0), reads=[eb], writes=[sp_])

            def s2(n):
                sps = self.ps[2 + n % 2]
                eb, sp_, wb, ab = ebuf[n % 3], spb[n % 3], wbuf[n % 2], abuf3[n % 3]
                cprev, cnew = cacc[(n + 1) % 2], cacc[n % 2]
                self.mm(sps.ap[:], self.tincl.ap[:], sp_.ap[:], True, n == 0, reads=[sp_, self.tincl], writes=[sps], inc=(n == 0))
                if n > 0:
                    self.mm(sps.ap[:], self.ones.ap[:], cprev.ap[:], False, True, reads=[cprev, self.ones], writes=[sps])
                P.op("act", lambda e: e.activation(wb.ap[:], sps.ap[:], AF.Exp, scale=-1.0), reads=[sps], writes=[wb])
                P.op("dve", lambda e: e.tensor_tensor(ab.ap[:], eb.ap[:], wb.ap[:], ALU.mult), reads=[eb, wb], writes=[ab])
                if n == 0:
                    P.op("pool", lambda e: e.tensor_copy(cnew.ap[:], sp_.ap[:]), reads=[sp_], writes=[cnew])
                elif n < nblk - 1:
                    P.op("pool", lambda e: e.tensor_tensor(cnew.ap[:], cprev.ap[:], sp_.ap[:], ALU.add), reads=[sp_, cprev], writes=[cnew])

            def s3(n):
                ch, ml, r, i = units[n]
                kb, vb = chunk_bufs[ch]
                ab = abuf3[n % 3]
                ks = slice(ml * 128, (ml + 1) * 128)
                self.mm(ops_.ap[:], vb.ap[:, r, ks], ab.ap[:], n == 0, n == nblk - 1, reads=[vb, ab], writes=[ops_], inc=True)

            for step in range(nblk + 2):
                if step < nblk:
                    s1(step)
                if 0 <= step - 1 < nblk:
                    s2(step - 1)
                if 0 <= step - 2 < nblk:
                    s3(step - 2)
            self.evac(lambda e, h=h, ops_=ops_: e.copy(osb.ap[:, h, :], ops_.ap[:]),
                      lambda e, h=h, ops_=ops_: e.tensor_copy(osb.ap[:, h, :], ops_.ap[:]), reads=[ops_], writes=[(osb, h)])
        P.release(m1)
        kpe = P.alloc("kpe", [64, 4, 1024], BF16)
        nk = nchunk * 512
        fb_, fap_ = kvx.full(X_KPE, nk, rows=(0, 64))
        P.dma("sp", kpe, kpe.ap[:, :, 0:nk], fb_, fap_)
        wuq = io["w_uq"].rearrange("(kc p) n -> p kc n", p=128)
        qn = [P.alloc(f"qn{i}", [128, TCW], BF16) for i in range(2)]
        qp = [P.alloc(f"qp{i}", [64, TCW], BF16) for i in range(2)]
        ra = P.alloc("ra2", [64, TCW], F32)
        rb = P.alloc("rb2", [64, TCW], F32)
        lsum = P.alloc("lsum", [128, TCW], F32)
        abuf3m = [P.alloc(f"abuf3m_{i}", [128, TCW], BF16) for i in range(3)]
        for h in range(NH):
            s = self.wslot()
            wv = s.ap[:, 0:4 * 256].rearrange("p (a b) -> p a b", a=4)
            P.dma_multi("pool", s, [(wv[:, :, 0:192], wuq[:, :, h * 192:h * 192 + 192]),
                                    (wv[:, :, 192:224], wuq[:, :, h * 192 + 160:h * 192 + 192]),
                                    (wv[:, :, 224:256], wuq[:, :, h * 192 + 128:h * 192 + 160])])
            p0, p1, p2 = self.ps[0], self.ps[1], self.ps[2]
            for kc in range(4):
                self.mm(p0.ap[:], wv[:, kc, 0:128], cqn.ap[:, kc, t0:t0 + TCW], kc == 0, kc == 3, reads=[s, cqn], writes=[p0])
            for kc in range(4):
                self.mm(p1.ap[0:64, :], wv[:, kc, 128:192], cqn.ap[:, kc, t0:t0 + TCW], kc == 0, kc == 3, reads=[s, cqn], writes=[p1])
            for kc in range(4):
                self.mm(p2.ap[0:64, :], wv[:, kc, 192:256], cqn.ap[:, kc, t0:t0 + TCW], kc == 0, kc == 3, reads=[s, cqn], writes=[p2])
            qn_, qp_ = qn[h % 2], qp[h % 2]
            P.op("act", lambda e, qn_=qn_, p0=p0: e.copy(qn_.ap[:], p0.ap[:]), reads=[p0], writes=[qn_])
            self.emit_rope(p1, p2, t0, qp_, qp_.ap[:], ra, rb)
            ops_ = self.ps[4 + h % 2]
            sums = self.ps[6 + h % 2]
            units = [(ch, ml, r, 4 * (4 * ch + ml) + r) for ch in range(nchunk) for ml in range(4) for r in range(4)]
            chunk_bufs = {}

            def m1_(n):
                ch, ml, r, i = units[n]
                if ch not in chunk_bufs:
                    chunk_bufs[ch] = load_chunk(X_KN, X_VM, h, ch)
                kb, vb = chunk_bufs[ch]
                sps = self.ps[n % 3] if False else self.ps[[0, 1, 3][n % 3]]
                ab = abuf3m[n % 3]
                ks = slice(ml * 128, (ml + 1) * 128)
                kg = slice((4 * ch + ml) * 128, (4 * ch + ml + 1) * 128)
                self.mm(sps.ap[:], kb.ap[:, r, ks], qn_.ap[:], True, False, reads=[kb, qn_], writes=[sps], inc=False)
                self.mm(sps.ap[:], kpe.ap[:, r, kg], qp_.ap[:], False, True, reads=[kpe, qp_], writes=[sps])
                P.op("act", lambda e: e.activation(ab.ap[:], sps.ap[:], AF.Exp, scale=MLA_SCALE), reads=[sps], writes=[ab])
                if i >= 16 * tc:
                    P.op("dve", lambda e: e.scalar_tensor_tensor(ab.ap[:], qpos, self.kposc.ap[:, i:i + 1], ab.ap[:], ALU.is_ge, ALU.mult),
                         reads=[ab, self.qposb, self.kposc], writes=[ab])

            def m2_(n):
                ch, ml, r, i = units[n]
                kb, vb = chunk_bufs[ch]
                ab = abuf3m[n % 3]
                ks = slice(ml * 128, (ml + 1) * 128)
                self.mm(ops_.ap[:], vb.ap[:, r, ks], ab.ap[:], n == 0, n == nblk - 1, reads=[vb, ab], writes=[ops_], inc=False)
                self.mm(sums.ap[:], self.ones.ap[:], ab.ap[:], n == 0, n == nblk - 1, reads=[self.ones, ab], writes=[sums], inc=True)

            for step in range(nblk + 1):
                if step < nblk:
                    m1_(step)
                if 0 <= step - 1 < nblk:
                    m2_(step - 1)
            P.op("act", lambda e, sums=sums: e.activation(lsum.ap[:], sums.ap[:], AF.Ln), reads=[sums], writes=[lsum])
            P.op("act", lambda e: e.activation(lsum.ap[:], lsum.ap[:], AF.Exp, scale=-1.0), reads=[lsum], writes=[lsum])
            P.op("dve", lambda e, h=h, ops_=ops_: e.tensor_tensor(omla.ap[:, h, :], ops_.ap[:], lsum.ap[:], ALU.mult),
                 reads=[ops_, lsum], writes=[(omla, h)])

    def emit_C(self, io, tc, osb, omla):
        P = self.P
        t0 = tc * TCW
        hT = P.alloc("hTc", [128, KC, TCW], BF16)
        self.emit_norm(tc, hT, 0, lambda kc: self.lc.ap[:, kc:kc + 1], lambda kc: self.modT.ap[:, kc:kc + 1],
                       gbufs=[self.lc, self.modT])
        yT = P.alloc("yT", [128, KC, TCW], BF16)
        win = io["w_in"].rearrange("(kc p) n -> p kc n", p=128)
        wsu = io["w_sb_up"].rearrange("(kc p) n -> p kc n", p=128)
        wmu = io["w_mla_up"].rearrange("(kc p) n -> p kc n", p=128)
        wo = io["w_o"].rearrange("(kc p) n -> p kc n", p=128)
        sg = [P.alloc(f"sg{i}", [128, TCW], F32) for i in range(4)]
        y1 = [P.alloc(f"y1{i}", [128, TCW], F32) for i in range(4)]
        for pair in range(8):
            s1, w1 = self.wtile(win[:, :, OFF_GSB + pair * 256:OFF_GSB + pair * 256 + 256], KC, 256)
            s2, w2 = self.wtile(win[:, :, OFF_GMLA + pair * 256:OFF_GMLA + pair * 256 + 256], KC, 256)
            s3, w3 = self.wtile(wsu[:, :, pair * 256:pair * 256 + 256], 8, 256)
            s4, w4 = self.wtile(wmu[:, :, pair * 256:pair * 256 + 256], 8, 256)
            for sub in range(2):
                dc = pair * 2 + sub
                b = (dc % 2) * 4
                pgs, pgm, pus, pum = self.ps[b], self.ps[b + 1], self.ps[b + 2], self.ps[b + 3]
                cs = slice(sub * 128, (sub + 1) * 128)
                for kc in range(KC):
                    self.mm(pgs.ap[:], w1[:, kc, cs], hT.ap[:, kc, :], kc == 0, kc == KC - 1, reads=[s1, hT], writes=[pgs])
                for kc in range(KC):
                    self.mm(pgm.ap[:], w2[:, kc, cs], hT.ap[:, kc, :], kc == 0, kc == KC - 1, reads=[s2, hT], writes=[pgm])
                for kc in range(8):
                    self.mm(pus.ap[:], w3[:, kc, cs], osb.ap[:, kc, :], kc == 0, kc == 7, reads=[s3, osb], writes=[pus])
                for kc in range(8):
                    self.mm(pum.ap[:], w4[:, kc, cs], omla.ap[:, kc, :], kc == 0, kc == 7, reads=[s4, omla], writes=[pum])
                ga, gb_ = sg[(dc % 2) * 2], sg[(dc % 2) * 2 + 1]
                ya, yb = y1[(dc % 2) * 2], y1[(dc % 2) * 2 + 1]
                P.op("act", lambda e, ga=ga, pgs=pgs: e.activation(ga.ap[:], pgs.ap[:], AF.Sigmoid), reads=[pgs], writes=[ga])
                P.op("act", lambda e, gb_=gb_, pgm=pgm: e.activation(gb_.ap[:], pgm.ap[:], AF.Sigmoid), reads=[pgm], writes=[gb_])
                P.op("dve", lambda e, ya=ya, ga=ga, pus=pus: e.tensor_tensor(ya.ap[:], ga.ap[:], pus.ap[:], ALU.mult), reads=[ga, pus], writes=[ya])
                P.op("dve", lambda e, yb=yb, gb_=gb_, pum=pum: e.tensor_tensor(yb.ap[:], gb_.ap[:], pum.ap[:], ALU.mult), reads=[gb_, pum], writes=[yb])
                P.op("pool", lambda e, dc=dc, ya=ya, yb=yb: e.tensor_tensor(yT.ap[:, dc, :], ya.ap[:], yb.ap[:], ALU.add),
                     reads=[ya, yb], writes=[(yT, dc)])
        for pair in range(8):
            s1, w1 = self.wtile(wo[:, :, pair * 256:pair * 256 + 256], KC, 256)
            for sub in range(2):
                dc = pair * 2 + sub
                pb = self.ps[dc % 4]
                for kc in range(KC):
                    self.mm(pb.ap[:], w1[:, kc, sub * 128:(sub + 1) * 128], yT.ap[:, kc, :], kc == 0, kc == KC - 1,
                            reads=[s1, (yT, kc)], writes=[pb])
                P.op("dve", lambda e, dc=dc, pb=pb: e.scalar_tensor_tensor(self.xT.ap[:, dc, t0:t0 + TCW], pb.ap[:],
                                                                          self.modT.ap[:, 32 + dc:33 + dc],
                                                                          self.xT.ap[:, dc, t0:t0 + TCW], ALU.mult, ALU.add),
                     reads=[pb, self.modT, (self.xT, (dc, tc))], writes=[(self.xT, (dc, tc))])

    def emit_ffn(self, io, moe):
        P = self.P
        m0 = P.mark()
        h2 = P.alloc("h2", [128, KC, TL], BF16)
        gbt = None
        g2cols = [self.lc, self.modT]
        if moe:
            gbt = P.alloc("gbt", [128, NE, TL], BF16)
        m1 = P.mark()
        if moe:
            lgT = P.alloc("lgT", [8, TL], F32)
            gT = P.alloc("gT", [8, TL], F32)
            wr = P.alloc("wr", [128, KC, 8], F32)
            ident = P.alloc("ident", [128, 128], F32)
            sel = P.alloc("sel", [8, 8, 128], F32)
            onesf = P.alloc("onesf", [128, 1024], F32)
            P.op("dve", lambda e: e.memset(onesf.ap[:], 1.0), writes=[onesf])
            P.op("pool", lambda e: e.affine_select(ident.ap[:], onesf.ap[:, 0:128], [[-1, 128]], ALU.is_equal, 0.0,
                                                   base=0, channel_multiplier=1), reads=[onesf], writes=[ident])
            P.op("pool", lambda e: e.affine_select(sel.ap[:], onesf.ap[0:8, :].rearrange("p (a b) -> p a b", a=8),
                                                   [[1, 8], [0, 128]], ALU.is_equal, 0.0, base=0, channel_multiplier=-1),
                 reads=[onesf], writes=[sel])
            P.dma("sp", wr, wr.ap[:], None, io["w_routerT"])
        for tc in range(NTC):
            hook = None
            if moe:
                pbr = self.ps[tc]

                def hook(kc, hb, pbr=pbr):
                    self.mm(pbr.ap[0:8, :], wr.ap[:, kc, :], hb.ap[:], kc == 0, kc == KC - 1, reads=[wr, hb], writes=[pbr], inc=True)
            self.emit_norm(tc, h2, tc * TCW, lambda kc: self.lc.ap[:, 16 + kc:17 + kc], lambda kc: self.modT.ap[:, 48 + kc:49 + kc],
                           gbufs=g2cols, hook=hook)
            if moe:
                P.op("act", lambda e, tc=tc, pbr=pbr: e.copy(lgT.ap[:, tc * TCW:(tc + 1) * TCW], pbr.ap[0:8, :]), reads=[pbr], writes=[(lgT, tc)])
        if moe:
            lg = P.alloc("lg", [128, 8, 8], F32)
            mx = P.alloc("mx", [128, 8, 8], F32)
            gts = P.alloc("gts", [128, 8, 8], F32)
            sc = P.alloc("sc", [128, 8, 4], F32)
            for blk in range(8):
                pb = self.ps[2 + blk % 2]
                P.op("pe", lambda e, pb=pb, blk=blk: e.transpose(pb.ap[:, 0:8], lgT.ap[0:8, blk * 128:(blk + 1) * 128], ident.ap[0:8, 0:8]),
                     reads=[lgT, ident], writes=[pb])
                P.op("dve", lambda e, pb=pb, blk=blk: e.tensor_copy(lg.ap[:, blk, :], pb.ap[:, 0:8]), reads=[pb], writes=[(lg, blk)])
                P.op("dve", lambda e, blk=blk: e.max(mx.ap[:, blk, :], lg.ap[:, blk, :]), reads=[(lg, blk)], writes=[(mx, blk)])
                P.op("dve", lambda e, blk=blk: e.tensor_scalar(sc.ap[:, blk, 0:1], mx.ap[:, blk, 0:1], -1.0, None, ALU.mult),
                     reads=[(mx, blk)], writes=[(sc, blk)])
                P.op("dve", lambda e, blk=blk: e.tensor_tensor(sc.ap[:, blk, 1:2], mx.ap[:, blk, 1:2], mx.ap[:, blk, 0:1], ALU.subtract),
                     reads=[(mx, blk), (sc, blk)], writes=[(sc, blk)])
                P.op("act", lambda e, blk=blk: e.activation(sc.ap[:, blk, 2:3], sc.ap[:, blk, 1:2], AF.Exp), reads=[(sc, blk)], writes=[(sc, blk)])
                P.op("dve", lambda e, blk=blk: e.tensor_scalar(sc.ap[:, blk, 2:3], sc.ap[:, blk, 2:3], 1.0, None, ALU.add),
                     reads=[(sc, blk)], writes=[(sc, blk)])
                P.op("dve", lambda e, blk=blk: e.reciprocal(sc.ap[:, blk, 3:4], sc.ap[:, blk, 2:3]), reads=[(sc, blk)], writes=[(sc, blk)])
                P.op("act", lambda e, blk=blk: e.activation(gts.ap[:, blk, :], lg.ap[:, blk, :], AF.Exp, bias=sc.ap[:, blk, 0:1], scale=1.0),
                     reads=[(lg, blk), (sc, blk)], writes=[(gts, blk)])
                P.op("dve", lambda e, blk=blk: e.scalar_tensor_tensor(gts.ap[:, blk, :], lg.ap[:, blk, :], mx.ap[:, blk, 1:2], gts.ap[:, blk, :],
                                                                      ALU.is_ge, ALU.mult),
                     reads=[(lg, blk), (mx, blk), (gts, blk)], writes=[(gts, blk)])
                P.op("dve", lambda e, blk=blk: e.tensor_scalar(gts.ap[:, blk, :], gts.ap[:, blk, :], sc.ap[:, blk, 3:4], None, ALU.mult),
                     reads=[(gts, blk), (sc, blk)], writes=[(gts, blk)])
                pt = self.ps[4 + blk % 2]
                P.op("pe", lambda e, pt=pt, blk=blk: e.transpose(pt.ap[0:8, 0:128], gts.ap[:, blk, :], ident.ap[:]),
                     reads=[(gts, blk), ident], writes=[pt])
                P.op("dve", lambda e, pt=pt, blk=blk: e.tensor_copy(gT.ap[:, blk * 128:(blk + 1) * 128], pt.ap[0:8, 0:128]),
                     reads=[pt], writes=[(gT, blk)])
            for ex in range(NE):
                for tc in range(NTC):
                    pb = self.ps[6 + (ex * 2 + tc) % 2]
                    self.mm(pb.ap[:], sel.ap[:, ex, :], gT.ap[:, tc * TCW:(tc + 1) * TCW], True, True, reads=[sel, gT], writes=[pb])
                    self.evac(lambda e, pb=pb, ex=ex, tc=tc: e.copy(gbt.ap[:, ex, tc * TCW:(tc + 1) * TCW], pb.ap[:]),
                              lambda e, pb=pb, ex=ex, tc=tc: e.tensor_copy(gbt.ap[:, ex, tc * TCW:(tc + 1) * TCW], pb.ap[:]),
                              reads=[pb], writes=[(gbt, (ex, tc))])
        P.release(m1)
        act = P.alloc("actT", [128, GMAX, TL], BF16)
        sil = [P.alloc(f"sil{i}", [128, TCW], BF16) for i in range(2)]
        tmpa = [P.alloc(f"ftmp{i}", [128, TCW], BF16) for i in range(2)]
        nexp = NE if moe else 1
        n = 0
        for ex in range(nexp):
            if moe:
                wg = io["w_exp_gate"][ex].rearrange("(kc p) n -> p kc n", p=128)
                wu = io["w_exp_up"][ex].rearrange("(kc p) n -> p kc n", p=128)
                wd = io["w_exp_down"][ex].rearrange("(fc p) n -> p fc n", p=128)
            else:
                wg = io["w_ffn_gate"].rearrange("(kc p) n -> p kc n", p=128)
                wu = io["w_ffn_up"].rearrange("(kc p) n -> p kc n", p=128)
                wd = io["w_ffn_down"].rearrange("(fc p) n -> p fc n", p=128)
            fbase = 0
            for gsz in GROUPS:
                for pr in range(gsz // 2):
                    f0 = (fbase + 2 * pr) * 128
                    sG, wG = self.wtile(wg[:, :, f0:f0 + 256], KC, 256)
                    sU, wU = self.wtile(wu[:, :, f0:f0 + 256], KC, 256)
                    for sub in range(2):
                        fc = 2 * pr + sub
                        cs = slice(sub * 128, (sub + 1) * 128)
                        for tc in range(NTC):
                            n += 1
                            pg, pu = self.ps[(n % 2) * 2], self.ps[(n % 2) * 2 + 1]
                            for kc in range(KC):
                                self.mm(pg.ap[:], wG[:, kc, cs], h2.ap[:, kc, tc * TCW:(tc + 1) * TCW], kc == 0, kc == KC - 1,
                                        reads=[sG, (h2, (kc, tc))], writes=[pg])
                            for kc in range(KC):
                                self.mm(pu.ap[:], wU[:, kc, cs], h2.ap[:, kc, tc * TCW:(tc + 1) * TCW], kc == 0, kc == KC - 1,
                                        reads=[sU, (h2, (kc, tc))], writes=[pu])
                            sl = sil[n % 2]
                            P.op("act", lambda e, sl=sl, pg=pg: e.activation(sl.ap[:], pg.ap[:], AF.Silu), reads=[pg], writes=[sl])
                            dst = act.ap[:, fc, tc * TCW:(tc + 1) * TCW]
                            if moe:
                                tt = tmpa[n % 2]
                                P.op("dve", lambda e, tt=tt, sl=sl, pu=pu: e.tensor_tensor(tt.ap[:], sl.ap[:], pu.ap[:], ALU.mult),
                                     reads=[sl, pu], writes=[tt])
                                P.op("pool", lambda e, tt=tt, dst=dst, ex=ex, tc=tc: e.tensor_tensor(dst, tt.ap[:], gbt.ap[:, ex, tc * TCW:(tc + 1) * TCW], ALU.mult),
                                     reads=[tt, (gbt, (ex, tc))], writes=[(act, (fc, tc))])
                            else:
                                P.op("dve", lambda e, dst=dst, sl=sl, pu=pu: e.tensor_tensor(dst, sl.ap[:], pu.ap[:], ALU.mult),
                                     reads=[sl, pu], writes=[(act, (fc, tc))])
                for pair in range(8):
                    s, wv = self.wtile(wd[:, fbase:fbase + gsz, pair * 256:pair * 256 + 256], gsz, 256)
                    for sub in range(2):
                        dc = pair * 2 + sub
                        for tc in range(NTC):
                            pb = self.ps[4 + (dc * 2 + tc) % 4]
                            for fc in range(gsz):
                                self.mm(pb.ap[:], wv[:, fc, sub * 128:(sub + 1) * 128], act.ap[:, fc, tc * TCW:(tc + 1) * TCW],
                                        fc == 0, fc == gsz - 1, reads=[s, (act, (fc, tc))], writes=[pb])
                            P.op("dve", lambda e, dc=dc, tc=tc, pb=pb: e.scalar_tensor_tensor(
                                self.xT.ap[:, dc, tc * TCW:(tc + 1) * TCW], pb.ap[:], self.modT.ap[:, 80 + dc:81 + dc],
                                self.xT.ap[:, dc, tc * TCW:(tc + 1) * TCW], ALU.mult, ALU.add),
                                reads=[pb, self.modT, (self.xT, (dc, tc))], writes=[(self.xT, (dc, tc))])
                fbase += gsz
        P.release(m0)

    def emit_final(self, io, out_ap):
        P = self.P
        m0 = P.mark()
        fg = P.alloc("fg", [128, KC], F32)
        P.dma("sp", fg, fg.ap[:], None, io["final_gT"])
        refs = []
        for tc in range(NTC):
            m1 = P.mark()
            o32 = P.alloc("o32", [128, KC, TCW], F32)
            self.emit_norm(tc, o32, 0, lambda kc: fg.ap[:, kc:kc + 1], None, gbufs=[fg])
            refs.append(P.dma("sp", None, out_ap[:, :, tc * TCW:(tc + 1) * TCW], o32, o32.ap[:]))
            P.release(m1)
        P.release(m0)
        return refs


def _dram_in(nc, name, shape, dt):
    return nc.dram_tensor(name, list(shape), dt, kind="ExternalInput").ap()


def _dram_out(nc, name, shape, dt):
    return nc.dram_tensor(name, list(shape), dt, kind="ExternalOutput").ap()


def build_A():
    nc = bass.Bass("TRN2", target_bir_lowering=False)
    io = {
        "xT": _dram_in(nc, "xT", [128, KC, TL], F32),
        "cT": _dram_in(nc, "cT", [128, KC], F32),
        "pos": _dram_in(nc, "pos", [64, TL], I32),
        "qpos": _dram_in(nc, "qpos", [128, TL], I32),
        "w_ada": _dram_in(nc, "w_ada", [D, 6 * D], F32),
        "b_adaT": _dram_in(nc, "b_adaT", [128, 96], F32),
        "g_mixT": _dram_in(nc, "g_mixT", [128, KC], F32),
        "g_ffnT": _dram_in(nc, "g_ffnT", [128, KC], F32),
        "q_gT": _dram_in(nc, "q_gT", [128, 4], F32),
        "kv_gT": _dram_in(nc, "kv_gT", [128, 4], F32),
        "w_in": _dram_in(nc, "w_in", [D, IN_COLS], F32),
        "w_ukv": _dram_in(nc, "w_ukv", [512, 2048], F32),
    }
    kvloc = _dram_out(nc, "kvloc", [128, XW], BF16)
    qsb_o = _dram_out(nc, "qsb", [128, NH, TL], BF16)
    cqn_o = _dram_out(nc, "cqn", [128, 4, TL], BF16)
    mod_o = _dram_out(nc, "modo", [128, 128], F32)
    k = K(nc)
    P = k.P
    P.dma("sp", k.xT, k.xT.ap[:], None, io["xT"])
    k.emit_setup(io)
    k.emit_mod(io)
    qsb = P.alloc("qsb", [128, NH, TL], BF16, phase=False)
    cqn = P.alloc("cqn", [128, 4, TL], BF16, phase=False)
    kvb = Buf("kvloc")
    kvx = KVX([kvloc], [kvb], XW)
    for tc in range(NTC):
        m = P.mark()
        k.emit_A(io, tc, qsb, cqn, kvx)
        P.release(m)
    refs = []
    refs.append(P.dma("sp", None, qsb_o, qsb, qsb.ap[:]))
    refs.append(P.dma("sp", None, cqn_o, cqn, cqn.ap[:]))
    refs.append(P.dma("sp", None, mod_o[:, 0:96], k.modT, k.modT.ap[:]))
    refs.append(P.dma("sp", None, mod_o[:, 96:128], k.lc, k.lc.ap[:]))
    for d in P.state.get(kvb.id, {}).values():
        if d[0] is not None:
            refs.append(d[0])
    P.finish(refs)
    P.emit()
    return nc, P


def build_B(moe, last):
    nc = bass.Bass("TRN2", target_bir_lowering=False)
    io = {
        "xT": _dram_in(nc, "xT", [128, KC, TL], F32),
        "cT": _dram_in(nc, "cT", [128, KC], F32),
        "pos": _dram_in(nc, "pos", [64, TL], I32),
        "qpos": _dram_in(nc, "qpos", [128, TL], I32),
        "kvfull": _dram_in(nc, "kvfull", [512, XW], BF16),
        "qsb": _dram_in(nc, "qsb", [128, NH, TL], BF16),
        "cqn": _dram_in(nc, "cqn", [128, 4, TL], BF16),
        "modo": _dram_in(nc, "modo", [128, 128], F32),
        "w_in": _dram_in(nc, "w_in", [D, IN_COLS], F32),
        "w_uq": _dram_in(nc, "w_uq", [512, 1536], F32),
        "w_sb_up": _dram_in(nc, "w_sb_up", [1024, D], F32),
        "w_mla_up": _dram_in(nc, "w_mla_up", [1024, D], F32),
        "w_o": _dram_in(nc, "w_o", [D, D], F32),
    }
    if moe:
        io["w_routerT"] = _dram_in(nc, "w_routerT", [128, KC, 8], F32)
        io["w_exp_gate"] = _dram_in(nc, "w_exp_gate", [NE, D, DFF], F32)
        io["w_exp_up"] = _dram_in(nc, "w_exp_up", [NE, D, DFF], F32)
        io["w_exp_down"] = _dram_in(nc, "w_exp_down", [NE, DFF, D], F32)
    else:
        io["w_ffn_gate"] = _dram_in(nc, "w_ffn_gate", [D, DFF], F32)
        io["w_ffn_up"] = _dram_in(nc, "w_ffn_up", [D, DFF], F32)
        io["w_ffn_down"] = _dram_in(nc, "w_ffn_down", [DFF, D], F32)
    if last:
        io["final_gT"] = _dram_in(nc, "final_gT", [128, KC], F32)
    xo = _dram_out(nc, "xo", [128, KC, TL], F32)
    k = K(nc)
    P = k.P
    P.dma("sp", k.xT, k.xT.ap[:], None, io["xT"])
    k.emit_setup(io)
    P.dma("sp", k.modT, k.modT.ap[:], None, io["modo"][:, 0:96])
    P.dma("sp", k.lc, k.lc.ap[:], None, io["modo"][:, 96:128])
    base = P.mark()
    qsb = P.alloc("qsb", [128, NH, TL], BF16)
    cqn = P.alloc("cqn", [128, 4, TL], BF16)
    P.dma("sp", qsb, qsb.ap[:], None, io["qsb"])
    P.dma("sp", cqn, cqn.ap[:], None, io["cqn"])
    kvb = Buf("kvfull")
    kvx = KVX([io["kvfull"]], [kvb], XW)
    for tc in range(NTC):
        m = P.mark()
        osb = P.alloc("osb", [128, NH, TCW], BF16)
        omla = P.alloc("omla", [128, NH, TCW], BF16)
        m2 = P.mark()
        k.emit_attn(io, tc, qsb, cqn, kvx, osb, omla)
        P.release(m2)
        k.emit_C(io, tc, osb, omla)
        P.release(m)
    P.release(base)
    k.emit_ffn(io, moe)
    if last:
        refs = k.emit_final(io, xo)
    else:
        refs = [P.dma("sp", None, xo, k.xT, k.xT.ap[:])]
    P.finish(refs)
    P.emit()
    return nc, P


def _layer_io(io, l):
    jj = l // 2
    d = {"w_in": io["w_in"][l], "w_ukv": io["w_ukv"][l], "w_uq": io["w_uq"][l], "w_sb_up": io["w_sb_up"][l],
         "w_mla_up": io["w_mla_up"][l], "w_o": io["w_o"][l], "q_gT": io["q_gT"][l], "kv_gT": io["kv_gT"][l]}
    if l % 2 == 1:
        d["w_routerT"] = io["w_routerT"][jj]
        d["w_exp_gate"] = io["w_exp_gate"][jj]
        d["w_exp_up"] = io["w_exp_up"][jj]
        d["w_exp_down"] = io["w_exp_down"][jj]
    else:
        d["w_ffn_gate"] = io["w_ffn_gate"][jj]
        d["w_ffn_up"] = io["w_ffn_up"][jj]
        d["w_ffn_down"] = io["w_ffn_down"][jj]
    return d


GROUPS4 = [[0, 1, 2, 3], [4, 5, 6, 7]]


def build_fused(depth=DEPTH, final=True):
    nc = bass.Bass("TRN2", target_bir_lowering=False, num_devices=8)
    nd, nm = (depth + 1) // 2, depth // 2
    io = {
        "xT": _dram_in(nc, "xT", [128, KC, TL], F32),
        "cT": _dram_in(nc, "cT", [128, KC], F32),
        "pos": _dram_in(nc, "pos", [64, TL], I32),
        "qpos": _dram_in(nc, "qpos", [128, TL], I32),
        "w_ada_sh": _dram_in(nc, "w_ada_sh", [DEPTH, D, 3072], F32),
        "b_adaT": _dram_in(nc, "b_adaT", [DEPTH, 128, 96], F32),
        "g_mixT": _dram_in(nc, "g_mixT", [DEPTH, 128, KC], F32),
        "g_ffnT": _dram_in(nc, "g_ffnT", [DEPTH, 128, KC], F32),
        "q_gT": _dram_in(nc, "q_gT", [DEPTH, 128, 4], F32),
        "kv_gT": _dram_in(nc, "kv_gT", [DEPTH, 128, 4], F32),
        "w_in": _dram_in(nc, "w_in", [depth, D, IN_COLS], F32),
        "w_ukv": _dram_in(nc, "w_ukv", [depth, 512, 2048], F32),
        "w_uq": _dram_in(nc, "w_uq", [depth, 512, 1536], F32),
        "w_sb_up": _dram_in(nc, "w_sb_up", [depth, 1024, D], F32),
        "w_mla_up": _dram_in(nc, "w_mla_up", [depth, 1024, D], F32),
        "w_o": _dram_in(nc, "w_o", [depth, D, D], F32),
        "w_ffn_gate": _dram_in(nc, "w_ffn_gate", [nd, D, DFF], F32),
        "w_ffn_up": _dram_in(nc, "w_ffn_up", [nd, D, DFF], F32),
        "w_ffn_down": _dram_in(nc, "w_ffn_down", [nd, DFF, D], F32),
        "final_gT": _dram_in(nc, "final_gT", [128, KC], F32),
    }
    if nm > 0:
        io["w_routerT"] = _dram_in(nc, "w_routerT", [nm, 128, KC, 8], F32)
        io["w_exp_gate"] = _dram_in(nc, "w_exp_gate", [nm, NE, D, DFF], F32)
        io["w_exp_up"] = _dram_in(nc, "w_exp_up", [nm, NE, D, DFF], F32)
        io["w_exp_down"] = _dram_in(nc, "w_exp_down", [nm, NE, DFF, D], F32)
    xo = _dram_out(nc, "xo", [128, KC, TL], F32)
    modloc = nc.dram_tensor("modloc", [1, DEPTH * 3072], F32, kind="Internal").ap()
    modfull = nc.dram_tensor("modfull", [4, DEPTH * 3072], F32, kind="Internal").ap()
    PW = 2048
    widths = [PW] * 16 + [1024]
    kvlx, kvfx = [], []
    for par in range(2):
        la = [nc.dram_tensor(f"kvloc{par}_{i}", [128, w], BF16, kind="Internal").ap() for i, w in enumerate(widths)]
        fa = [nc.dram_tensor(f"kvfull{par}_{i}", [512, w], BF16, kind="Internal").ap() for i, w in enumerate(widths)]
        kvlx.append(KVX(la, [Buf(f"kvl{par}_{i}") for i in range(17)], PW))
        kvfx.append(KVX(fa, [Buf(f"kvf{par}_{i}") for i in range(17)], PW))
    k = K(nc)
    P = k.P
    P.dma("sp", k.xT, k.xT.ap[:], None, io["xT"])
    k.emit_setup(io)
    mlb, mfb = Buf("modloc"), Buf("modfull")
    k.emit_mod_shard(io, mlb, modloc)
    P.collective_allgather(mlb, modloc, mfb, modfull, GROUPS4)
    base = P.mark()
    for l in range(depth):
        lio = _layer_io(io, l)
        k.emit_mod_load(io, l, mfb, modfull)
        qsb = P.alloc("qsb", [128, NH, TL], BF16)
        cqn = P.alloc("cqn", [128, 4, TL], BF16)
        par = l % 2
        for tc in range(NTC):
            m = P.mark()
            k.emit_A(lio, tc, qsb, cqn, kvlx[par])
            P.release(m)
        order = []
        for hp in range(4):
            order += [X_K // PW + hp, X_V // PW + hp]
        order.append(16)
        for hp in range(4):
            order += [X_KN // PW + hp, X_VM // PW + hp]
        for i in order:
            P.collective_allgather(kvlx[par].bufs[i], kvlx[par].aps[i], kvfx[par].bufs[i], kvfx[par].aps[i], GROUPS4)
        for tc in range(NTC):
            m = P.mark()
            osb = P.alloc("osb", [128, NH, TCW], BF16)
            omla = P.alloc("omla", [128, NH, TCW], BF16)
            m2 = P.mark()
            k.emit_attn(lio, tc, qsb, cqn, kvfx[par], osb, omla)
            P.release(m2)
            k.emit_C(lio, tc, osb, omla)
            P.release(m)
        P.release(base)
        k.emit_ffn(lio, l % 2 == 1)
    if final:
        refs = k.emit_final(io, xo)
    else:
        refs = [P.dma("sp", None, xo, k.xT, k.xT.ap[:])]
    P.finish(refs)
    P.emit()
    return nc, P


_CACHE = {}


def _get(name, fn):
    if name not in _CACHE:
        _CACHE[name] = fn()
    return _CACHE[name]


def _colT(v, n):
    return np.ascontiguousarray(np.asarray(v, np.float32).reshape(n, 128).T)


def _tok_index(j):
    m = np.arange(8)[:, None]
    i = np.arange(128)[None, :]
    return ((4 * m + j) * 128 + i).reshape(-1)


def _host_inputs(x, c, positions, w_ada, b_ada, norm_mix_g, norm_ffn_g, w_in, q_norm_g, kv_norm_g,
                 w_uq, w_ukv, w_sb_up, w_mla_up, w_o, w_ffn_gate, w_ffn_up, w_ffn_down,
                 w_router, w_exp_gate, w_exp_up, w_exp_down, final_norm_g, depth=DEPTH):
    f32 = lambda a: np.ascontiguousarray(np.asarray(a, np.float32))
    x = np.asarray(x, np.float32)
    nd, nm = (depth + 1) // 2, depth // 2
    shared = {
        "b_adaT": np.stack([_colT(np.asarray(b_ada)[l], 96) for l in range(DEPTH)]),
        "g_mixT": np.stack([_colT(np.asarray(norm_mix_g)[l], KC) for l in range(DEPTH)]),
        "g_ffnT": np.stack([_colT(np.asarray(norm_ffn_g)[l], KC) for l in range(DEPTH)]),
        "q_gT": np.stack([_colT(np.asarray(q_norm_g)[l], 4) for l in range(DEPTH)]),
        "kv_gT": np.stack([_colT(np.asarray(kv_norm_g)[l], 4) for l in range(DEPTH)]),
        "w_in": f32(np.asarray(w_in)[:depth]), "w_ukv": f32(np.asarray(w_ukv)[:depth]), "w_uq": f32(np.asarray(w_uq)[:depth]),
        "w_sb_up": f32(np.asarray(w_sb_up)[:depth]), "w_mla_up": f32(np.asarray(w_mla_up)[:depth]), "w_o": f32(np.asarray(w_o)[:depth]),
        "w_ffn_gate": f32(np.asarray(w_ffn_gate)[:nd]), "w_ffn_up": f32(np.asarray(w_ffn_up)[:nd]),
        "w_ffn_down": f32(np.asarray(w_ffn_down)[:nd]),
        "final_gT": _colT(final_norm_g, KC),
    }
    if nm > 0:
        shared["w_routerT"] = np.ascontiguousarray(
            np.asarray(w_router, np.float32)[:nm].reshape(nm, KC, 128, 8).transpose(0, 2, 1, 3))
        shared["w_exp_gate"] = f32(np.asarray(w_exp_gate)[:nm])
        shared["w_exp_up"] = f32(np.asarray(w_exp_up)[:nm])
        shared["w_exp_down"] = f32(np.asarray(w_exp_down)[:nm])
    w_ada = np.asarray(w_ada, np.float32)
    ada_sh = [np.ascontiguousarray(w_ada[:, :, j * 3072:(j + 1) * 3072]) for j in range(4)]
    in_maps, toks = [], []
    for cid in range(8):
        b, j = cid // 4, cid % 4
        tok = _tok_index(j)
        toks.append(tok)
        xs = x[b, tok, :]
        d = dict(shared)
        d["xT"] = np.ascontiguousarray(xs.T.reshape(KC, 128, TL).transpose(1, 0, 2))
        d["cT"] = _colT(np.asarray(c)[b], KC)
        d["pos"] = np.ascontiguousarray(np.broadcast_to(np.asarray(positions)[b, tok].astype(np.int32)[None, :], (64, TL)))
        d["qpos"] = np.ascontiguousarray(np.broadcast_to(tok.astype(np.int32)[None, :], (128, TL)))
        d["w_ada_sh"] = ada_sh[j]
        in_maps.append(d)
    return in_maps, toks


def kernel(x, c, positions, w_ada, b_ada, norm_mix_g, norm_ffn_g, w_in, q_norm_g, kv_norm_g,
           w_uq, w_ukv, w_sb_up, w_mla_up, w_o, w_ffn_gate, w_ffn_up, w_ffn_down,
           w_router, w_exp_gate, w_exp_up, w_exp_down, final_norm_g):
    in_maps, toks = _host_inputs(x, c, positions, w_ada, b_ada, norm_mix_g, norm_ffn_g, w_in, q_norm_g, kv_norm_g,
                                 w_uq, w_ukv, w_sb_up, w_mla_up, w_o, w_ffn_gate, w_ffn_up, w_ffn_down,
                                 w_router, w_exp_gate, w_exp_up, w_exp_down, final_norm_g)
    nc = _get("fused", build_fused)[0]
    res = run_bass_kernel_spmd(nc, in_maps, core_ids=list(range(8))).results
    out = np.zeros((NB, SEQ, D), np.float32)
    for cid in range(8):
        xs = np.asarray(res[cid]["xo"]).transpose(1, 0, 2).reshape(D, TL).T
        out[cid // 4, toks[cid], :] = xs
    return out
```

```python
import contextlib
import math
import numpy as np
import ml_dtypes
import concourse.bass as bass
import concourse.mybir as mybir
from concourse.bass_utils import run_bass_kernel_spmd

F32 = mybir.dt.float32
BF16 = mybir.dt.bfloat16
I32 = mybir.dt.int32
AF = mybir.ActivationFunctionType
ALU = mybir.AluOpType

ENGS = ["pe", "act", "dve", "pool", "sp"]

D = 2048
KC = 16
SEQ = 4096
NB = 2
DEPTH = 4
TL = 1024
NTC = 2
TCW = 512
NH = 8
DFF = 5632
NFC = 44
GROUPS = [12, 12, 10, 10]
GMAX = 12
NE = 8
IN_COLS = 8256
OFF_Q, OFF_K, OFF_V, OFF_CQ, OFF_CKV, OFF_KPE, OFF_GSB, OFF_GMLA = 0, 1024, 2048, 3072, 3584, 4096, 4160, 6208
X_K, X_V, X_KN, X_VM, X_KPE = 0, 8192, 16384, 24576, 32768
XW = 33792
EPS = 1e-6
SB_SCALE = 128 ** -0.5
MLA_SCALE = 192 ** -0.5
SLOT_EL = 4096
NSLOT = 5


class Buf:
    _n = 0

    def __init__(self, name, ap=None, phase=False):
        Buf._n += 1
        self.id = Buf._n
        self.name = name
        self.ap = ap
        self.phase = phase
        self.dsem = None
        self.dcnt = 0
        self.rsem = None
        self.rcnt = 0


class Prog:
    def __init__(self, nc, arena_kb=206):
        self.nc = nc
        self.eng = {"pe": nc.tensor, "act": nc.scalar, "dve": nc.vector,
                    "pool": nc.gpsimd, "sp": nc.sync}
        self.q = {e: [] for e in ENGS}
        self.cnt = {e: 0 for e in ENGS}
        self.known = {e: {} for e in ENGS}
        self.stack = contextlib.ExitStack()
        self.esem = {e: self.stack.enter_context(nc.semaphore("s_" + e)) for e in ENGS}
        self.state = {}
        self.nsem = 0
        self.ninstr = 0
        self.open_dma = []
        self.arena_words = arena_kb * 256
        self.arena = self.stack.enter_context(nc.sbuf_tensor("arena", [128, self.arena_words], F32))
        self.top = 0
        self.peak = 0
        self.sem_pool = {}
        self.ccsem = None
        self.cccnt = 0
        self.live = []
        self.free_sems = []
        self.pool_pending = []

    def alloc(self, name, shape, dtype, phase=True):
        esz = 2 if dtype == BF16 else 4
        nparts = shape[0]
        nel = 1
        for s in shape[1:]:
            nel *= s
        nbytes = (nel * esz + 31) // 32 * 32
        off = self.top
        self.top += nbytes
        self.peak = max(self.peak, self.top)
        assert self.top <= self.arena_words * 4, f"SBUF arena overflow at {name}: {self.top}"
        v = self.arena[0:nparts, off // 4:(off + nbytes) // 4]
        if esz == 2:
            v = v.bitcast(BF16)
        elif dtype != F32:
            v = v.bitcast(dtype)
        v = v[:, 0:nel]
        if len(shape) == 3:
            v = v.rearrange("p (a b) -> p a b", a=shape[1])
        elif len(shape) == 4:
            v = v.rearrange("p (a b c) -> p a b c", a=shape[1], b=shape[2])
        b = Buf(name, v, phase=phase)
        self.live.append((off, b))
        return b

    def mark(self):
        return self.top

    def release(self, mark):
        self.barrier()
        self.top = mark
        keep = []
        for (off, b) in self.live:
            if off >= mark:
                if b.dsem is not None:
                    self.free_sems.append((b.dsem, b.dcnt))
                    b.dsem = None
                if b.rsem is not None:
                    self.free_sems.append((b.rsem, b.rcnt))
                    b.rsem = None
            else:
                keep.append((off, b))
        self.live = keep

    def sem_for(self, buf, kind):
        if self.free_sems:
            sem, cnt = self.free_sems.pop()
        else:
            sem, cnt = self.new_sem(kind), 0
        if kind == "d":
            buf.dsem, buf.dcnt = sem, cnt
        else:
            buf.rsem, buf.rcnt = sem, cnt

    def collective_allgather(self, in_buf, in_ap, out_buf, out_ap, groups):
        e = "pool"
        reads = [(in_buf, None)]
        writes = [(out_buf, None)]
        self._flush_pool_pending()
        self._emit_waits(e, self._collect(reads, writes))
        if self.ccsem is None:
            self.ccsem = self.new_sem("cc")
        self.cccnt += 1
        sem = self.ccsem
        self.ninstr += 1
        self.q[e].append(lambda eng=self.eng[e], sem=sem, i=in_ap, o=out_ap, groups=groups:
                         eng.collective_compute("AllGather", ALU.bypass, replica_groups=groups, ins=[i], outs=[o]).then_inc(sem, 1))
        ref = ("sem", sem, ("cc",), self.cccnt)
        self._record(ref, reads, writes)
        return ref

    def _flush_pool_pending(self):
        if self.pool_pending:
            refs = self.pool_pending
            self.pool_pending = []
            self._emit_waits("pool", refs)

    def psum(self, name):
        t = self.stack.enter_context(self.nc.psum_tensor(name, [128, 512], F32))
        return Buf(name, t)

    def new_sem(self, name):
        self.nsem += 1
        return self.stack.enter_context(self.nc.semaphore(f"{name}_{self.nsem}"))

    def _conf(self, b, key):
        d = self.state.get(b.id)
        if not d:
            return []
        if key is None:
            return list(d.values())
        out = []
        s = d.get(key)
        if s is not None:
            out.append(s)
        s = d.get(None)
        if s is not None:
            out.append(s)
        return out

    def _collect(self, reads, writes):
        refs = []
        for (b, key) in reads:
            for s in self._conf(b, key):
                if s[0] is not None:
                    refs.append(s[0])
        for (b, key) in writes:
            for s in self._conf(b, key):
                if s[0] is not None:
                    refs.append(s[0])
                refs.extend(s[1].values())
        return refs

    @staticmethod
    def _rk(ref):
        return ("eng", ref[1]) if ref[0] == "eng" else ("sem", ref[2])

    @staticmethod
    def _rv(ref):
        return ref[2] if ref[0] == "eng" else ref[3]

    def _record(self, ref, reads, writes):
        rk = self._rk(ref)
        for (b, key) in reads:
            d = self.state.setdefault(b.id, {})
            s = d.get(key)
            if s is None:
                s = [None, {}]
                d[key] = s
            old = s[1].get(rk)
            if old is None or self._rv(old) < self._rv(ref):
                s[1][rk] = ref
        for (b, key) in writes:
            d = self.state.setdefault(b.id, {})
            if key is None:
                d.clear()
            d[key] = [ref, {}]

    def _emit_waits(self, e, refs):
        need = {}
        for r in refs:
            if r[0] == "eng" and r[1] == e and e == "pe":
                continue
            k = self._rk(r)
            v = self._rv(r)
            sem = self.esem[r[1]] if r[0] == "eng" else r[1]
            if k not in need or need[k][1] < v:
                need[k] = (sem, v)
        kn = self.known[e]
        for k, (sem, v) in need.items():
            if kn.get(k, 0) >= v:
                continue
            kn[k] = v
            self.q[e].append(lambda eng=self.eng[e], sem=sem, v=v: eng.wait_ge(sem, v))

    @staticmethod
    def _norm(lst):
        out = []
        for x in lst or []:
            out.append((x, None) if isinstance(x, Buf) else x)
        return out

    def op(self, e, fn, reads=None, writes=None, inc=True):
        reads = self._norm(reads)
        writes = self._norm(writes)
        if e == "pool":
            self._flush_pool_pending()
        self._emit_waits(e, self._collect(reads, writes))
        self.ninstr += 1
        if inc:
            self.cnt[e] += 1
            idx = self.cnt[e]
            self.q[e].append(lambda eng=self.eng[e], fn=fn, sem=self.esem[e]: fn(eng).then_inc(sem, 1))
        else:
            idx = self.cnt[e] + 1
            self.q[e].append(lambda eng=self.eng[e], fn=fn: fn(eng))
        self._record(("eng", e, idx), reads, writes)

    def dma(self, e, out_buf, out_ap, in_buf, in_ap, out_key=None, in_key=None, **kw):
        reads = [(in_buf, in_key)] if in_buf is not None else []
        writes = [(out_buf, out_key)] if out_buf is not None else []
        if e == "pool" and ((out_buf is not None and out_buf.phase) or (in_buf is not None and in_buf.phase)):
            self._flush_pool_pending()
        self._emit_waits(e, self._collect(reads, writes))
        self.ninstr += 1
        tb = out_buf if (out_buf is not None and out_buf.ap is not None) else (in_buf if in_buf is not None else out_buf)
        if tb is out_buf:
            if tb.dsem is None:
                self.sem_for(tb, "d")
            tb.dcnt += 16
            ref = ("sem", tb.dsem, ("d", tb.id), tb.dcnt)
            sem = tb.dsem
        else:
            if tb.rsem is None:
                self.sem_for(tb, "r")
            tb.rcnt += 16
            ref = ("sem", tb.rsem, ("r", tb.id), tb.rcnt)
            sem = tb.rsem
        self.q[e].append(lambda eng=self.eng[e], o=out_ap, i=in_ap, sem=sem, kw=kw:
                         eng.dma_start(out=o, in_=i, **kw).then_inc(sem, 16))
        self._record(ref, reads, writes)
        if (out_buf is not None and out_buf.phase) or (in_buf is not None and in_buf.phase):
            self.open_dma.append(ref)
        return ref

    def dma_multi(self, e, out_buf, parts, **kw):
        writes = [(out_buf, None)]
        if e == "pool" and out_buf.phase:
            self._flush_pool_pending()
        self._emit_waits(e, self._collect([], writes))
        if out_buf.dsem is None:
            self.sem_for(out_buf, "d")
        sem = out_buf.dsem
        for (o, i) in parts:
            self.ninstr += 1
            out_buf.dcnt += 16
            self.q[e].append(lambda eng=self.eng[e], o=o, i=i, sem=sem, kw=kw:
                             eng.dma_start(out=o, in_=i, **kw).then_inc(sem, 16))
        ref = ("sem", sem, ("d", out_buf.id), out_buf.dcnt)
        self._record(ref, [], writes)
        if out_buf.phase:
            self.open_dma.append(ref)
        return ref

    def barrier(self, engines=ENGS, lazy_pool=True):
        for e in engines:
            refs = [("eng", o, self.cnt[o]) for o in ENGS if o != e and self.cnt[o] > 0]
            refs += self.open_dma
            if e == "pool" and lazy_pool:
                self.pool_pending = self.pool_pending + refs
                continue
            self._emit_waits(e, refs)
        self.open_dma = []

    def finish(self, final_refs):
        self._emit_waits("sp", list(final_refs))
        self.barrier(["sp"])

    def emit(self):
        with self.nc.Block() as block:
            @block.tensor
            def _(eng):
                for f in self.q["pe"]:
                    f()

            @block.scalar
            def _(eng):
                for f in self.q["act"]:
                    f()

            @block.vector
            def _(eng):
                for f in self.q["dve"]:
                    f()

            @block.gpsimd
            def _(eng):
                for f in self.q["pool"]:
                    f()

            @block.sync
            def _(eng):
                for f in self.q["sp"]:
                    f()
        self.stack.close()


class KVX:
    def __init__(self, aps, bufs, pw):
        self.aps, self.bufs, self.pw = aps, bufs, pw

    def loc(self, x0, w, rows=None):
        i, off = x0 // self.pw, x0 % self.pw
        assert off + w <= self.pw
        ap = self.aps[i]
        ap = ap[:, off:off + w] if rows is None else ap[rows[0]:rows[1], off:off + w]
        return self.bufs[i], ap

    def full(self, x0, w, rows=None):
        i, off = x0 // self.pw, x0 % self.pw
        assert off + w <= self.pw
        ap = self.aps[i].rearrange("(r p) x -> p r x", p=128)
        ap = ap[:, :, off:off + w] if rows is None else ap[rows[0]:rows[1], :, off:off + w]
        return self.bufs[i], ap


class K:
    def __init__(self, nc):
        self.nc = nc
        P = self.P = Prog(nc)
        self.ps = [P.psum(f"ps{i}") for i in range(8)]
        self.xT = P.alloc("xT", [128, KC, TL], F32, phase=False)
        self.slots = [P.alloc(f"slot{i}", [128, SLOT_EL], BF16, phase=False) for i in range(NSLOT)]
        self.slot_i = 0
        self.ones = P.alloc("ones", [128, 128], BF16, phase=False)
        self.tincl = P.alloc("tincl", [128, 128], BF16, phase=False)
        self.kposc = P.alloc("kposc", [128, 32], F32, phase=False)
        self.qposb = P.alloc("qposb", [128, TL], F32, phase=False)
        self.cc = P.alloc("cc", [64, TL], F32, phase=False)
        self.ss = P.alloc("ss", [64, TL], F32, phase=False)
        self.modT = P.alloc("modT", [128, 96], F32, phase=False)
        self.lc = P.alloc("lc", [128, 32], F32, phase=False)
        self.cact = P.alloc("cact", [128, KC], BF16, phase=False)
        self.one1 = P.alloc("one1", [1, 8], F32, phase=False)
        self.rr = 0

    def mm(self, out, lhsT, rhs, start, stop, reads, writes, inc=None):
        if inc is None:
            inc = stop
        self.P.op("pe", lambda e: e.matmul(out, lhsT, rhs, start=start, stop=stop),
                  reads=reads, writes=writes, inc=inc)

    def wslot(self):
        s = self.slots[self.slot_i % NSLOT]
        self.slot_i += 1
        return s

    def wload(self, parts):
        P = self.P
        s = self.wslot()
        for (view, src) in parts(s):
            P.dma("pool", s, view, None, src)
        return s

    def wtile(self, src, a, b):
        s = self.wslot()
        v = s.ap[:, 0:a * b].rearrange("p (a b) -> p a b", a=a)
        self.P.dma("pool", s, v, None, src)
        return s, v

    def evac(self, fn_act, fn_dve, reads, writes):
        self.rr += 1
        if self.rr % 2 == 0:
            self.P.op("act", fn_act, reads=reads, writes=writes)
        else:
            self.P.op("dve", fn_dve, reads=reads, writes=writes)

    def emit_setup(self, io):
        P = self.P
        nc = self.nc
        ones, tincl = self.ones, self.tincl
        P.op("dve", lambda e: e.memset(ones.ap[:], 1.0), writes=[ones])
        P.op("pool", lambda e: e.affine_select(tincl.ap[:], ones.ap[:], [[-1, 128]], ALU.is_ge, 0.0,
                                               base=0, channel_multiplier=1), reads=[ones], writes=[tincl])
        m0 = P.mark()
        ti = P.alloc("ti", [128, 32], I32)
        P.op("dve", lambda e: e.memset(self.one1.ap[:], 1.0), writes=[self.one1])
        P.op("pool", lambda e: e.iota(ti.ap[:], [[128, 32]], base=0, channel_multiplier=1), writes=[ti])
        P.op("dve", lambda e: e.tensor_copy(self.kposc.ap[:], ti.ap[:]), reads=[ti], writes=[self.kposc])
        qi = P.alloc("qi", [128, TL], I32)
        P.dma("sp", qi, qi.ap[:], None, io["qpos"])
        P.op("dve", lambda e: e.tensor_copy(self.qposb.ap[:], qi.ap[:]), reads=[qi], writes=[self.qposb])
        pi = P.alloc("pi", [64, TL], I32)
        P.dma("sp", pi, pi.ap[:], None, io["pos"])
        posf = P.alloc("posf", [64, TL], F32)
        P.op("dve", lambda e: e.tensor_copy(posf.ap[:], pi.ap[:]), reads=[pi], writes=[posf])
        fi = P.alloc("fi", [64, 2], I32)
        ff = P.alloc("ff", [64, 4], F32)
        P.op("pool", lambda e: e.iota(fi.ap[0:32, 0:1], [[0, 1]], base=0, channel_multiplier=1), writes=[fi])
        P.op("pool", lambda e: e.iota(fi.ap[32:64, 0:1], [[0, 1]], base=0, channel_multiplier=1), writes=[fi])
        P.op("dve", lambda e: e.tensor_copy(ff.ap[:, 0:1], fi.ap[:, 0:1]), reads=[fi], writes=[ff])
        P.op("act", lambda e: e.activation(ff.ap[:, 1:2], ff.ap[:, 0:1], AF.Exp, scale=-math.log(10000.0) / 32.0),
             reads=[ff], writes=[ff])
        P.op("dve", lambda e: e.memset(ff.ap[0:32, 2:3], -1.0), reads=[ff], writes=[ff])
        P.op("dve", lambda e: e.memset(ff.ap[32:64, 2:3], 1.0), reads=[ff], writes=[ff])
        ang = P.alloc("ang", [64, TL], F32)
        P.op("dve", lambda e: e.tensor_scalar(ang.ap[:], posf.ap[:], ff.ap[:, 1:2], None, ALU.mult),
             reads=[posf, ff], writes=[ang])
        kf = P.alloc("kf", [64, TL], F32)
        ki = P.alloc("ki", [64, TL], I32)
        r = P.alloc("r", [64, TL], F32)
        g = P.alloc("g", [64, TL], F32)
        TWO_PI = 2.0 * math.pi
        C1 = 6.28125
        C2 = TWO_PI - C1

        def reduce_sin(dst, shift):
            P.op("dve", lambda e: e.tensor_scalar(kf.ap[:], ang.ap[:], shift, 1.0 / TWO_PI, ALU.add, ALU.mult),
                 reads=[ang], writes=[kf])
            P.op("dve", lambda e: e.tensor_copy(ki.ap[:], kf.ap[:]), reads=[kf], writes=[ki])
            P.op("dve", lambda e: e.tensor_copy(kf.ap[:], ki.ap[:]), reads=[ki], writes=[kf])
            P.op("dve", lambda e: e.scalar_tensor_tensor(r.ap[:], kf.ap[:], -C1, ang.ap[:], ALU.mult, ALU.add),
                 reads=[kf, ang], writes=[r])
            P.op("dve", lambda e: e.scalar_tensor_tensor(r.ap[:], kf.ap[:], -C2, r.ap[:], ALU.mult, ALU.add),
                 reads=[kf, r], writes=[r])
            if shift != 0.0:
                P.op("dve", lambda e: e.tensor_scalar(r.ap[:], r.ap[:], shift, None, ALU.add), reads=[r], writes=[r])
            P.op("dve", lambda e: e.tensor_scalar(g.ap[:], r.ap[:], math.pi, -TWO_PI, ALU.is_gt, ALU.mult),
                 reads=[r], writes=[g])
            P.op("dve", lambda e: e.tensor_tensor(r.ap[:], r.ap[:], g.ap[:], ALU.add), reads=[r, g], writes=[r])
            P.op("dve", lambda e: e.tensor_scalar(g.ap[:], r.ap[:], -math.pi, TWO_PI, ALU.is_lt, ALU.mult),
                 reads=[r], writes=[g])
            P.op("dve", lambda e: e.tensor_tensor(r.ap[:], r.ap[:], g.ap[:], ALU.add), reads=[r, g], writes=[r])
            P.op("dve", lambda e: e.tensor_scalar(r.ap[:], r.ap[:], 3.1415925, -3.1415925, ALU.min, ALU.max),
                 reads=[r], writes=[r])
            P.op("act", lambda e: e.activation(dst, r.ap[:], AF.Sin), reads=[r], writes=[self.cc, self.ss])

        reduce_sin(self.cc.ap[:], math.pi / 2.0)
        reduce_sin(self.ss.ap[:], 0.0)
        P.op("dve", lambda e: e.tensor_scalar(self.ss.ap[:], self.ss.ap[:], ff.ap[:, 2:3], None, ALU.mult),
             reads=[self.ss, ff], writes=[self.ss])
        ct = P.alloc("ct", [128, KC], F32)
        P.dma("sp", ct, ct.ap[:], None, io["cT"])
        P.op("act", lambda e: e.activation(self.cact.ap[:], ct.ap[:], AF.Silu), reads=[ct], writes=[self.cact])
        P.release(m0)

    def emit_mod(self, io):
        P = self.P
        m0 = P.mark()
        wada = io["w_ada"].rearrange("(kc p) n -> p kc n", p=128)
        rowt = [P.alloc(f"modrow{i}", [1, 512], F32) for i in range(2)]
        bT = P.alloc("bT", [128, 96], F32)
        P.dma("sp", bT, bT.ap[:], None, io["b_adaT"])
        gm = P.alloc("gm", [128, 32], F32)
        P.dma_multi("sp", gm, [(gm.ap[:, 0:16], io["g_mixT"]), (gm.ap[:, 16:32], io["g_ffnT"])])
        mps = self.ps[7]
        for blk in range(24):
            pb = self.ps[blk % 2]
            for half in range(2):
                s, wv = self.wtile(wada[:, :, blk * 512 + half * 256: blk * 512 + half * 256 + 256], KC, 256)
                for kc in range(KC):
                    self.mm(pb.ap[0:1, half * 256:(half + 1) * 256], self.cact.ap[:, kc:kc + 1], wv[:, kc, :],
                            kc == 0, kc == KC - 1, reads=[s, self.cact], writes=[(pb, half)])
            row = rowt[blk % 2]
            P.op("act", lambda e, row=row, pb=pb: e.copy(row.ap[:], pb.ap[0:1, :]), reads=[pb], writes=[row])
            for s4 in range(4):
                j = blk * 4 + s4
                self.mm(mps.ap[:, j:j + 1], row.ap[0:1, s4 * 128:(s4 + 1) * 128], self.one1.ap[0:1, 0:1],
                        True, True, reads=[row, self.one1], writes=[(mps, j)], inc=True)
        P.op("dve", lambda e: e.tensor_tensor(self.modT.ap[:], mps.ap[:, 0:96], bT.ap[:], ALU.add),
             reads=[mps, bT], writes=[self.modT])
        P.op("dve", lambda e: e.scalar_tensor_tensor(self.lc.ap[:, 0:16], self.modT.ap[:, 16:32], 1.0, gm.ap[:, 0:16],
                                                     ALU.add, ALU.mult), reads=[self.modT, gm], writes=[self.lc])
        P.op("dve", lambda e: e.scalar_tensor_tensor(self.lc.ap[:, 16:32], self.modT.ap[:, 64:80], 1.0, gm.ap[:, 16:32],
                                                     ALU.add, ALU.mult), reads=[self.modT, gm, self.lc], writes=[self.lc])
        P.release(m0)

    def emit_mod_shard(self, io, modloc_buf, modloc):
        P = self.P
        m0 = P.mark()
        rowt = [P.alloc(f"modrow{i}", [1, 512], F32) for i in range(2)]
        n = 0
        for l in range(DEPTH):
            wada = io["w_ada_sh"][l].rearrange("(kc p) n -> p kc n", p=128)
            for blk in range(6):
                pb = self.ps[n % 2]
                for half in range(2):
                    c0 = blk * 512 + half * 256
                    s, wv = self.wtile(wada[:, :, c0:c0 + 256], KC, 256)
                    for kc in range(KC):
                        self.mm(pb.ap[0:1, half * 256:(half + 1) * 256], self.cact.ap[:, kc:kc + 1], wv[:, kc, :],
                                kc == 0, kc == KC - 1, reads=[s, self.cact], writes=[(pb, half)])
                row = rowt[n % 2]
                P.op("act", lambda e, row=row, pb=pb: e.copy(row.ap[:], pb.ap[0:1, :]), reads=[pb], writes=[row])
                P.dma("sp", modloc_buf, modloc[0:1, l * 3072 + blk * 512: l * 3072 + blk * 512 + 512], row, row.ap[:], out_key=(l, blk))
                n += 1
        P.release(m0)

    def emit_mod_load(self, io, l, modfull_buf, modfull):
        P = self.P
        m0 = P.mark()
        bT = P.alloc("bT", [128, 96], F32)
        P.dma("sp", bT, bT.ap[:], None, io["b_adaT"][l])
        gm = P.alloc("gm", [128, 32], F32)
        P.dma_multi("sp", gm, [(gm.ap[:, 0:16], io["g_mixT"][l]), (gm.ap[:, 16:32], io["g_ffnT"][l])])
        mrow = P.alloc("mrow", [1, 4 * 3072], F32)
        P._emit_waits("sp", P._collect([(modfull_buf, None)], []))
        P.dma_multi("sp", mrow, [(mrow.ap[0:1, r * 3072:(r + 1) * 3072], modfull[r:r + 1, l * 3072:(l + 1) * 3072]) for r in range(4)])
        mraw = self.ps[7]
        for j in range(96):
            self.mm(mraw.ap[:, j:j + 1], mrow.ap[0:1, j * 128:(j + 1) * 128], self.one1.ap[0:1, 0:1],
                    True, True, reads=[mrow, self.one1], writes=[(mraw, j)], inc=True)
        P.op("dve", lambda e: e.tensor_tensor(self.modT.ap[:], mraw.ap[:, 0:96], bT.ap[:], ALU.add), reads=[mraw, bT], writes=[self.modT])
        P.op("dve", lambda e: e.scalar_tensor_tensor(self.lc.ap[:, 0:16], self.modT.ap[:, 16:32], 1.0, gm.ap[:, 0:16],
                                                     ALU.add, ALU.mult), reads=[self.modT, gm], writes=[self.lc])
        P.op("dve", lambda e: e.scalar_tensor_tensor(self.lc.ap[:, 16:32], self.modT.ap[:, 64:80], 1.0, gm.ap[:, 16:32],
                                                     ALU.add, ALU.mult), reads=[self.modT, gm, self.lc], writes=[self.lc])
        P.release(m0)

    def emit_norm(self, tc, hT, hoff, gcol, shcol, gbufs=(), hook=None):
        P = self.P
        t0 = tc * TCW
        m0 = P.mark()
        sq = [P.alloc(f"sq{i}", [128, TCW], BF16) for i in range(3)]
        rstd = P.alloc("rstd", [128, TCW], F32)
        tmp = [P.alloc(f"ntmp{i}", [128, TCW], F32) for i in range(2)]
        h32 = [P.alloc(f"h32_{i}", [128, TCW], F32) for i in range(3)] if hook is not None else None
        pb = self.ps[6]
        gb = list(gbufs)
        for kc in range(KC):
            s = sq[kc % 3]
            P.op("act", lambda e, s=s, kc=kc: e.activation(s.ap[:], self.xT.ap[:, kc, t0:t0 + TCW], AF.Square),
                 reads=[(self.xT, (kc, tc))], writes=[s])
            self.mm(pb.ap[:], self.ones.ap[:], s.ap[:], kc == 0, kc == KC - 1, reads=[s, self.ones], writes=[pb], inc=True)
        P.op("act", lambda e: e.activation(rstd.ap[:], pb.ap[:], AF.Ln, bias=EPS, scale=1.0 / D), reads=[pb], writes=[rstd])
        P.op("act", lambda e: e.activation(rstd.ap[:], rstd.ap[:], AF.Exp, scale=-0.5), reads=[rstd], writes=[rstd])
        for kc in range(KC):
            t = tmp[kc % 2]
            dst = hT.ap[:, kc, hoff:hoff + TCW]
            if shcol is None:
                P.op("dve", lambda e, kc=kc, dst=dst: e.scalar_tensor_tensor(dst, self.xT.ap[:, kc, t0:t0 + TCW], gcol(kc), rstd.ap[:],
                                                                            ALU.mult, ALU.mult),
                     reads=[(self.xT, (kc, tc)), rstd] + gb, writes=[(hT, (kc, tc))])
                continue
            P.op("dve", lambda e, kc=kc, t=t: e.scalar_tensor_tensor(t.ap[:], self.xT.ap[:, kc, t0:t0 + TCW], gcol(kc), rstd.ap[:],
                                                                      ALU.mult, ALU.mult),
                 reads=[(self.xT, (kc, tc)), rstd] + gb, writes=[t])
            if hook is not None:
                hb = h32[kc % 3]
                P.op("act", lambda e, kc=kc, t=t, hb=hb: e.activation(hb.ap[:], t.ap[:], AF.Identity, bias=shcol(kc), scale=1.0),
                     reads=[t] + gb, writes=[hb])
                P.op("dve", lambda e, dst=dst, hb=hb: e.tensor_copy(dst, hb.ap[:]), reads=[hb], writes=[(hT, (kc, tc))])
                hook(kc, hb)
            else:
                P.op("act", lambda e, kc=kc, t=t, dst=dst: e.activation(dst, t.ap[:], AF.Identity, bias=shcol(kc), scale=1.0),
                     reads=[t] + gb, writes=[(hT, (kc, tc))])
        P.release(m0)

    def emit_norm512(self, raw, rbase, out, ooff, gT, goff):
        P = self.P
        m0 = P.mark()
        sq = [P.alloc(f"sqb{i}", [128, TCW], BF16) for i in range(2)]
        rstd = P.alloc("rstdb", [128, TCW], F32)
        pb = self.ps[6]
        for kc in range(4):
            s = sq[kc % 2]
            P.op("act", lambda e, s=s, kc=kc: e.activation(s.ap[:], raw.ap[:, rbase + kc, :], AF.Square), reads=[(raw, rbase + kc)], writes=[s])
            self.mm(pb.ap[:], self.ones.ap[:], s.ap[:], kc == 0, kc == 3, reads=[s, self.ones], writes=[pb], inc=True)
        P.op("act", lambda e: e.activation(rstd.ap[:], pb.ap[:], AF.Ln, bias=EPS, scale=1.0 / 512.0), reads=[pb], writes=[rstd])
        P.op("act", lambda e: e.activation(rstd.ap[:], rstd.ap[:], AF.Exp, scale=-0.5), reads=[rstd], writes=[rstd])
        for kc in range(4):
            P.op("dve", lambda e, kc=kc: e.scalar_tensor_tensor(out.ap[:, kc, ooff:ooff + TCW], raw.ap[:, rbase + kc, :],
                                                                gT.ap[:, goff + kc:goff + kc + 1], rstd.ap[:], ALU.mult, ALU.mult),
                 reads=[(raw, rbase + kc), rstd, gT], writes=[(out, (kc, ooff))])
        P.release(m0)

    def emit_rope(self, p_n, p_sw, t0, dst_buf, dst_ap, tmpa, tmpb):
        P = self.P
        P.op("dve", lambda e: e.tensor_tensor(tmpa.ap[:], p_n.ap[0:64, :], self.cc.ap[:, t0:t0 + TCW], ALU.mult),
             reads=[p_n, self.cc], writes=[tmpa])
        P.op("dve", lambda e: e.tensor_tensor(tmpb.ap[:], p_sw.ap[0:64, :], self.ss.ap[:, t0:t0 + TCW], ALU.mult),
             reads=[p_sw, self.ss], writes=[tmpb])
        P.op("pool", lambda e: e.tensor_tensor(dst_ap, tmpa.ap[:], tmpb.ap[:], ALU.add), reads=[tmpa, tmpb], writes=[dst_buf])

    def emit_A(self, io, tc, qsb, cqn, kvx):
        P = self.P
        t0 = tc * TCW
        gq = P.alloc("gq", [128, 8], F32)
        P.dma_multi("sp", gq, [(gq.ap[:, 0:4], io["q_gT"]), (gq.ap[:, 4:8], io["kv_gT"])])
        hT = P.alloc("hT", [128, KC, TCW], BF16)
        self.emit_norm(tc, hT, 0, lambda kc: self.lc.ap[:, kc:kc + 1], lambda kc: self.modT.ap[:, kc:kc + 1],
                       gbufs=[self.lc, self.modT])
        win = io["w_in"].rearrange("(kc p) n -> p kc n", p=128)
        craw = P.alloc("craw", [128, 8, TCW], F32)
        kst = [P.alloc(f"kst{i}", [128, TCW], BF16) for i in range(2)]
        nst = 0
        for c0 in list(range(0, 2048, 256)) + list(range(OFF_CQ, OFF_KPE, 256)):
            s, wv = self.wtile(win[:, :, c0:c0 + 256], KC, 256)
            for sub in range(2):
                col = c0 + sub * 128
                pb = self.ps[(col // 128) % 4]
                for kc in range(KC):
                    self.mm(pb.ap[:], wv[:, kc, sub * 128:(sub + 1) * 128], hT.ap[:, kc, :], kc == 0, kc == KC - 1,
                            reads=[s, (hT, (kc, tc))], writes=[pb])
                if col < OFF_K:
                    h = col // 128
                    self.evac(lambda e, h=h, pb=pb: e.copy(qsb.ap[:, h, t0:t0 + TCW], pb.ap[:]),
                              lambda e, h=h, pb=pb: e.tensor_copy(qsb.ap[:, h, t0:t0 + TCW], pb.ap[:]),
                              reads=[pb], writes=[(qsb, (h, tc))])
                elif col < OFF_V:
                    h = (col - OFF_K) // 128
                    st = kst[nst % 2]
                    nst += 1
                    self.evac(lambda e, st=st, pb=pb: e.copy(st.ap[:], pb.ap[:]),
                              lambda e, st=st, pb=pb: e.tensor_copy(st.ap[:], pb.ap[:]), reads=[pb], writes=[st])
                    kb_, kap_ = kvx.loc(X_K + h * 1024 + t0, TCW)
                    P.dma("sp", kb_, kap_, st, st.ap[:], out_key=("k", h, tc))
                else:
                    ci = (col - OFF_CQ) // 128
                    self.evac(lambda e, ci=ci, pb=pb: e.copy(craw.ap[:, ci, :], pb.ap[:]),
                              lambda e, ci=ci, pb=pb: e.tensor_copy(craw.ap[:, ci, :], pb.ap[:]),
                              reads=[pb], writes=[(craw, ci)])
        s = self.wslot()
        wv = s.ap[:, 0:KC * 128].rearrange("p (a b) -> p a b", a=KC)
        P.dma_multi("pool", s, [(wv[:, :, 0:64], win[:, :, OFF_KPE:OFF_KPE + 64]),
                                (wv[:, :, 64:96], win[:, :, OFF_KPE + 32:OFF_KPE + 64]),
                                (wv[:, :, 96:128], win[:, :, OFF_KPE:OFF_KPE + 32])])
        pn, psw = self.ps[4], self.ps[5]
        for kc in range(KC):
            self.mm(pn.ap[0:64, :], wv[:, kc, 0:64], hT.ap[:, kc, :], kc == 0, kc == KC - 1, reads=[s, (hT, (kc, tc))], writes=[pn])
        for kc in range(KC):
            self.mm(psw.ap[0:64, :], wv[:, kc, 64:128], hT.ap[:, kc, :], kc == 0, kc == KC - 1, reads=[s, (hT, (kc, tc))], writes=[psw])
        ra = P.alloc("ra", [64, TCW], F32)
        rb = P.alloc("rb", [64, TCW], F32)
        kpst = P.alloc("kpst", [64, TCW], BF16)
        self.emit_rope(pn, psw, t0, kpst, kpst.ap[:], ra, rb)
        kb_, kap_ = kvx.loc(X_KPE + t0, TCW, rows=(0, 64))
        P.dma("sp", kb_, kap_, kpst, kpst.ap[:], out_key=("kpe", tc))
        kb_, kap_ = kvx.loc(X_KPE + t0, TCW, rows=(64, 128))
        P.dma("sp", kb_, kap_, kpst, kpst.ap[:], out_key=("kpe2", tc))
        vst = [P.alloc(f"vst{i}", [128, 2, 4, 128], BF16) for i in range(2)]
        for wi in range(4):
            s, wv = self.wtile(win[:, :, OFF_V + wi * 256:OFF_V + wi * 256 + 256], KC, 256)
            st = vst[wi % 2]
            for mm_ in range(4):
                pb = self.ps[mm_ % 4]
                for kc in range(KC):
                    self.mm(pb.ap[:, 0:256], hT.ap[:, kc, mm_ * 128:(mm_ + 1) * 128], wv[:, kc, :], kc == 0, kc == KC - 1,
                            reads=[s, (hT, (kc, tc))], writes=[pb])
                self.evac(lambda e, st=st, pb=pb, mm_=mm_: e.copy(st.ap[:, :, mm_, :], pb.ap[:, 0:256].rearrange("p (a b) -> p a b", a=2)),
                          lambda e, st=st, pb=pb, mm_=mm_: e.tensor_copy(st.ap[:, :, mm_, :], pb.ap[:, 0:256].rearrange("p (a b) -> p a b", a=2)),
                          reads=[pb], writes=[(st, mm_)])
            kb_, kap_ = kvx.loc(X_V + (2 * wi) * 1024, 2048)
            dst = kap_.rearrange("p (a b) -> p a b", a=2)[:, :, t0:t0 + TCW]
            P.dma("sp", kb_, dst, st, st.ap[:].rearrange("p a m d -> p a (m d)"), out_key=("v", wi, tc))
        ckvn = P.alloc("ckvn", [128, 4, TCW], BF16)
        self.emit_norm512(craw, 0, cqn, t0, gq, 0)
        self.emit_norm512(craw, 4, ckvn, 0, gq, 4)
        wukv = io["w_ukv"].rearrange("(kc p) n -> p kc n", p=128)
        vmst = [P.alloc(f"vmst{i}", [128, 4, 128], BF16) for i in range(2)]
        for h in range(NH):
            s, wv = self.wtile(wukv[:, :, h * 256:(h + 1) * 256], 4, 256)
            pb = self.ps[h % 2]
            for kc in range(4):
                self.mm(pb.ap[:], wv[:, kc, 0:128], ckvn.ap[:, kc, :], kc == 0, kc == 3, reads=[s, ckvn], writes=[pb])
            st = kst[nst % 2]
            nst += 1
            self.evac(lambda e, st=st, pb=pb: e.copy(st.ap[:], pb.ap[:]),
                      lambda e, st=st, pb=pb: e.tensor_copy(st.ap[:], pb.ap[:]), reads=[pb], writes=[st])
            kb_, kap_ = kvx.loc(X_KN + h * 1024 + t0, TCW)
            P.dma("sp", kb_, kap_, st, st.ap[:], out_key=("kn", h, tc))
            pv = self.ps[2 + h % 2]
            for mm_ in range(4):
                for kc in range(4):
                    self.mm(pv.ap[:, mm_ * 128:(mm_ + 1) * 128], ckvn.ap[:, kc, mm_ * 128:(mm_ + 1) * 128], wv[:, kc, 128:256],
                            kc == 0, kc == 3, reads=[s, ckvn], writes=[(pv, mm_)], inc=(kc == 3))
            vs = vmst[h % 2]
            self.evac(lambda e, vs=vs, pv=pv: e.copy(vs.ap[:].rearrange("p a b -> p (a b)"), pv.ap[:]),
                      lambda e, vs=vs, pv=pv: e.tensor_copy(vs.ap[:].rearrange("p a b -> p (a b)"), pv.ap[:]),
                      reads=[pv], writes=[vs])
            kb_, kap_ = kvx.loc(X_VM + h * 1024 + t0, TCW)
            P.dma("sp", kb_, kap_, vs, vs.ap[:].rearrange("p a b -> p (a b)"), out_key=("vm", h, tc))

    def emit_attn(self, io, tc, qsb, cqn, kvx, osb, omla):
        P = self.P
        t0 = tc * TCW
        nchunk = tc + 1
        nblk = 16 * nchunk
        kbuf = [P.alloc(f"kbuf{i}", [128, 4, 512], BF16) for i in range(2)]
        vbuf = [P.alloc(f"vbuf{i}", [128, 4, 512], BF16) for i in range(2)]
        qpos = self.qposb.ap[:, t0:t0 + TCW]
        nld = [0]

        def load_chunk(xk, xv, h, ch):
            kb, vb = kbuf[nld[0] % 2], vbuf[nld[0] % 2]
            nld[0] += 1
            c0 = h * 1024 + ch * 512
            fb_, fap_ = kvx.full(xk + c0, 512)
            P.dma("sp", kb, kb.ap[:], fb_, fap_)
            fb_, fap_ = kvx.full(xv + c0, 512)
            P.dma("sp", vb, vb.ap[:], fb_, fap_)
            return kb, vb

        m1 = P.mark()
        ebuf = [P.alloc(f"ebuf{i}", [128, TCW], F32) for i in range(3)]
        spb = [P.alloc(f"spb{i}", [128, TCW], BF16) for i in range(3)]
        wbuf = [P.alloc(f"wbuf{i}", [128, TCW], F32) for i in range(2)]
        abuf3 = [P.alloc(f"abuf3_{i}", [128, TCW], BF16) for i in range(3)]
        cacc = [P.alloc(f"cacc{i}", [128, TCW], BF16) for i in range(2)]
        for h in range(NH):
            ops_ = self.ps[4 + h % 2]
            q = qsb.ap[:, h, t0:t0 + TCW]
            units = []
            for ch in range(nchunk - 1, -1, -1):
                for ml in range(3, -1, -1):
                    for r in range(3, -1, -1):
                        units.append((ch, ml, r, 4 * (4 * ch + ml) + r))
            chunk_bufs = {}

            def s1(n):
                ch, ml, r, i = units[n]
                if ch not in chunk_bufs:
                    chunk_bufs[ch] = load_chunk(X_K, X_V, h, ch)
                kb, vb = chunk_bufs[ch]
                zps = self.ps[n % 2]
                eb, sp_ = ebuf[n % 3], spb[n % 3]
                ks = slice(ml * 128, (ml + 1) * 128)
                self.mm(zps.ap[:], kb.ap[:, r, ks], q, True, True, reads=[kb, (qsb, (h, tc))], writes=[zps])
                P.op("act", lambda e: e.activation(eb.ap[:], zps.ap[:], AF.Exp, scale=SB_SCALE), reads=[zps], writes=[eb])
                if i >= 16 * tc:
                    P.op("dve", lambda e: e.scalar_tensor_tensor(eb.ap[:], qpos, self.kposc.ap[:, i:i + 1], eb.ap[:], ALU.is_gt, ALU.mult),
                         reads=[eb, self.qposb, self.kposc], writes=[eb])
                P.op("act", lambda e: e.activation(sp_.ap[:], eb.ap[:], AF.Ln, bias=1.0, scale=1.0), reads=[eb], writes=[sp_])

            def s2(n):
                sps = self.ps[2 + n % 2]
                eb, sp_, wb, ab = ebuf[n % 3], spb[n % 3], wbuf[n % 2], abuf3[n % 3]
                cprev, cnew = cacc[(n + 1) % 2], cacc[n % 2]
                self.mm(sps.ap[:], self.tincl.ap[:], sp_.ap[:], True, n == 0, reads=[sp_, self.tincl], writes=[sps], inc=(n == 0))
                if n > 0:
                    self.mm(sps.ap[:], self.ones.ap[:], cprev.ap[:], False, True, reads=[cprev, self.ones], writes=[sps])
                P.op("act", lambda e: e.activation(wb.ap[:], sps.ap[:], AF.Exp, scale=-1.0), reads=[sps], writes=[wb])
                P.op("dve", lambda e: e.tensor_tensor(ab.ap[:], eb.ap[:], wb.ap[:], ALU.mult), reads=[eb, wb], writes=[ab])
                if n == 0:
                    P.op("pool", lambda e: e.tensor_copy(cnew.ap[:], sp_.ap[:]), reads=[sp_], writes=[cnew])
                elif n < nblk - 1:
                    P.op("pool", lambda e: e.tensor_tensor(cnew.ap[:], cprev.ap[:], sp_.ap[:], ALU.add), reads=[sp_, cprev], writes=[cnew])

            def s3(n):
                ch, ml, r, i = units[n]
                kb, vb = chunk_bufs[ch]
                ab = abuf3[n % 3]
                ks = slice(ml * 128, (ml + 1) * 128)
                self.mm(ops_.ap[:], vb.ap[:, r, ks], ab.ap[:], n == 0, n == nblk - 1, reads=[vb, ab], writes=[ops_], inc=True)

            for step in range(nblk + 2):
                if step < nblk:
                    s1(step)
                if 0 <= step - 1 < nblk:
                    s2(step - 1)
                if 0 <= step - 2 < nblk:
                    s3(step - 2)
            self.evac(lambda e, h=h, ops_=ops_: e.copy(osb.ap[:, h, :], ops_.ap[:]),
                      lambda e, h=h, ops_=ops_: e.tensor_copy(osb.ap[:, h, :], ops_.ap[:]), reads=[ops_], writes=[(osb, h)])
        P.release(m1)
        kpe = P.alloc("kpe", [64, 4, 1024], BF16)
        nk = nchunk * 512
        fb_, fap_ = kvx.full(X_KPE, nk, rows=(0, 64))
        P.dma("sp", kpe, kpe.ap[:, :, 0:nk], fb_, fap_)
        wuq = io["w_uq"].rearrange("(kc p) n -> p kc n", p=128)
        qn = [P.alloc(f"qn{i}", [128, TCW], BF16) for i in range(2)]
        qp = [P.alloc(f"qp{i}", [64, TCW], BF16) for i in range(2)]
        ra = P.alloc("ra2", [64, TCW], F32)
        rb = P.alloc("rb2", [64, TCW], F32)
        lsum = P.alloc("lsum", [128, TCW], F32)
        abuf3m = [P.alloc(f"abuf3m_{i}", [128, TCW], BF16) for i in range(3)]
        for h in range(NH):
            s = self.wslot()
            wv = s.ap[:, 0:4 * 256].rearrange("p (a b) -> p a b", a=4)
            P.dma_multi("pool", s, [(wv[:, :, 0:192], wuq[:, :, h * 192:h * 192 + 192]),
                                    (wv[:, :, 192:224], wuq[:, :, h * 192 + 160:h * 192 + 192]),
                                    (wv[:, :, 224:256], wuq[:, :, h * 192 + 128:h * 192 + 160])])
            p0, p1, p2 = self.ps[0], self.ps[1], self.ps[2]
            for kc in range(4):
                self.mm(p0.ap[:], wv[:, kc, 0:128], cqn.ap[:, kc, t0:t0 + TCW], kc == 0, kc == 3, reads=[s, cqn], writes=[p0])
            for kc in range(4):
                self.mm(p1.ap[0:64, :], wv[:, kc, 128:192], cqn.ap[:, kc, t0:t0 + TCW], kc == 0, kc == 3, reads=[s, cqn], writes=[p1])
            for kc in range(4):
                self.mm(p2.ap[0:64, :], wv[:, kc, 192:256], cqn.ap[:, kc, t0:t0 + TCW], kc == 0, kc == 3, reads=[s, cqn], writes=[p2])
            qn_, qp_ = qn[h % 2], qp[h % 2]
            P.op("act", lambda e, qn_=qn_, p0=p0: e.copy(qn_.ap[:], p0.ap[:]), reads=[p0], writes=[qn_])
            self.emit_rope(p1, p2, t0, qp_, qp_.ap[:], ra, rb)
            ops_ = self.ps[4 + h % 2]
            sums = self.ps[6 + h % 2]
            units = [(ch, ml, r, 4 * (4 * ch + ml) + r) for ch in range(nchunk) for ml in range(4) for r in range(4)]
            chunk_bufs = {}

            def m1_(n):
                ch, ml, r, i = units[n]
                if ch not in chunk_bufs:
                    chunk_bufs[ch] = load_chunk(X_KN, X_VM, h, ch)
                kb, vb = chunk_bufs[ch]
                sps = self.ps[n % 3] if False else self.ps[[0, 1, 3][n % 3]]
                ab = abuf3m[n % 3]
                ks = slice(ml * 128, (ml + 1) * 128)
                kg = slice((4 * ch + ml) * 128, (4 * ch + ml + 1) * 128)
                self.mm(sps.ap[:], kb.ap[:, r, ks], qn_.ap[:], True, False, reads=[kb, qn_], writes=[sps], inc=False)
                self.mm(sps.ap[:], kpe.ap[:, r, kg], qp_.ap[:], False, True, reads=[kpe, qp_], writes=[sps])
                P.op("act", lambda e: e.activation(ab.ap[:], sps.ap[:], AF.Exp, scale=MLA_SCALE), reads=[sps], writes=[ab])
                if i >= 16 * tc:
                    P.op("dve", lambda e: e.scalar_tensor_tensor(ab.ap[:], qpos, self.kposc.ap[:, i:i + 1], ab.ap[:], ALU.is_ge, ALU.mult),
                         reads=[ab, self.qposb, self.kposc], writes=[ab])

            def m2_(n):
                ch, ml, r, i = units[n]
                kb, vb = chunk_bufs[ch]
                ab = abuf3m[n % 3]
                ks = slice(ml * 128, (ml + 1) * 128)
                self.mm(ops_.ap[:], vb.ap[:, r, ks], ab.ap[:], n == 0, n == nblk - 1, reads=[vb, ab], writes=[ops_], inc=False)
                self.mm(sums.ap[:], self.ones.ap[:], ab.ap[:], n == 0, n == nblk - 1, reads=[self.ones, ab], writes=[sums], inc=True)

            for step in range(nblk + 1):
                if step < nblk:
                    m1_(step)
                if 0 <= step - 1 < nblk:
                    m2_(step - 1)
            P.op("act", lambda e, sums=sums: e.activation(lsum.ap[:], sums.ap[:], AF.Ln), reads=[sums], writes=[lsum])
            P.op("act", lambda e: e.activation(lsum.ap[:], lsum.ap[:], AF.Exp, scale=-1.0), reads=[lsum], writes=[lsum])
            P.op("dve", lambda e, h=h, ops_=ops_: e.tensor_tensor(omla.ap[:, h, :], ops_.ap[:], lsum.ap[:], ALU.mult),
                 reads=[ops_, lsum], writes=[(omla, h)])

    def emit_C(self, io, tc, osb, omla):
        P = self.P
        t0 = tc * TCW
        hT = P.alloc("hTc", [128, KC, TCW], BF16)
        self.emit_norm(tc, hT, 0, lambda kc: self.lc.ap[:, kc:kc + 1], lambda kc: self.modT.ap[:, kc:kc + 1],
                       gbufs=[self.lc, self.modT])
        yT = P.alloc("yT", [128, KC, TCW], BF16)
        win = io["w_in"].rearrange("(kc p) n -> p kc n", p=128)
        wsu = io["w_sb_up"].rearrange("(kc p) n -> p kc n", p=128)
        wmu = io["w_mla_up"].rearrange("(kc p) n -> p kc n", p=128)
        wo = io["w_o"].rearrange("(kc p) n -> p kc n", p=128)
        sg = [P.alloc(f"sg{i}", [128, TCW], F32) for i in range(4)]
        y1 = [P.alloc(f"y1{i}", [128, TCW], F32) for i in range(4)]
        for pair in range(8):
            s1, w1 = self.wtile(win[:, :, OFF_GSB + pair * 256:OFF_GSB + pair * 256 + 256], KC, 256)
            s2, w2 = self.wtile(win[:, :, OFF_GMLA + pair * 256:OFF_GMLA + pair * 256 + 256], KC, 256)
            s3, w3 = self.wtile(wsu[:, :, pair * 256:pair * 256 + 256], 8, 256)
            s4, w4 = self.wtile(wmu[:, :, pair * 256:pair * 256 + 256], 8, 256)
            for sub in range(2):
                dc = pair * 2 + sub
                b = (dc % 2) * 4
                pgs, pgm, pus, pum = self.ps[b], self.ps[b + 1], self.ps[b + 2], self.ps[b + 3]
                cs = slice(sub * 128, (sub + 1) * 128)
                for kc in range(KC):
                    self.mm(pgs.ap[:], w1[:, kc, cs], hT.ap[:, kc, :], kc == 0, kc == KC - 1, reads=[s1, hT], writes=[pgs])
                for kc in range(KC):
                    self.mm(pgm.ap[:], w2[:, kc, cs], hT.ap[:, kc, :], kc == 0, kc == KC - 1, reads=[s2, hT], writes=[pgm])
                for kc in range(8):
                    self.mm(pus.ap[:], w3[:, kc, cs], osb.ap[:, kc, :], kc == 0, kc == 7, reads=[s3, osb], writes=[pus])
                for kc in range(8):
                    self.mm(pum.ap[:], w4[:, kc, cs], omla.ap[:, kc, :], kc == 0, kc == 7, reads=[s4, omla], writes=[pum])
                ga, gb_ = sg[(dc % 2) * 2], sg[(dc % 2) * 2 + 1]
                ya, yb = y1[(dc % 2) * 2], y1[(dc % 2) * 2 + 1]
                P.op("act", lambda e, ga=ga, pgs=pgs: e.activation(ga.ap[:], pgs.ap[:], AF.Sigmoid), reads=[pgs], writes=[ga])
                P.op("act", lambda e, gb_=gb_, pgm=pgm: e.activation(gb_.ap[:], pgm.ap[:], AF.Sigmoid), reads=[pgm], writes=[gb_])
                P.op("dve", lambda e, ya=ya, ga=ga, pus=pus: e.tensor_tensor(ya.ap[:], ga.ap[:], pus.ap[:], ALU.mult), reads=[ga, pus], writes=[ya])
                P.op("dve", lambda e, yb=yb, gb_=gb_, pum=pum: e.tensor_tensor(yb.ap[:], gb_.ap[:], pum.ap[:], ALU.mult), reads=[gb_, pum], writes=[yb])
                P.op("dve", lambda e, dc=dc, ya=ya, yb=yb: e.tensor_tensor(yT.ap[:, dc, :], ya.ap[:], yb.ap[:], ALU.add),
                     reads=[ya, yb], writes=[(yT, dc)])
        for pair in range(8):
            s1, w1 = self.wtile(wo[:, :, pair * 256:pair * 256 + 256], KC, 256)
            for sub in range(2):
                dc = pair * 2 + sub
                pb = self.ps[dc % 4]
                for kc in range(KC):
                    self.mm(pb.ap[:], w1[:, kc, sub * 128:(sub + 1) * 128], yT.ap[:, kc, :], kc == 0, kc == KC - 1,
                            reads=[s1, (yT, kc)], writes=[pb])
                P.op("dve", lambda e, dc=dc, pb=pb: e.scalar_tensor_tensor(self.xT.ap[:, dc, t0:t0 + TCW], pb.ap[:],
                                                                          self.modT.ap[:, 32 + dc:33 + dc],
                                                                          self.xT.ap[:, dc, t0:t0 + TCW], ALU.mult, ALU.add),
                     reads=[pb, self.modT, (self.xT, (dc, tc))], writes=[(self.xT, (dc, tc))])

    def emit_ffn(self, io, moe):
        P = self.P
        m0 = P.mark()
        h2 = P.alloc("h2", [128, KC, TL], BF16)
        gbt = None
        g2cols = [self.lc, self.modT]
        if moe:
            gbt = P.alloc("gbt", [128, NE, TL], BF16)
        m1 = P.mark()
        if moe:
            lgT = P.alloc("lgT", [8, TL], F32)
            gT = P.alloc("gT", [8, TL], F32)
            wr = P.alloc("wr", [128, KC, 8], F32)
            ident = P.alloc("ident", [128, 128], F32)
            sel = P.alloc("sel", [8, 8, 128], F32)
            onesf = P.alloc("onesf", [128, 1024], F32)
            P.op("dve", lambda e: e.memset(onesf.ap[:], 1.0), writes=[onesf])
            P.op("pool", lambda e: e.affine_select(ident.ap[:], onesf.ap[:, 0:128], [[-1, 128]], ALU.is_equal, 0.0,
                                                   base=0, channel_multiplier=1), reads=[onesf], writes=[ident])
            P.op("pool", lambda e: e.affine_select(sel.ap[:], onesf.ap[0:8, :].rearrange("p (a b) -> p a b", a=8),
                                                   [[1, 8], [0, 128]], ALU.is_equal, 0.0, base=0, channel_multiplier=-1),
                 reads=[onesf], writes=[sel])
            P.dma("sp", wr, wr.ap[:], None, io["w_routerT"])
        for tc in range(NTC):
            hook = None
            if moe:
                pbr = self.ps[tc]

                def hook(kc, hb, pbr=pbr):
                    self.mm(pbr.ap[0:8, :], wr.ap[:, kc, :], hb.ap[:], kc == 0, kc == KC - 1, reads=[wr, hb], writes=[pbr], inc=True)
            self.emit_norm(tc, h2, tc * TCW, lambda kc: self.lc.ap[:, 16 + kc:17 + kc], lambda kc: self.modT.ap[:, 48 + kc:49 + kc],
                           gbufs=g2cols, hook=hook)
            if moe:
                P.op("act", lambda e, tc=tc, pbr=pbr: e.copy(lgT.ap[:, tc * TCW:(tc + 1) * TCW], pbr.ap[0:8, :]), reads=[pbr], writes=[(lgT, tc)])
        if moe:
            lg = P.alloc("lg", [128, 8, 8], F32)
            mx = P.alloc("mx", [128, 8, 8], F32)
            gts = P.alloc("gts", [128, 8, 8], F32)
            sc = P.alloc("sc", [128, 8, 4], F32)
            for blk in range(8):
                pb = self.ps[2 + blk % 2]
                P.op("pe", lambda e, pb=pb, blk=blk: e.transpose(pb.ap[:, 0:8], lgT.ap[0:8, blk * 128:(blk + 1) * 128], ident.ap[0:8, 0:8]),
                     reads=[lgT, ident], writes=[pb])
                P.op("dve", lambda e, pb=pb, blk=blk: e.tensor_copy(lg.ap[:, blk, :], pb.ap[:, 0:8]), reads=[pb], writes=[(lg, blk)])
                P.op("dve", lambda e, blk=blk: e.max(mx.ap[:, blk, :], lg.ap[:, blk, :]), reads=[(lg, blk)], writes=[(mx, blk)])
                P.op("dve", lambda e, blk=blk: e.tensor_scalar(sc.ap[:, blk, 0:1], mx.ap[:, blk, 0:1], -1.0, None, ALU.mult),
                     reads=[(mx, blk)], writes=[(sc, blk)])
                P.op("dve", lambda e, blk=blk: e.tensor_tensor(sc.ap[:, blk, 1:2], mx.ap[:, blk, 1:2], mx.ap[:, blk, 0:1], ALU.subtract),
                     reads=[(mx, blk), (sc, blk)], writes=[(sc, blk)])
                P.op("act", lambda e, blk=blk: e.activation(sc.ap[:, blk, 2:3], sc.ap[:, blk, 1:2], AF.Exp), reads=[(sc, blk)], writes=[(sc, blk)])
                P.op("dve", lambda e, blk=blk: e.tensor_scalar(sc.ap[:, blk, 2:3], sc.ap[:, blk, 2:3], 1.0, None, ALU.add),
                     reads=[(sc, blk)], writes=[(sc, blk)])
                P.op("dve", lambda e, blk=blk: e.reciprocal(sc.ap[:, blk, 3:4], sc.ap[:, blk, 2:3]), reads=[(sc, blk)], writes=[(sc, blk)])
                P.op("act", lambda e, blk=blk: e.activation(gts.ap[:, blk, :], lg.ap[:, blk, :], AF.Exp, bias=sc.ap[:, blk, 0:1], scale=1.0),
                     reads=[(lg, blk), (sc, blk)], writes=[(gts, blk)])
                P.op("dve", lambda e, blk=blk: e.scalar_tensor_tensor(gts.ap[:, blk, :], lg.ap[:, blk, :], mx.ap[:, blk, 1:2], gts.ap[:, blk, :],
                                                                      ALU.is_ge, ALU.mult),
                     reads=[(lg, blk), (mx, blk), (gts, blk)], writes=[(gts, blk)])
                P.op("dve", lambda e, blk=blk: e.tensor_scalar(gts.ap[:, blk, :], gts.ap[:, blk, :], sc.ap[:, blk, 3:4], None, ALU.mult),
                     reads=[(gts, blk), (sc, blk)], writes=[(gts, blk)])
                pt = self.ps[4 + blk % 2]
                P.op("pe", lambda e, pt=pt, blk=blk: e.transpose(pt.ap[0:8, 0:128], gts.ap[:, blk, :], ident.ap[:]),
                     reads=[(gts, blk), ident], writes=[pt])
                P.op("dve", lambda e, pt=pt, blk=blk: e.tensor_copy(gT.ap[:, blk * 128:(blk + 1) * 128], pt.ap[0:8, 0:128]),
                     reads=[pt], writes=[(gT, blk)])
            for ex in range(NE):
                for tc in range(NTC):
                    pb = self.ps[6 + (ex * 2 + tc) % 2]
                    self.mm(pb.ap[:], sel.ap[:, ex, :], gT.ap[:, tc * TCW:(tc + 1) * TCW], True, True, reads=[sel, gT], writes=[pb])
                    self.evac(lambda e, pb=pb, ex=ex, tc=tc: e.copy(gbt.ap[:, ex, tc * TCW:(tc + 1) * TCW], pb.ap[:]),
                              lambda e, pb=pb, ex=ex, tc=tc: e.tensor_copy(gbt.ap[:, ex, tc * TCW:(tc + 1) * TCW], pb.ap[:]),
                              reads=[pb], writes=[(gbt, (ex, tc))])
        P.release(m1)
        act = P.alloc("actT", [128, GMAX, TL], BF16)
        sil = [P.alloc(f"sil{i}", [128, TCW], BF16) for i in range(2)]
        tmpa = [P.alloc(f"ftmp{i}", [128, TCW], BF16) for i in range(2)]
        nexp = NE if moe else 1
        n = 0
        for ex in range(nexp):
            if moe:
                wg = io["w_exp_gate"][ex].rearrange("(kc p) n -> p kc n", p=128)
                wu = io["w_exp_up"][ex].rearrange("(kc p) n -> p kc n", p=128)
                wd = io["w_exp_down"][ex].rearrange("(fc p) n -> p fc n", p=128)
            else:
                wg = io["w_ffn_gate"].rearrange("(kc p) n -> p kc n", p=128)
                wu = io["w_ffn_up"].rearrange("(kc p) n -> p kc n", p=128)
                wd = io["w_ffn_down"].rearrange("(fc p) n -> p fc n", p=128)
            fbase = 0
            for gsz in GROUPS:
                for pr in range(gsz // 2):
                    f0 = (fbase + 2 * pr) * 128
                    sG, wG = self.wtile(wg[:, :, f0:f0 + 256], KC, 256)
                    sU, wU = self.wtile(wu[:, :, f0:f0 + 256], KC, 256)
                    for sub in range(2):
                        fc = 2 * pr + sub
                        cs = slice(sub * 128, (sub + 1) * 128)
                        for tc in range(NTC):
                            n += 1
                            pg, pu = self.ps[(n % 2) * 2], self.ps[(n % 2) * 2 + 1]
                            for kc in range(KC):
                                self.mm(pg.ap[:], wG[:, kc, cs], h2.ap[:, kc, tc * TCW:(tc + 1) * TCW], kc == 0, kc == KC - 1,
                                        reads=[sG, (h2, (kc, tc))], writes=[pg])
                            for kc in range(KC):
                                self.mm(pu.ap[:], wU[:, kc, cs], h2.ap[:, kc, tc * TCW:(tc + 1) * TCW], kc == 0, kc == KC - 1,
                                        reads=[sU, (h2, (kc, tc))], writes=[pu])
                            sl = sil[n % 2]
                            P.op("act", lambda e, sl=sl, pg=pg: e.activation(sl.ap[:], pg.ap[:], AF.Silu), reads=[pg], writes=[sl])
                            dst = act.ap[:, fc, tc * TCW:(tc + 1) * TCW]
                            if moe:
                                tt = tmpa[n % 2]
                                P.op("dve", lambda e, tt=tt, sl=sl, pu=pu: e.tensor_tensor(tt.ap[:], sl.ap[:], pu.ap[:], ALU.mult),
                                     reads=[sl, pu], writes=[tt])
                                P.op("dve", lambda e, tt=tt, dst=dst, ex=ex, tc=tc: e.tensor_tensor(dst, tt.ap[:], gbt.ap[:, ex, tc * TCW:(tc + 1) * TCW], ALU.mult),
                                     reads=[tt, (gbt, (ex, tc))], writes=[(act, (fc, tc))])
                            else:
                                P.op("dve", lambda e, dst=dst, sl=sl, pu=pu: e.tensor_tensor(dst, sl.ap[:], pu.ap[:], ALU.mult),
                                     reads=[sl, pu], writes=[(act, (fc, tc))])
                for pair in range(8):
                    s, wv = self.wtile(wd[:, fbase:fbase + gsz, pair * 256:pair * 256 + 256], gsz, 256)
                    for sub in range(2):
                        dc = pair * 2 + sub
                        for tc in range(NTC):
                            pb = self.ps[4 + (dc * 2 + tc) % 4]
                            for fc in range(gsz):
                                self.mm(pb.ap[:], wv[:, fc, sub * 128:(sub + 1) * 128], act.ap[:, fc, tc * TCW:(tc + 1) * TCW],
                                        fc == 0, fc == gsz - 1, reads=[s, (act, (fc, tc))], writes=[pb])
                            P.op("dve", lambda e, dc=dc, tc=tc, pb=pb: e.scalar_tensor_tensor(
                                self.xT.ap[:, dc, tc * TCW:(tc + 1) * TCW], pb.ap[:], self.modT.ap[:, 80 + dc:81 + dc],
                                self.xT.ap[:, dc, tc * TCW:(tc + 1) * TCW], ALU.mult, ALU.add),
                                reads=[pb, self.modT, (self.xT, (dc, tc))], writes=[(self.xT, (dc, tc))])
                fbase += gsz
        P.release(m0)

    def emit_final(self, io, out_ap):
        P = self.P
        m0 = P.mark()
        fg = P.alloc("fg", [128, KC], F32)
        P.dma("sp", fg, fg.ap[:], None, io["final_gT"])
        refs = []
        for tc in range(NTC):
            m1 = P.mark()
            o32 = P.alloc("o32", [128, KC, TCW], F32)
            self.emit_norm(tc, o32, 0, lambda kc: fg.ap[:, kc:kc + 1], None, gbufs=[fg])
            refs.append(P.dma("sp", None, out_ap[:, :, tc * TCW:(tc + 1) * TCW], o32, o32.ap[:]))
            P.release(m1)
        P.release(m0)
        return refs


def _dram_in(nc, name, shape, dt):
    return nc.dram_tensor(name, list(shape), dt, kind="ExternalInput").ap()


def _dram_out(nc, name, shape, dt):
    return nc.dram_tensor(name, list(shape), dt, kind="ExternalOutput").ap()


def build_A():
    nc = bass.Bass("TRN2", target_bir_lowering=False)
    io = {
        "xT": _dram_in(nc, "xT", [128, KC, TL], F32),
        "cT": _dram_in(nc, "cT", [128, KC], F32),
        "pos": _dram_in(nc, "pos", [64, TL], I32),
        "qpos": _dram_in(nc, "qpos", [128, TL], I32),
        "w_ada": _dram_in(nc, "w_ada", [D, 6 * D], F32),
        "b_adaT": _dram_in(nc, "b_adaT", [128, 96], F32),
        "g_mixT": _dram_in(nc, "g_mixT", [128, KC], F32),
        "g_ffnT": _dram_in(nc, "g_ffnT", [128, KC], F32),
        "q_gT": _dram_in(nc, "q_gT", [128, 4], F32),
        "kv_gT": _dram_in(nc, "kv_gT", [128, 4], F32),
        "w_in": _dram_in(nc, "w_in", [D, IN_COLS], F32),
        "w_ukv": _dram_in(nc, "w_ukv", [512, 2048], F32),
    }
    kvloc = _dram_out(nc, "kvloc", [128, XW], BF16)
    qsb_o = _dram_out(nc, "qsb", [128, NH, TL], BF16)
    cqn_o = _dram_out(nc, "cqn", [128, 4, TL], BF16)
    mod_o = _dram_out(nc, "modo", [128, 128], F32)
    k = K(nc)
    P = k.P
    P.dma("sp", k.xT, k.xT.ap[:], None, io["xT"])
    k.emit_setup(io)
    k.emit_mod(io)
    qsb = P.alloc("qsb", [128, NH, TL], BF16, phase=False)
    cqn = P.alloc("cqn", [128, 4, TL], BF16, phase=False)
    kvb = Buf("kvloc")
    kvx = KVX([kvloc], [kvb], XW)
    for tc in range(NTC):
        m = P.mark()
        k.emit_A(io, tc, qsb, cqn, kvx)
        P.release(m)
    refs = []
    refs.append(P.dma("sp", None, qsb_o, qsb, qsb.ap[:]))
    refs.append(P.dma("sp", None, cqn_o, cqn, cqn.ap[:]))
    refs.append(P.dma("sp", None, mod_o[:, 0:96], k.modT, k.modT.ap[:]))
    refs.append(P.dma("sp", None, mod_o[:, 96:128], k.lc, k.lc.ap[:]))
    for d in P.state.get(kvb.id, {}).values():
        if d[0] is not None:
            refs.append(d[0])
    P.finish(refs)
    P.emit()
    return nc, P


def build_B(moe, last):
    nc = bass.Bass("TRN2", target_bir_lowering=False)
    io = {
        "xT": _dram_in(nc, "xT", [128, KC, TL], F32),
        "cT": _dram_in(nc, "cT", [128, KC], F32),
        "pos": _dram_in(nc, "pos", [64, TL], I32),
        "qpos": _dram_in(nc, "qpos", [128, TL], I32),
        "kvfull": _dram_in(nc, "kvfull", [512, XW], BF16),
        "qsb": _dram_in(nc, "qsb", [128, NH, TL], BF16),
        "cqn": _dram_in(nc, "cqn", [128, 4, TL], BF16),
        "modo": _dram_in(nc, "modo", [128, 128], F32),
        "w_in": _dram_in(nc, "w_in", [D, IN_COLS], F32),
        "w_uq": _dram_in(nc, "w_uq", [512, 1536], F32),
        "w_sb_up": _dram_in(nc, "w_sb_up", [1024, D], F32),
        "w_mla_up": _dram_in(nc, "w_mla_up", [1024, D], F32),
        "w_o": _dram_in(nc, "w_o", [D, D], F32),
    }
    if moe:
        io["w_routerT"] = _dram_in(nc, "w_routerT", [128, KC, 8], F32)
        io["w_exp_gate"] = _dram_in(nc, "w_exp_gate", [NE, D, DFF], F32)
        io["w_exp_up"] = _dram_in(nc, "w_exp_up", [NE, D, DFF], F32)
        io["w_exp_down"] = _dram_in(nc, "w_exp_down", [NE, DFF, D], F32)
    else:
        io["w_ffn_gate"] = _dram_in(nc, "w_ffn_gate", [D, DFF], F32)
        io["w_ffn_up"] = _dram_in(nc, "w_ffn_up", [D, DFF], F32)
        io["w_ffn_down"] = _dram_in(nc, "w_ffn_down", [DFF, D], F32)
    if last:
        io["final_gT"] = _dram_in(nc, "final_gT", [128, KC], F32)
    xo = _dram_out(nc, "xo", [128, KC, TL], F32)
    k = K(nc)
    P = k.P
    P.dma("sp", k.xT, k.xT.ap[:], None, io["xT"])
    k.emit_setup(io)
    P.dma("sp", k.modT, k.modT.ap[:], None, io["modo"][:, 0:96])
    P.dma("sp", k.lc, k.lc.ap[:], None, io["modo"][:, 96:128])
    base = P.mark()
    qsb = P.alloc("qsb", [128, NH, TL], BF16)
    cqn = P.alloc("cqn", [128, 4, TL], BF16)
    P.dma("sp", qsb, qsb.ap[:], None, io["qsb"])
    P.dma("sp", cqn, cqn.ap[:], None, io["cqn"])
    kvb = Buf("kvfull")
    kvx = KVX([io["kvfull"]], [kvb], XW)
    for tc in range(NTC):
        m = P.mark()
        osb = P.alloc("osb", [128, NH, TCW], BF16)
        omla = P.alloc("omla", [128, NH, TCW], BF16)
        m2 = P.mark()
        k.emit_attn(io, tc, qsb, cqn, kvx, osb, omla)
        P.release(m2)
        k.emit_C(io, tc, osb, omla)
        P.release(m)
    P.release(base)
    k.emit_ffn(io, moe)
    if last:
        refs = k.emit_final(io, xo)
    else:
        refs = [P.dma("sp", None, xo, k.xT, k.xT.ap[:])]
    P.finish(refs)
    P.emit()
    return nc, P


def _layer_io(io, l):
    jj = l // 2
    d = {"w_in": io["w_in"][l], "w_ukv": io["w_ukv"][l], "w_uq": io["w_uq"][l], "w_sb_up": io["w_sb_up"][l],
         "w_mla_up": io["w_mla_up"][l], "w_o": io["w_o"][l], "q_gT": io["q_gT"][l], "kv_gT": io["kv_gT"][l]}
    if l % 2 == 1:
        d["w_routerT"] = io["w_routerT"][jj]
        d["w_exp_gate"] = io["w_exp_gate"][jj]
        d["w_exp_up"] = io["w_exp_up"][jj]
        d["w_exp_down"] = io["w_exp_down"][jj]
    else:
        d["w_ffn_gate"] = io["w_ffn_gate"][jj]
        d["w_ffn_up"] = io["w_ffn_up"][jj]
        d["w_ffn_down"] = io["w_ffn_down"][jj]
    return d


GROUPS4 = [[0, 1, 2, 3], [4, 5, 6, 7]]


def build_fused(depth=DEPTH, final=True):
    nc = bass.Bass("TRN2", target_bir_lowering=False, num_devices=8)
    nd, nm = (depth + 1) // 2, depth // 2
    io = {
        "xT": _dram_in(nc, "xT", [128, KC, TL], F32),
        "cT": _dram_in(nc, "cT", [128, KC], F32),
        "pos": _dram_in(nc, "pos", [64, TL], I32),
        "qpos": _dram_in(nc, "qpos", [128, TL], I32),
        "w_ada_sh": _dram_in(nc, "w_ada_sh", [DEPTH, D, 3072], F32),
        "b_adaT": _dram_in(nc, "b_adaT", [DEPTH, 128, 96], F32),
        "g_mixT": _dram_in(nc, "g_mixT", [DEPTH, 128, KC], F32),
        "g_ffnT": _dram_in(nc, "g_ffnT", [DEPTH, 128, KC], F32),
        "q_gT": _dram_in(nc, "q_gT", [DEPTH, 128, 4], F32),
        "kv_gT": _dram_in(nc, "kv_gT", [DEPTH, 128, 4], F32),
        "w_in": _dram_in(nc, "w_in", [depth, D, IN_COLS], F32),
        "w_ukv": _dram_in(nc, "w_ukv", [depth, 512, 2048], F32),
        "w_uq": _dram_in(nc, "w_uq", [depth, 512, 1536], F32),
        "w_sb_up": _dram_in(nc, "w_sb_up", [depth, 1024, D], F32),
        "w_mla_up": _dram_in(nc, "w_mla_up", [depth, 1024, D], F32),
        "w_o": _dram_in(nc, "w_o", [depth, D, D], F32),
        "w_ffn_gate": _dram_in(nc, "w_ffn_gate", [nd, D, DFF], F32),
        "w_ffn_up": _dram_in(nc, "w_ffn_up", [nd, D, DFF], F32),
        "w_ffn_down": _dram_in(nc, "w_ffn_down", [nd, DFF, D], F32),
        "final_gT": _dram_in(nc, "final_gT", [128, KC], F32),
    }
    if nm > 0:
        io["w_routerT"] = _dram_in(nc, "w_routerT", [nm, 128, KC, 8], F32)
        io["w_exp_gate"] = _dram_in(nc, "w_exp_gate", [nm, NE, D, DFF], F32)
        io["w_exp_up"] = _dram_in(nc, "w_exp_up", [nm, NE, D, DFF], F32)
        io["w_exp_down"] = _dram_in(nc, "w_exp_down", [nm, NE, DFF, D], F32)
    xo = _dram_out(nc, "xo", [128, KC, TL], F32)
    modloc = nc.dram_tensor("modloc", [1, DEPTH * 3072], F32, kind="Internal").ap()
    modfull = nc.dram_tensor("modfull", [4, DEPTH * 3072], F32, kind="Internal").ap()
    PW = 2048
    widths = [PW] * 16 + [1024]
    kvlx, kvfx = [], []
    for par in range(2):
        la = [nc.dram_tensor(f"kvloc{par}_{i}", [128, w], BF16, kind="Internal").ap() for i, w in enumerate(widths)]
        fa = [nc.dram_tensor(f"kvfull{par}_{i}", [512, w], BF16, kind="Internal").ap() for i, w in enumerate(widths)]
        kvlx.append(KVX(la, [Buf(f"kvl{par}_{i}") for i in range(17)], PW))
        kvfx.append(KVX(fa, [Buf(f"kvf{par}_{i}") for i in range(17)], PW))
    k = K(nc)
    P = k.P
    P.dma("sp", k.xT, k.xT.ap[:], None, io["xT"])
    k.emit_setup(io)
    mlb, mfb = Buf("modloc"), Buf("modfull")
    k.emit_mod_shard(io, mlb, modloc)
    P.collective_allgather(mlb, modloc, mfb, modfull, GROUPS4)
    base = P.mark()
    for l in range(depth):
        lio = _layer_io(io, l)
        k.emit_mod_load(io, l, mfb, modfull)
        qsb = P.alloc("qsb", [128, NH, TL], BF16)
        cqn = P.alloc("cqn", [128, 4, TL], BF16)
        par = l % 2
        for tc in range(NTC):
            m = P.mark()
            k.emit_A(lio, tc, qsb, cqn, kvlx[par])
            P.release(m)
        order = []
        for hp in range(4):
            order += [X_K // PW + hp, X_V // PW + hp]
        order.append(16)
        for hp in range(4):
            order += [X_KN // PW + hp, X_VM // PW + hp]
        for i in order:
            P.collective_allgather(kvlx[par].bufs[i], kvlx[par].aps[i], kvfx[par].bufs[i], kvfx[par].aps[i], GROUPS4)
        for tc in range(NTC):
            m = P.mark()
            osb = P.alloc("osb", [128, NH, TCW], BF16)
            omla = P.alloc("omla", [128, NH, TCW], BF16)
            m2 = P.mark()
            k.emit_attn(lio, tc, qsb, cqn, kvfx[par], osb, omla)
            P.release(m2)
            k.emit_C(lio, tc, osb, omla)
            P.release(m)
        P.release(base)
        k.emit_ffn(lio, l % 2 == 1)
    if final:
        refs = k.emit_final(io, xo)
    else:
        refs = [P.dma("sp", None, xo, k.xT, k.xT.ap[:])]
    P.finish(refs)
    P.emit()
    return nc, P


_CACHE = {}


def _get(name, fn):
    if name not in _CACHE:
        _CACHE[name] = fn()
    return _CACHE[name]


def _colT(v, n):
    return np.ascontiguousarray(np.asarray(v, np.float32).reshape(n, 128).T)


def _tok_index(j):
    m = np.arange(8)[:, None]
    i = np.arange(128)[None, :]
    return ((4 * m + j) * 128 + i).reshape(-1)


def _host_inputs(x, c, positions, w_ada, b_ada, norm_mix_g, norm_ffn_g, w_in, q_norm_g, kv_norm_g,
                 w_uq, w_ukv, w_sb_up, w_mla_up, w_o, w_ffn_gate, w_ffn_up, w_ffn_down,
                 w_router, w_exp_gate, w_exp_up, w_exp_down, final_norm_g, depth=DEPTH):
    f32 = lambda a: np.ascontiguousarray(np.asarray(a, np.float32))
    x = np.asarray(x, np.float32)
    nd, nm = (depth + 1) // 2, depth // 2
    shared = {
        "b_adaT": np.stack([_colT(np.asarray(b_ada)[l], 96) for l in range(DEPTH)]),
        "g_mixT": np.stack([_colT(np.asarray(norm_mix_g)[l], KC) for l in range(DEPTH)]),
        "g_ffnT": np.stack([_colT(np.asarray(norm_ffn_g)[l], KC) for l in range(DEPTH)]),
        "q_gT": np.stack([_colT(np.asarray(q_norm_g)[l], 4) for l in range(DEPTH)]),
        "kv_gT": np.stack([_colT(np.asarray(kv_norm_g)[l], 4) for l in range(DEPTH)]),
        "w_in": f32(np.asarray(w_in)[:depth]), "w_ukv": f32(np.asarray(w_ukv)[:depth]), "w_uq": f32(np.asarray(w_uq)[:depth]),
        "w_sb_up": f32(np.asarray(w_sb_up)[:depth]), "w_mla_up": f32(np.asarray(w_mla_up)[:depth]), "w_o": f32(np.asarray(w_o)[:depth]),
        "w_ffn_gate": f32(np.asarray(w_ffn_gate)[:nd]), "w_ffn_up": f32(np.asarray(w_ffn_up)[:nd]),
        "w_ffn_down": f32(np.asarray(w_ffn_down)[:nd]),
        "final_gT": _colT(final_norm_g, KC),
    }
    if nm > 0:
        shared["w_routerT"] = np.ascontiguousarray(
            np.asarray(w_router, np.float32)[:nm].reshape(nm, KC, 128, 8).transpose(0, 2, 1, 3))
        shared["w_exp_gate"] = f32(np.asarray(w_exp_gate)[:nm])
        shared["w_exp_up"] = f32(np.asarray(w_exp_up)[:nm])
        shared["w_exp_down"] = f32(np.asarray(w_exp_down)[:nm])
    w_ada = np.asarray(w_ada, np.float32)
    ada_sh = [np.ascontiguousarray(w_ada[:, :, j * 3072:(j + 1) * 3072]) for j in range(4)]
    in_maps, toks = [], []
    for cid in range(8):
        b, j = cid // 4, cid % 4
        tok = _tok_index(j)
        toks.append(tok)
        xs = x[b, tok, :]
        d = dict(shared)
        d["xT"] = np.ascontiguousarray(xs.T.reshape(KC, 128, TL).transpose(1, 0, 2))
        d["cT"] = _colT(np.asarray(c)[b], KC)
        d["pos"] = np.ascontiguousarray(np.broadcast_to(np.asarray(positions)[b, tok].astype(np.int32)[None, :], (64, TL)))
        d["qpos"] = np.ascontiguousarray(np.broadcast_to(tok.astype(np.int32)[None, :], (128, TL)))
        d["w_ada_sh"] = ada_sh[j]
        in_maps.append(d)
    return in_maps, toks


def kernel(x, c, positions, w_ada, b_ada, norm_mix_g, norm_ffn_g, w_in, q_norm_g, kv_norm_g,
           w_uq, w_ukv, w_sb_up, w_mla_up, w_o, w_ffn_gate, w_ffn_up, w_ffn_down,
           w_router, w_exp_gate, w_exp_up, w_exp_down, final_norm_g):
    in_maps, toks = _host_inputs(x, c, positions, w_ada, b_ada, norm_mix_g, norm_ffn_g, w_in, q_norm_g, kv_norm_g,
                                 w_uq, w_ukv, w_sb_up, w_mla_up, w_o, w_ffn_gate, w_ffn_up, w_ffn_down,
                                 w_router, w_exp_gate, w_exp_up, w_exp_down, final_norm_g)
    nc = _get("fused", build_fused)[0]
    res = run_bass_kernel_spmd(nc, in_maps, core_ids=list(range(8))).results
    out = np.zeros((NB, SEQ, D), np.float32)
    for cid in range(8):
        xs = np.asarray(res[cid]["xo"]).transpose(1, 0, 2).reshape(D, TL).T
        out[cid // 4, toks[cid], :] = xs
    return out
```

```python
import contextlib
import math
import numpy as np
import ml_dtypes
import concourse.bass as bass
import concourse.mybir as mybir
from concourse.bass_utils import run_bass_kernel_spmd

F32 = mybir.dt.float32
BF16 = mybir.dt.bfloat16
I32 = mybir.dt.int32
AF = mybir.ActivationFunctionType
ALU = mybir.AluOpType

ENGS = ["pe", "act", "dve", "pool", "sp"]

D = 2048
KC = 16
SEQ = 4096
NB = 2
DEPTH = 4
TL = 1024
NTC = 2
TCW = 512
NH = 8
DFF = 5632
NFC = 44
GROUPS = [12, 12, 10, 10]
GMAX = 12
NE = 8
IN_COLS = 8256
OFF_Q, OFF_K, OFF_V, OFF_CQ, OFF_CKV, OFF_KPE, OFF_GSB, OFF_GMLA = 0, 1024, 2048, 3072, 3584, 4096, 4160, 6208
X_K, X_V, X_KN, X_VM, X_KPE = 0, 8192, 16384, 24576, 32768
XW = 33792
EPS = 1e-6
SB_SCALE = 128 ** -0.5
MLA_SCALE = 192 ** -0.5
SLOT_EL = 4096
NSLOT = 5


class Buf:
    _n = 0

    def __init__(self, name, ap=None, phase=False):
        Buf._n += 1
        self.id = Buf._n
        self.name = name
        self.ap = ap
        self.phase = phase
        self.dsem = None
        self.dcnt = 0
        self.rsem = None
        self.rcnt = 0


class Prog:
    def __init__(self, nc, arena_kb=206):
        self.nc = nc
        self.eng = {"pe": nc.tensor, "act": nc.scalar, "dve": nc.vector,
                    "pool": nc.gpsimd, "sp": nc.sync}
        self.q = {e: [] for e in ENGS}
        self.cnt = {e: 0 for e in ENGS}
        self.known = {e: {} for e in ENGS}
        self.stack = contextlib.ExitStack()
        self.esem = {e: self.stack.enter_context(nc.semaphore("s_" + e)) for e in ENGS}
        self.state = {}
        self.nsem = 0
        self.ninstr = 0
        self.open_dma = []
        self.arena_words = arena_kb * 256
        self.arena = self.stack.enter_context(nc.sbuf_tensor("arena", [128, self.arena_words], F32))
        self.top = 0
        self.peak = 0
        self.sem_pool = {}
        self.ccsem = None
        self.cccnt = 0
        self.live = []
        self.free_sems = []
        self.pool_pending = []

    def alloc(self, name, shape, dtype, phase=True):
        esz = 2 if dtype == BF16 else 4
        nparts = shape[0]
        nel = 1
        for s in shape[1:]:
            nel *= s
        nbytes = (nel * esz + 31) // 32 * 32
        off = self.top
        self.top += nbytes
        self.peak = max(self.peak, self.top)
        assert self.top <= self.arena_words * 4, f"SBUF arena overflow at {name}: {self.top}"
        v = self.arena[0:nparts, off // 4:(off + nbytes) // 4]
        if esz == 2:
            v = v.bitcast(BF16)
        elif dtype != F32:
            v = v.bitcast(dtype)
        v = v[:, 0:nel]
        if len(shape) == 3:
            v = v.rearrange("p (a b) -> p a b", a=shape[1])
        elif len(shape) == 4:
            v = v.rearrange("p (a b c) -> p a b c", a=shape[1], b=shape[2])
        b = Buf(name, v, phase=phase)
        self.live.append((off, b))
        return b

    def mark(self):
        return self.top

    def release(self, mark):
        self.barrier()
        self.top = mark
        keep = []
        for (off, b) in self.live:
            if off >= mark:
                if b.dsem is not None:
                    self.free_sems.append((b.dsem, b.dcnt))
                    b.dsem = None
                if b.rsem is not None:
                    self.free_sems.append((b.rsem, b.rcnt))
                    b.rsem = None
            else:
                keep.append((off, b))
        self.live = keep

    def sem_for(self, buf, kind):
        if self.free_sems:
            sem, cnt = self.free_sems.pop()
        else:
            sem, cnt = self.new_sem(kind), 0
        if kind == "d":
            buf.dsem, buf.dcnt = sem, cnt
        else:
            buf.rsem, buf.rcnt = sem, cnt

    def collective_allgather(self, in_buf, in_ap, out_buf, out_ap, groups):
        e = "pool"
        reads = [(in_buf, None)]
        writes = [(out_buf, None)]
        self._flush_pool_pending()
        self._emit_waits(e, self._collect(reads, writes))
        if self.ccsem is None:
            self.ccsem = self.new_sem("cc")
        self.cccnt += 1
        sem = self.ccsem
        self.ninstr += 1
        self.q[e].append(lambda eng=self.eng[e], sem=sem, i=in_ap, o=out_ap, groups=groups:
                         eng.collective_compute("AllGather", ALU.bypass, replica_groups=groups, ins=[i], outs=[o]).then_inc(sem, 1))
        ref = ("sem", sem, ("cc",), self.cccnt)
        self._record(ref, reads, writes)
        return ref

    def _flush_pool_pending(self):
        if self.pool_pending:
            refs = self.pool_pending
            self.pool_pending = []
            self._emit_waits("pool", refs)

    def psum(self, name):
        t = self.stack.enter_context(self.nc.psum_tensor(name, [128, 512], F32))
        return Buf(name, t)

    def new_sem(self, name):
        self.nsem += 1
        return self.stack.enter_context(self.nc.semaphore(f"{name}_{self.nsem}"))

    def _conf(self, b, key):
        d = self.state.get(b.id)
        if not d:
            return []
        if key is None:
            return list(d.values())
        out = []
        s = d.get(key)
        if s is not None:
            out.append(s)
        s = d.get(None)
        if s is not None:
            out.append(s)
        return out

    def _collect(self, reads, writes):
        refs = []
        for (b, key) in reads:
            for s in self._conf(b, key):
                if s[0] is not None:
                    refs.append(s[0])
        for (b, key) in writes:
            for s in self._conf(b, key):
                if s[0] is not None:
                    refs.append(s[0])
                refs.extend(s[1].values())
        return refs

    @staticmethod
    def _rk(ref):
        return ("eng", ref[1]) if ref[0] == "eng" else ("sem", ref[2])

    @staticmethod
    def _rv(ref):
        return ref[2] if ref[0] == "eng" else ref[3]

    def _record(self, ref, reads, writes):
        rk = self._rk(ref)
        for (b, key) in reads:
            d = self.state.setdefault(b.id, {})
            s = d.get(key)
            if s is None:
                s = [None, {}]
                d[key] = s
            old = s[1].get(rk)
            if old is None or self._rv(old) < self._rv(ref):
                s[1][rk] = ref
        for (b, key) in writes:
            d = self.state.setdefault(b.id, {})
            if key is None:
                d.clear()
            d[key] = [ref, {}]

    def _emit_waits(self, e, refs):
        need = {}
        for r in refs:
            if r[0] == "eng" and r[1] == e and e == "pe":
                continue
            k = self._rk(r)
            v = self._rv(r)
            sem = self.esem[r[1]] if r[0] == "eng" else r[1]
            if k not in need or need[k][1] < v:
                need[k] = (sem, v)
        kn = self.known[e]
        for k, (sem, v) in need.items():
            if kn.get(k, 0) >= v:
                continue
            kn[k] = v
            self.q[e].append(lambda eng=self.eng[e], sem=sem, v=v: eng.wait_ge(sem, v))

    @staticmethod
    def _norm(lst):
        out = []
        for x in lst or []:
            out.append((x, None) if isinstance(x, Buf) else x)
        return out

    def op(self, e, fn, reads=None, writes=None, inc=True):
        reads = self._norm(reads)
        writes = self._norm(writes)
        if e == "pool":
            self._flush_pool_pending()
        self._emit_waits(e, self._collect(reads, writes))
        self.ninstr += 1
        if inc:
            self.cnt[e] += 1
            idx = self.cnt[e]
            self.q[e].append(lambda eng=self.eng[e], fn=fn, sem=self.esem[e]: fn(eng).then_inc(sem, 1))
        else:
            idx = self.cnt[e] + 1
            self.q[e].append(lambda eng=self.eng[e], fn=fn: fn(eng))
        self._record(("eng", e, idx), reads, writes)

    def dma(self, e, out_buf, out_ap, in_buf, in_ap, out_key=None, in_key=None, **kw):
        reads = [(in_buf, in_key)] if in_buf is not None else []
        writes = [(out_buf, out_key)] if out_buf is not None else []
        if e == "pool" and ((out_buf is not None and out_buf.phase) or (in_buf is not None and in_buf.phase)):
            self._flush_pool_pending()
        self._emit_waits(e, self._collect(reads, writes))
        self.ninstr += 1
        tb = out_buf if (out_buf is not None and out_buf.ap is not None) else (in_buf if in_buf is not None else out_buf)
        if tb is out_buf:
            if tb.dsem is None:
                self.sem_for(tb, "d")
            tb.dcnt += 16
            ref = ("sem", tb.dsem, ("d", tb.id), tb.dcnt)
            sem = tb.dsem
        else:
            if tb.rsem is None:
                self.sem_for(tb, "r")
            tb.rcnt += 16
            ref = ("sem", tb.rsem, ("r", tb.id), tb.rcnt)
            sem = tb.rsem
        self.q[e].append(lambda eng=self.eng[e], o=out_ap, i=in_ap, sem=sem, kw=kw:
                         eng.dma_start(out=o, in_=i, **kw).then_inc(sem, 16))
        self._record(ref, reads, writes)
        if (out_buf is not None and out_buf.phase) or (in_buf is not None and in_buf.phase):
            self.open_dma.append(ref)
        return ref

    def dma_multi(self, e, out_buf, parts, **kw):
        writes = [(out_buf, None)]
        if e == "pool" and out_buf.phase:
            self._flush_pool_pending()
        self._emit_waits(e, self._collect([], writes))
        if out_buf.dsem is None:
            self.sem_for(out_buf, "d")
        sem = out_buf.dsem
        for (o, i) in parts:
            self.ninstr += 1
            out_buf.dcnt += 16
            self.q[e].append(lambda eng=self.eng[e], o=o, i=i, sem=sem, kw=kw:
                             eng.dma_start(out=o, in_=i, **kw).then_inc(sem, 16))
        ref = ("sem", sem, ("d", out_buf.id), out_buf.dcnt)
        self._record(ref, [], writes)
        if out_buf.phase:
            self.open_dma.append(ref)
        return ref

    def barrier(self, engines=ENGS, lazy_pool=True):
        for e in engines:
            refs = [("eng", o, self.cnt[o]) for o in ENGS if o != e and self.cnt[o] > 0]
            refs += self.open_dma
            if e == "pool" and lazy_pool:
                self.pool_pending = self.pool_pending + refs
                continue
            self._emit_waits(e, refs)
        self.open_dma = []

    def finish(self, final_refs):
        self._emit_waits("sp", list(final_refs))
        self.barrier(["sp"])

    def emit(self):
        with self.nc.Block() as block:
            @block.tensor
            def _(eng):
                for f in self.q["pe"]:
                    f()

            @block.scalar
            def _(eng):
                for f in self.q["act"]:
                    f()

            @block.vector
            def _(eng):
                for f in self.q["dve"]:
                    f()

            @block.gpsimd
            def _(eng):
                for f in self.q["pool"]:
                    f()

            @block.sync
            def _(eng):
                for f in self.q["sp"]:
                    f()
        self.stack.close()


class KVX:
    def __init__(self, aps, bufs, pw):
        self.aps, self.bufs, self.pw = aps, bufs, pw

    def loc(self, x0, w, rows=None):
        i, off = x0 // self.pw, x0 % self.pw
        assert off + w <= self.pw
        ap = self.aps[i]
        ap = ap[:, off:off + w] if rows is None else ap[rows[0]:rows[1], off:off + w]
        return self.bufs[i], ap

    def full(self, x0, w, rows=None):
        i, off = x0 // self.pw, x0 % self.pw
        assert off + w <= self.pw
        ap = self.aps[i].rearrange("(r p) x -> p r x", p=128)
        ap = ap[:, :, off:off + w] if rows is None else ap[rows[0]:rows[1], :, off:off + w]
        return self.bufs[i], ap


class K:
    def __init__(self, nc):
        self.nc = nc
        P = self.P = Prog(nc)
        self.ps = [P.psum(f"ps{i}") for i in range(8)]
        self.xT = P.alloc("xT", [128, KC, TL], F32, phase=False)
        self.slots = [P.alloc(f"slot{i}", [128, SLOT_EL], BF16, phase=False) for i in range(NSLOT)]
        self.slot_i = 0
        self.ones = P.alloc("ones", [128, 128], BF16, phase=False)
        self.tincl = P.alloc("tincl", [128, 128], BF16, phase=False)
        self.kposc = P.alloc("kposc", [128, 32], F32, phase=False)
        self.qposb = P.alloc("qposb", [128, TL], F32, phase=False)
        self.cc = P.alloc("cc", [64, TL], F32, phase=False)
        self.ss = P.alloc("ss", [64, TL], F32, phase=False)
        self.modT = P.alloc("modT", [128, 96], F32, phase=False)
        self.lc = P.alloc("lc", [128, 32], F32, phase=False)
        self.cact = P.alloc("cact", [128, KC], BF16, phase=False)
        self.one1 = P.alloc("one1", [1, 8], F32, phase=False)
        self.rr = 0

    def mm(self, out, lhsT, rhs, start, stop, reads, writes, inc=None):
        if inc is None:
            inc = stop
        self.P.op("pe", lambda e: e.matmul(out, lhsT, rhs, start=start, stop=stop),
                  reads=reads, writes=writes, inc=inc)

    def wslot(self):
        s = self.slots[self.slot_i % NSLOT]
        self.slot_i += 1
        return s

    def wload(self, parts):
        P = self.P
        s = self.wslot()
        for (view, src) in parts(s):
            P.dma("pool", s, view, None, src)
        return s

    def wtile(self, src, a, b):
        s = self.wslot()
        v = s.ap[:, 0:a * b].rearrange("p (a b) -> p a b", a=a)
        self.P.dma("pool", s, v, None, src)
        return s, v

    def evac(self, fn_act, fn_dve, reads, writes):
        self.rr += 1
        if self.rr % 2 == 0:
            self.P.op("act", fn_act, reads=reads, writes=writes)
        else:
            self.P.op("dve", fn_dve, reads=reads, writes=writes)

    def emit_setup(self, io):
        P = self.P
        nc = self.nc
        ones, tincl = self.ones, self.tincl
        P.op("dve", lambda e: e.memset(ones.ap[:], 1.0), writes=[ones])
        P.op("pool", lambda e: e.affine_select(tincl.ap[:], ones.ap[:], [[-1, 128]], ALU.is_ge, 0.0,
                                               base=0, channel_multiplier=1), reads=[ones], writes=[tincl])
        m0 = P.mark()
        ti = P.alloc("ti", [128, 32], I32)
        P.op("dve", lambda e: e.memset(self.one1.ap[:], 1.0), writes=[self.one1])
        P.op("pool", lambda e: e.iota(ti.ap[:], [[128, 32]], base=0, channel_multiplier=1), writes=[ti])
        P.op("dve", lambda e: e.tensor_copy(self.kposc.ap[:], ti.ap[:]), reads=[ti], writes=[self.kposc])
        qi = P.alloc("qi", [128, TL], I32)
        P.dma("sp", qi, qi.ap[:], None, io["qpos"])
        P.op("dve", lambda e: e.tensor_copy(self.qposb.ap[:], qi.ap[:]), reads=[qi], writes=[self.qposb])
        pi = P.alloc("pi", [64, TL], I32)
        P.dma("sp", pi, pi.ap[:], None, io["pos"])
        posf = P.alloc("posf", [64, TL], F32)
        P.op("dve", lambda e: e.tensor_copy(posf.ap[:], pi.ap[:]), reads=[pi], writes=[posf])
        fi = P.alloc("fi", [64, 2], I32)
        ff = P.alloc("ff", [64, 4], F32)
        P.op("pool", lambda e: e.iota(fi.ap[0:32, 0:1], [[0, 1]], base=0, channel_multiplier=1), writes=[fi])
        P.op("pool", lambda e: e.iota(fi.ap[32:64, 0:1], [[0, 1]], base=0, channel_multiplier=1), writes=[fi])
        P.op("dve", lambda e: e.tensor_copy(ff.ap[:, 0:1], fi.ap[:, 0:1]), reads=[fi], writes=[ff])
        P.op("act", lambda e: e.activation(ff.ap[:, 1:2], ff.ap[:, 0:1], AF.Exp, scale=-math.log(10000.0) / 32.0),
             reads=[ff], writes=[ff])
        P.op("dve", lambda e: e.memset(ff.ap[0:32, 2:3], -1.0), reads=[ff], writes=[ff])
        P.op("dve", lambda e: e.memset(ff.ap[32:64, 2:3], 1.0), reads=[ff], writes=[ff])
        ang = P.alloc("ang", [64, TL], F32)
        P.op("dve", lambda e: e.tensor_scalar(ang.ap[:], posf.ap[:], ff.ap[:, 1:2], None, ALU.mult),
             reads=[posf, ff], writes=[ang])
        kf = P.alloc("kf", [64, TL], F32)
        ki = P.alloc("ki", [64, TL], I32)
        r = P.alloc("r", [64, TL], F32)
        g = P.alloc("g", [64, TL], F32)
        TWO_PI = 2.0 * math.pi
        C1 = 6.28125
        C2 = TWO_PI - C1

        def reduce_sin(dst, shift):
            P.op("dve", lambda e: e.tensor_scalar(kf.ap[:], ang.ap[:], shift, 1.0 / TWO_PI, ALU.add, ALU.mult),
                 reads=[ang], writes=[kf])
            P.op("dve", lambda e: e.tensor_copy(ki.ap[:], kf.ap[:]), reads=[kf], writes=[ki])
            P.op("dve", lambda e: e.tensor_copy(kf.ap[:], ki.ap[:]), reads=[ki], writes=[kf])
            P.op("dve", lambda e: e.scalar_tensor_tensor(r.ap[:], kf.ap[:], -C1, ang.ap[:], ALU.mult, ALU.add),
                 reads=[kf, ang], writes=[r])
            P.op("dve", lambda e: e.scalar_tensor_tensor(r.ap[:], kf.ap[:], -C2, r.ap[:], ALU.mult, ALU.add),
                 reads=[kf, r], writes=[r])
            if shift != 0.0:
                P.op("dve", lambda e: e.tensor_scalar(r.ap[:], r.ap[:], shift, None, ALU.add), reads=[r], writes=[r])
            P.op("dve", lambda e: e.tensor_scalar(g.ap[:], r.ap[:], math.pi, -TWO_PI, ALU.is_gt, ALU.mult),
                 reads=[r], writes=[g])
            P.op("dve", lambda e: e.tensor_tensor(r.ap[:], r.ap[:], g.ap[:], ALU.add), reads=[r, g], writes=[r])
            P.op("dve", lambda e: e.tensor_scalar(g.ap[:], r.ap[:], -math.pi, TWO_PI, ALU.is_lt, ALU.mult),
                 reads=[r], writes=[g])
            P.op("dve", lambda e: e.tensor_tensor(r.ap[:], r.ap[:], g.ap[:], ALU.add), reads=[r, g], writes=[r])
            P.op("dve", lambda e: e.tensor_scalar(r.ap[:], r.ap[:], 3.1415925, -3.1415925, ALU.min, ALU.max),
                 reads=[r], writes=[r])
            P.op("act", lambda e: e.activation(dst, r.ap[:], AF.Sin), reads=[r], writes=[self.cc, self.ss])

        reduce_sin(self.cc.ap[:], math.pi / 2.0)
        reduce_sin(self.ss.ap[:], 0.0)
        P.op("dve", lambda e: e.tensor_scalar(self.ss.ap[:], self.ss.ap[:], ff.ap[:, 2:3], None, ALU.mult),
             reads=[self.ss, ff], writes=[self.ss])
        ct = P.alloc("ct", [128, KC], F32)
        P.dma("sp", ct, ct.ap[:], None, io["cT"])
        P.op("act", lambda e: e.activation(self.cact.ap[:], ct.ap[:], AF.Silu), reads=[ct], writes=[self.cact])
        P.release(m0)

    def emit_mod(self, io):
        P = self.P
        m0 = P.mark()
        wada = io["w_ada"].rearrange("(kc p) n -> p kc n", p=128)
        rowt = [P.alloc(f"modrow{i}", [1, 512], F32) for i in range(2)]
        bT = P.alloc("bT", [128, 96], F32)
        P.dma("sp", bT, bT.ap[:], None, io["b_adaT"])
        gm = P.alloc("gm", [128, 32], F32)
        P.dma_multi("sp", gm, [(gm.ap[:, 0:16], io["g_mixT"]), (gm.ap[:, 16:32], io["g_ffnT"])])
        mps = self.ps[7]
        for blk in range(24):
            pb = self.ps[blk % 2]
            for half in range(2):
                s, wv = self.wtile(wada[:, :, blk * 512 + half * 256: blk * 512 + half * 256 + 256], KC, 256)
                for kc in range(KC):
                    self.mm(pb.ap[0:1, half * 256:(half + 1) * 256], self.cact.ap[:, kc:kc + 1], wv[:, kc, :],
                            kc == 0, kc == KC - 1, reads=[s, self.cact], writes=[(pb, half)])
            row = rowt[blk % 2]
            P.op("act", lambda e, row=row, pb=pb: e.copy(row.ap[:], pb.ap[0:1, :]), reads=[pb], writes=[row])
            for s4 in range(4):
                j = blk * 4 + s4
                self.mm(mps.ap[:, j:j + 1], row.ap[0:1, s4 * 128:(s4 + 1) * 128], self.one1.ap[0:1, 0:1],
                        True, True, reads=[row, self.one1], writes=[(mps, j)], inc=True)
        P.op("dve", lambda e: e.tensor_tensor(self.modT.ap[:], mps.ap[:, 0:96], bT.ap[:], ALU.add),
             reads=[mps, bT], writes=[self.modT])
        P.op("dve", lambda e: e.scalar_tensor_tensor(self.lc.ap[:, 0:16], self.modT.ap[:, 16:32], 1.0, gm.ap[:, 0:16],
                                                     ALU.add, ALU.mult), reads=[self.modT, gm], writes=[self.lc])
        P.op("dve", lambda e: e.scalar_tensor_tensor(self.lc.ap[:, 16:32], self.modT.ap[:, 64:80], 1.0, gm.ap[:, 16:32],
                                                     ALU.add, ALU.mult), reads=[self.modT, gm, self.lc], writes=[self.lc])
        P.release(m0)

    def emit_mod_shard(self, io, modloc_buf, modloc):
        P = self.P
        m0 = P.mark()
        rowt = [P.alloc(f"modrow{i}", [1, 512], F32) for i in range(2)]
        n = 0
        for l in range(DEPTH):
            wada = io["w_ada_sh"][l].rearrange("(kc p) n -> p kc n", p=128)
            for blk in range(6):
                pb = self.ps[n % 2]
                for half in range(2):
                    c0 = blk * 512 + half * 256
                    s, wv = self.wtile(wada[:, :, c0:c0 + 256], KC, 256)
                    for kc in range(KC):
                        self.mm(pb.ap[0:1, half * 256:(half + 1) * 256], self.cact.ap[:, kc:kc + 1], wv[:, kc, :],
                                kc == 0, kc == KC - 1, reads=[s, self.cact], writes=[(pb, half)])
                row = rowt[n % 2]
                P.op("act", lambda e, row=row, pb=pb: e.copy(row.ap[:], pb.ap[0:1, :]), reads=[pb], writes=[row])
                P.dma("sp", modloc_buf, modloc[0:1, l * 3072 + blk * 512: l * 3072 + blk * 512 + 512], row, row.ap[:], out_key=(l, blk))
                n += 1
        P.release(m0)

    def emit_mod_load(self, io, l, modfull_buf, modfull):
        P = self.P
        m0 = P.mark()
        bT = P.alloc("bT", [128, 96], F32)
        P.dma("sp", bT, bT.ap[:], None, io["b_adaT"][l])
        gm = P.alloc("gm", [128, 32], F32)
        P.dma_multi("sp", gm, [(gm.ap[:, 0:16], io["g_mixT"][l]), (gm.ap[:, 16:32], io["g_ffnT"][l])])
        mrow = P.alloc("mrow", [1, 4 * 3072], F32)
        P._emit_waits("sp", P._collect([(modfull_buf, None)], []))
        P.dma_multi("sp", mrow, [(mrow.ap[0:1, r * 3072:(r + 1) * 3072], modfull[r:r + 1, l * 3072:(l + 1) * 3072]) for r in range(4)])
        mraw = self.ps[7]
        for j in range(96):
            self.mm(mraw.ap[:, j:j + 1], mrow.ap[0:1, j * 128:(j + 1) * 128], self.one1.ap[0:1, 0:1],
                    True, True, reads=[mrow, self.one1], writes=[(mraw, j)], inc=True)
        P.op("dve", lambda e: e.tensor_tensor(self.modT.ap[:], mraw.ap[:, 0:96], bT.ap[:], ALU.add), reads=[mraw, bT], writes=[self.modT])
        P.op("dve", lambda e: e.scalar_tensor_tensor(self.lc.ap[:, 0:16], self.modT.ap[:, 16:32], 1.0, gm.ap[:, 0:16],
                                                     ALU.add, ALU.mult), reads=[self.modT, gm], writes=[self.lc])
        P.op("dve", lambda e: e.scalar_tensor_tensor(self.lc.ap[:, 16:32], self.modT.ap[:, 64:80], 1.0, gm.ap[:, 16:32],
                                                     ALU.add, ALU.mult), reads=[self.modT, gm, self.lc], writes=[self.lc])
        P.release(m0)

    def emit_norm(self, tc, hT, hoff, gcol, shcol, gbufs=(), hook=None):
        P = self.P
        t0 = tc * TCW
        m0 = P.mark()
        sq = [P.alloc(f"sq{i}", [128, TCW], BF16) for i in range(3)]
        rstd = P.alloc("rstd", [128, TCW], F32)
        tmp = [P.alloc(f"ntmp{i}", [128, TCW], F32) for i in range(2)]
        h32 = [P.alloc(f"h32_{i}", [128, TCW], F32) for i in range(3)] if hook is not None else None
        pb = self.ps[6]
        gb = list(gbufs)
        for kc in range(KC):
            s = sq[kc % 3]
            P.op("act", lambda e, s=s, kc=kc: e.activation(s.ap[:], self.xT.ap[:, kc, t0:t0 + TCW], AF.Square),
                 reads=[(self.xT, (kc, tc))], writes=[s])
            self.mm(pb.ap[:], self.ones.ap[:], s.ap[:], kc == 0, kc == KC - 1, reads=[s, self.ones], writes=[pb], inc=True)
        P.op("act", lambda e: e.activation(rstd.ap[:], pb.ap[:], AF.Ln, bias=EPS, scale=1.0 / D), reads=[pb], writes=[rstd])
        P.op("act", lambda e: e.activation(rstd.ap[:], rstd.ap[:], AF.Exp, scale=-0.5), reads=[rstd], writes=[rstd])
        for kc in range(KC):
            t = tmp[kc % 2]
            dst = hT.ap[:, kc, hoff:hoff + TCW]
            if shcol is None:
                P.op("dve", lambda e, kc=kc, dst=dst: e.scalar_tensor_tensor(dst, self.xT.ap[:, kc, t0:t0 + TCW], gcol(kc), rstd.ap[:],
                                                                            ALU.mult, ALU.mult),
                     reads=[(self.xT, (kc, tc)), rstd] + gb, writes=[(hT, (kc, tc))])
                continue
            P.op("dve", lambda e, kc=kc, t=t: e.scalar_tensor_tensor(t.ap[:], self.xT.ap[:, kc, t0:t0 + TCW], gcol(kc), rstd.ap[:],
                                                                      ALU.mult, ALU.mult),
                 reads=[(self.xT, (kc, tc)), rstd] + gb, writes=[t])
            if hook is not None:
                hb = h32[kc % 3]
                P.op("act", lambda e, kc=kc, t=t, hb=hb: e.activation(hb.ap[:], t.ap[:], AF.Identity, bias=shcol(kc), scale=1.0),
                     reads=[t] + gb, writes=[hb])
                P.op("dve", lambda e, dst=dst, hb=hb: e.tensor_copy(dst, hb.ap[:]), reads=[hb], writes=[(hT, (kc, tc))])
                hook(kc, hb)
            else:
                P.op("act", lambda e, kc=kc, t=t, dst=dst: e.activation(dst, t.ap[:], AF.Identity, bias=shcol(kc), scale=1.0),
                     reads=[t] + gb, writes=[(hT, (kc, tc))])
        P.release(m0)

    def emit_norm512(self, raw, rbase, out, ooff, gT, goff):
        P = self.P
        m0 = P.mark()
        sq = [P.alloc(f"sqb{i}", [128, TCW], BF16) for i in range(2)]
        rstd = P.alloc("rstdb", [128, TCW], F32)
        pb = self.ps[6]
        for kc in range(4):
            s = sq[kc % 2]
            P.op("act", lambda e, s=s, kc=kc: e.activation(s.ap[:], raw.ap[:, rbase + kc, :], AF.Square), reads=[(raw, rbase + kc)], writes=[s])
            self.mm(pb.ap[:], self.ones.ap[:], s.ap[:], kc == 0, kc == 3, reads=[s, self.ones], writes=[pb], inc=True)
        P.op("act", lambda e: e.activation(rstd.ap[:], pb.ap[:], AF.Ln, bias=EPS, scale=1.0 / 512.0), reads=[pb], writes=[rstd])
        P.op("act", lambda e: e.activation(rstd.ap[:], rstd.ap[:], AF.Exp, scale=-0.5), reads=[rstd], writes=[rstd])
        for kc in range(4):
            P.op("dve", lambda e, kc=kc: e.scalar_tensor_tensor(out.ap[:, kc, ooff:ooff + TCW], raw.ap[:, rbase + kc, :],
                                                                gT.ap[:, goff + kc:goff + kc + 1], rstd.ap[:], ALU.mult, ALU.mult),
                 reads=[(raw, rbase + kc), rstd, gT], writes=[(out, (kc, ooff))])
        P.release(m0)

    def emit_rope(self, p_n, p_sw, t0, dst_buf, dst_ap, tmpa, tmpb):
        P = self.P
        P.op("dve", lambda e: e.tensor_tensor(tmpa.ap[:], p_n.ap[0:64, :], self.cc.ap[:, t0:t0 + TCW], ALU.mult),
             reads=[p_n, self.cc], writes=[tmpa])
        P.op("dve", lambda e: e.tensor_tensor(tmpb.ap[:], p_sw.ap[0:64, :], self.ss.ap[:, t0:t0 + TCW], ALU.mult),
             reads=[p_sw, self.ss], writes=[tmpb])
        P.op("pool", lambda e: e.tensor_tensor(dst_ap, tmpa.ap[:], tmpb.ap[:], ALU.add), reads=[tmpa, tmpb], writes=[dst_buf])

    def emit_A(self, io, tc, qsb, cqn, kvx):
        P = self.P
        t0 = tc * TCW
        gq = P.alloc("gq", [128, 8], F32)
        P.dma_multi("sp", gq, [(gq.ap[:, 0:4], io["q_gT"]), (gq.ap[:, 4:8], io["kv_gT"])])
        hT = P.alloc("hT", [128, KC, TCW], BF16)
        self.emit_norm(tc, hT, 0, lambda kc: self.lc.ap[:, kc:kc + 1], lambda kc: self.modT.ap[:, kc:kc + 1],
                       gbufs=[self.lc, self.modT])
        win = io["w_in"].rearrange("(kc p) n -> p kc n", p=128)
        craw = P.alloc("craw", [128, 8, TCW], F32)
        kst = [P.alloc(f"kst{i}", [128, TCW], BF16) for i in range(2)]
        nst = 0
        for c0 in list(range(0, 2048, 256)) + list(range(OFF_CQ, OFF_KPE, 256)):
            s, wv = self.wtile(win[:, :, c0:c0 + 256], KC, 256)
            for sub in range(2):
                col = c0 + sub * 128
                pb = self.ps[(col // 128) % 4]
                for kc in range(KC):
                    self.mm(pb.ap[:], wv[:, kc, sub * 128:(sub + 1) * 128], hT.ap[:, kc, :], kc == 0, kc == KC - 1,
                            reads=[s, (hT, (kc, tc))], writes=[pb])
                if col < OFF_K:
                    h = col // 128
                    self.evac(lambda e, h=h, pb=pb: e.copy(qsb.ap[:, h, t0:t0 + TCW], pb.ap[:]),
                              lambda e, h=h, pb=pb: e.tensor_copy(qsb.ap[:, h, t0:t0 + TCW], pb.ap[:]),
                              reads=[pb], writes=[(qsb, (h, tc))])
                elif col < OFF_V:
                    h = (col - OFF_K) // 128
                    st = kst[nst % 2]
                    nst += 1
                    self.evac(lambda e, st=st, pb=pb: e.copy(st.ap[:], pb.ap[:]),
                              lambda e, st=st, pb=pb: e.tensor_copy(st.ap[:], pb.ap[:]), reads=[pb], writes=[st])
                    kb_, kap_ = kvx.loc(X_K + h * 1024 + t0, TCW)
                    P.dma("sp", kb_, kap_, st, st.ap[:], out_key=("k", h, tc))
                else:
                    ci = (col - OFF_CQ) // 128
                    self.evac(lambda e, ci=ci, pb=pb: e.copy(craw.ap[:, ci, :], pb.ap[:]),
                              lambda e, ci=ci, pb=pb: e.tensor_copy(craw.ap[:, ci, :], pb.ap[:]),
                              reads=[pb], writes=[(craw, ci)])
        s = self.wslot()
        wv = s.ap[:, 0:KC * 128].rearrange("p (a b) -> p a b", a=KC)
        P.dma_multi("pool", s, [(wv[:, :, 0:64], win[:, :, OFF_KPE:OFF_KPE + 64]),
                                (wv[:, :, 64:96], win[:, :, OFF_KPE + 32:OFF_KPE + 64]),
                                (wv[:, :, 96:128], win[:, :, OFF_KPE:OFF_KPE + 32])])
        pn, psw = self.ps[4], self.ps[5]
        for kc in range(KC):
            self.mm(pn.ap[0:64, :], wv[:, kc, 0:64], hT.ap[:, kc, :], kc == 0, kc == KC - 1, reads=[s, (hT, (kc, tc))], writes=[pn])
        for kc in range(KC):
            self.mm(psw.ap[0:64, :], wv[:, kc, 64:128], hT.ap[:, kc, :], kc == 0, kc == KC - 1, reads=[s, (hT, (kc, tc))], writes=[psw])
        ra = P.alloc("ra", [64, TCW], F32)
        rb = P.alloc("rb", [64, TCW], F32)
        kpst = P.alloc("kpst", [64, TCW], BF16)
        self.emit_rope(pn, psw, t0, kpst, kpst.ap[:], ra, rb)
        kb_, kap_ = kvx.loc(X_KPE + t0, TCW, rows=(0, 64))
        P.dma("sp", kb_, kap_, kpst, kpst.ap[:], out_key=("kpe", tc))
        kb_, kap_ = kvx.loc(X_KPE + t0, TCW, rows=(64, 128))
        P.dma("sp", kb_, kap_, kpst, kpst.ap[:], out_key=("kpe2", tc))
        vst = [P.alloc(f"vst{i}", [128, 2, 4, 128], BF16) for i in range(2)]
        for wi in range(4):
            s, wv = self.wtile(win[:, :, OFF_V + wi * 256:OFF_V + wi * 256 + 256], KC, 256)
            st = vst[wi % 2]
            for mm_ in range(4):
                pb = self.ps[mm_ % 4]
                for kc in range(KC):
                    self.mm(pb.ap[:, 0:256], hT.ap[:, kc, mm_ * 128:(mm_ + 1) * 128], wv[:, kc, :], kc == 0, kc == KC - 1,
                            reads=[s, (hT, (kc, tc))], writes=[pb])
                self.evac(lambda e, st=st, pb=pb, mm_=mm_: e.copy(st.ap[:, :, mm_, :], pb.ap[:, 0:256].rearrange("p (a b) -> p a b", a=2)),
                          lambda e, st=st, pb=pb, mm_=mm_: e.tensor_copy(st.ap[:, :, mm_, :], pb.ap[:, 0:256].rearrange("p (a b) -> p a b", a=2)),
                          reads=[pb], writes=[(st, mm_)])
            kb_, kap_ = kvx.loc(X_V + (2 * wi) * 1024, 2048)
            dst = kap_.rearrange("p (a b) -> p a b", a=2)[:, :, t0:t0 + TCW]
            P.dma("sp", kb_, dst, st, st.ap[:].rearrange("p a m d -> p a (m d)"), out_key=("v", wi, tc))
        ckvn = P.alloc("ckvn", [128, 4, TCW], BF16)
        self.emit_norm512(craw, 0, cqn, t0, gq, 0)
        self.emit_norm512(craw, 4, ckvn, 0, gq, 4)
        wukv = io["w_ukv"].rearrange("(kc p) n -> p kc n", p=128)
        vmst = [P.alloc(f"vmst{i}", [128, 4, 128], BF16) for i in range(2)]
        for h in range(NH):
            s, wv = self.wtile(wukv[:, :, h * 256:(h + 1) * 256], 4, 256)
            pb = self.ps[h % 2]
            for kc in range(4):
                self.mm(pb.ap[:], wv[:, kc, 0:128], ckvn.ap[:, kc, :], kc == 0, kc == 3, reads=[s, ckvn], writes=[pb])
            st = kst[nst % 2]
            nst += 1
            self.evac(lambda e, st=st, pb=pb: e.copy(st.ap[:], pb.ap[:]),
                      lambda e, st=st, pb=pb: e.tensor_copy(st.ap[:], pb.ap[:]), reads=[pb], writes=[st])
            kb_, kap_ = kvx.loc(X_KN + h * 1024 + t0, TCW)
            P.dma("sp", kb_, kap_, st, st.ap[:], out_key=("kn", h, tc))
            pv = self.ps[2 + h % 2]
            for mm_ in range(4):
                for kc in range(4):
                    self.mm(pv.ap[:, mm_ * 128:(mm_ + 1) * 128], ckvn.ap[:, kc, mm_ * 128:(mm_ + 1) * 128], wv[:, kc, 128:256],
                            kc == 0, kc == 3, reads=[s, ckvn], writes=[(pv, mm_)], inc=(kc == 3))
            vs = vmst[h % 2]
            self.evac(lambda e, vs=vs, pv=pv: e.copy(vs.ap[:].rearrange("p a b -> p (a b)"), pv.ap[:]),
                      lambda e, vs=vs, pv=pv: e.tensor_copy(vs.ap[:].rearrange("p a b -> p (a b)"), pv.ap[:]),
                      reads=[pv], writes=[vs])
            kb_, kap_ = kvx.loc(X_VM + h * 1024 + t0, TCW)
            P.dma("sp", kb_, kap_, vs, vs.ap[:].rearrange("p a b -> p (a b)"), out_key=("vm", h, tc))

    def emit_attn(self, io, tc, qsb, cqn, kvx, osb, omla):
        P = self.P
        t0 = tc * TCW
        nchunk = tc + 1
        nblk = 16 * nchunk
        kbuf = [P.alloc(f"kbuf{i}", [128, 4, 512], BF16) for i in range(2)]
        vbuf = [P.alloc(f"vbuf{i}", [128, 4, 512], BF16) for i in range(2)]
        qpos = self.qposb.ap[:, t0:t0 + TCW]
        nld = [0]

        def load_chunk(xk, xv, h, ch):
            kb, vb = kbuf[nld[0] % 2], vbuf[nld[0] % 2]
            nld[0] += 1
            c0 = h * 1024 + ch * 512
            fb_, fap_ = kvx.full(xk + c0, 512)
            P.dma("sp", kb, kb.ap[:], fb_, fap_)
            fb_, fap_ = kvx.full(xv + c0, 512)
            P.dma("sp", vb, vb.ap[:], fb_, fap_)
            return kb, vb

        m1 = P.mark()
        NE_ = 7
        ebuf = [P.alloc(f"ebuf{i}", [128, TCW], F32) for i in range(NE_)]
        spb = [P.alloc(f"spb{i}", [128, TCW], BF16) for i in range(3)]
        wbuf = [P.alloc(f"wbuf{i}", [128, TCW], F32) for i in range(3)]
        abuf3 = [P.alloc(f"abuf3_{i}", [128, TCW], BF16) for i in range(3)]
        cacc = [P.alloc(f"cacc{i}", [128, TCW], BF16) for i in range(2)]
        units = []
        for h in range(NH):
            n = 0
            for ch in range(nchunk - 1, -1, -1):
                for ml in range(3, -1, -1):
                    for r in range(3, -1, -1):
                        units.append(dict(h=h, n=n, ch=ch, ml=ml, r=r, i=4 * (4 * ch + ml) + r))
                        n += 1
        chunk_bufs = {}
        NU = len(units)

        def st0(u):
            d = units[u]
            key = (d["h"], d["ch"])
            if key not in chunk_bufs:
                chunk_bufs[key] = load_chunk(X_K, X_V, d["h"], d["ch"])
            kb, vb = chunk_bufs[key]
            zps = self.ps[u % 2]
            ks = slice(d["ml"] * 128, (d["ml"] + 1) * 128)
            self.mm(zps.ap[:], kb.ap[:, d["r"], ks], qsb.ap[:, d["h"], t0:t0 + TCW], True, True,
                    reads=[kb, (qsb, (d["h"], tc))], writes=[zps])

        def st1(u):
            zps, eb = self.ps[u % 2], ebuf[u % NE_]
            P.op("act", lambda e: e.activation(eb.ap[:], zps.ap[:], AF.Exp, scale=SB_SCALE), reads=[zps], writes=[eb])

        def st2(u):
            i = units[u]["i"]
            eb = ebuf[u % NE_]
            if i >= 16 * tc:
                P.op("dve", lambda e: e.scalar_tensor_tensor(eb.ap[:], qpos, self.kposc.ap[:, i:i + 1], eb.ap[:], ALU.is_gt, ALU.mult),
                     reads=[eb, self.qposb, self.kposc], writes=[eb])

        def st3(u):
            eb, sp_ = ebuf[u % NE_], spb[u % 3]
            P.op("act", lambda e: e.activation(sp_.ap[:], eb.ap[:], AF.Ln, bias=1.0, scale=1.0), reads=[eb], writes=[sp_])

        def st4(u):
            n = units[u]["n"]
            sps, sp_ = self.ps[2 + u % 2], spb[u % 3]
            cprev, cnew = cacc[(n + 1) % 2], cacc[n % 2]
            self.mm(sps.ap[:], self.tincl.ap[:], sp_.ap[:], True, n == 0, reads=[sp_, self.tincl], writes=[sps], inc=(n == 0))
            if n > 0:
                self.mm(sps.ap[:], self.ones.ap[:], cprev.ap[:], False, True, reads=[cprev, self.ones], writes=[sps])
            if n == 0:
                P.op("pool", lambda e: e.tensor_copy(cnew.ap[:], sp_.ap[:]), reads=[sp_], writes=[cnew])
            elif n < nblk - 1:
                P.op("pool", lambda e: e.tensor_tensor(cnew.ap[:], cprev.ap[:], sp_.ap[:], ALU.add), reads=[sp_, cprev], writes=[cnew])

        def st5(u):
            sps, wb = self.ps[2 + u % 2], wbuf[u % 3]
            P.op("act", lambda e: e.activation(wb.ap[:], sps.ap[:], AF.Exp, scale=-1.0), reads=[sps], writes=[wb])

        def st6(u):
            eb, wb, ab = ebuf[u % NE_], wbuf[u % 3], abuf3[u % 3]
            P.op("dve", lambda e: e.tensor_tensor(ab.ap[:], eb.ap[:], wb.ap[:], ALU.mult), reads=[eb, wb], writes=[ab])

        def st7(u):
            d = units[u]
            kb, vb = chunk_bufs[(d["h"], d["ch"])]
            ab = abuf3[u % 3]
            h, n = d["h"], d["n"]
            ops_ = self.ps[4 + h % 2]
            ks = slice(d["ml"] * 128, (d["ml"] + 1) * 128)
            self.mm(ops_.ap[:], vb.ap[:, d["r"], ks], ab.ap[:], n == 0, n == nblk - 1, reads=[vb, ab], writes=[ops_], inc=True)
            if n == nblk - 1:
                self.evac(lambda e, h=h, ops_=ops_: e.copy(osb.ap[:, h, :], ops_.ap[:]),
                          lambda e, h=h, ops_=ops_: e.tensor_copy(osb.ap[:, h, :], ops_.ap[:]), reads=[ops_], writes=[(osb, h)])

        stages = [st0, st1, st2, st3, st4, st5, st6, st7]
        for step in range(NU + len(stages) - 1):
            for k_, st in enumerate(stages):
                u = step - k_
                if 0 <= u < NU:
                    st(u)
        P.release(m1)
        kpe = P.alloc("kpe", [64, 4, 1024], BF16)
        nk = nchunk * 512
        fb_, fap_ = kvx.full(X_KPE, nk, rows=(0, 64))
        P.dma("sp", kpe, kpe.ap[:, :, 0:nk], fb_, fap_)
        wuq = io["w_uq"].rearrange("(kc p) n -> p kc n", p=128)
        qn = [P.alloc(f"qn{i}", [128, TCW], BF16) for i in range(2)]
        qp = [P.alloc(f"qp{i}", [64, TCW], BF16) for i in range(2)]
        ra = P.alloc("ra2", [64, TCW], F32)
        rb = P.alloc("rb2", [64, TCW], F32)
        lsum = P.alloc("lsum", [128, TCW], F32)
        abuf3m = [P.alloc(f"abuf3m_{i}", [128, TCW], BF16) for i in range(4)]
        for h in range(NH):
            s = self.wslot()
            wv = s.ap[:, 0:4 * 256].rearrange("p (a b) -> p a b", a=4)
            P.dma_multi("pool", s, [(wv[:, :, 0:192], wuq[:, :, h * 192:h * 192 + 192]),
                                    (wv[:, :, 192:224], wuq[:, :, h * 192 + 160:h * 192 + 192]),
                                    (wv[:, :, 224:256], wuq[:, :, h * 192 + 128:h * 192 + 160])])
            p0, p1, p2 = self.ps[0], self.ps[1], self.ps[2]
            for kc in range(4):
                self.mm(p0.ap[:], wv[:, kc, 0:128], cqn.ap[:, kc, t0:t0 + TCW], kc == 0, kc == 3, reads=[s, cqn], writes=[p0])
            for kc in range(4):
                self.mm(p1.ap[0:64, :], wv[:, kc, 128:192], cqn.ap[:, kc, t0:t0 + TCW], kc == 0, kc == 3, reads=[s, cqn], writes=[p1])
            for kc in range(4):
                self.mm(p2.ap[0:64, :], wv[:, kc, 192:256], cqn.ap[:, kc, t0:t0 + TCW], kc == 0, kc == 3, reads=[s, cqn], writes=[p2])
            qn_, qp_ = qn[h % 2], qp[h % 2]
            P.op("act", lambda e, qn_=qn_, p0=p0: e.copy(qn_.ap[:], p0.ap[:]), reads=[p0], writes=[qn_])
            self.emit_rope(p1, p2, t0, qp_, qp_.ap[:], ra, rb)
            ops_ = self.ps[4 + h % 2]
            sums = self.ps[6 + h % 2]
            units = [(ch, ml, r, 4 * (4 * ch + ml) + r) for ch in range(nchunk) for ml in range(4) for r in range(4)]
            chunk_bufs = {}

            def ma(n):
                ch, ml, r, i = units[n]
                if ch not in chunk_bufs:
                    chunk_bufs[ch] = load_chunk(X_KN, X_VM, h, ch)
                kb, vb = chunk_bufs[ch]
                sps = self.ps[[0, 1, 3][n % 3]]
                ks = slice(ml * 128, (ml + 1) * 128)
                kg = slice((4 * ch + ml) * 128, (4 * ch + ml + 1) * 128)
                self.mm(sps.ap[:], kb.ap[:, r, ks], qn_.ap[:], True, False, reads=[kb, qn_], writes=[sps], inc=False)
                self.mm(sps.ap[:], kpe.ap[:, r, kg], qp_.ap[:], False, True, reads=[kpe, qp_], writes=[sps])

            def mb(n):
                sps, ab = self.ps[[0, 1, 3][n % 3]], abuf3m[n % 4]
                P.op("act", lambda e: e.activation(ab.ap[:], sps.ap[:], AF.Exp, scale=MLA_SCALE), reads=[sps], writes=[ab])

            def mc(n):
                i = units[n][3]
                ab = abuf3m[n % 4]
                if i >= 16 * tc:
                    P.op("dve", lambda e: e.scalar_tensor_tensor(ab.ap[:], qpos, self.kposc.ap[:, i:i + 1], ab.ap[:], ALU.is_ge, ALU.mult),
                         reads=[ab, self.qposb, self.kposc], writes=[ab])

            def md(n):
                ch, ml, r, i = units[n]
                kb, vb = chunk_bufs[ch]
                ab = abuf3m[n % 4]
                ks = slice(ml * 128, (ml + 1) * 128)
                self.mm(ops_.ap[:], vb.ap[:, r, ks], ab.ap[:], n == 0, n == nblk - 1, reads=[vb, ab], writes=[ops_], inc=False)
                self.mm(sums.ap[:], self.ones.ap[:], ab.ap[:], n == 0, n == nblk - 1, reads=[self.ones, ab], writes=[sums], inc=True)

            mst = [ma, mb, mc, md]
            for step in range(nblk + len(mst) - 1):
                for k_, st in enumerate(mst):
                    n_ = step - k_
                    if 0 <= n_ < nblk:
                        st(n_)
            P.op("act", lambda e, sums=sums: e.activation(lsum.ap[:], sums.ap[:], AF.Ln), reads=[sums], writes=[lsum])
            P.op("act", lambda e: e.activation(lsum.ap[:], lsum.ap[:], AF.Exp, scale=-1.0), reads=[lsum], writes=[lsum])
            P.op("dve", lambda e, h=h, ops_=ops_: e.tensor_tensor(omla.ap[:, h, :], ops_.ap[:], lsum.ap[:], ALU.mult),
                 reads=[ops_, lsum], writes=[(omla, h)])

    def emit_C(self, io, tc, osb, omla):
        P = self.P
        t0 = tc * TCW
        hT = P.alloc("hTc", [128, KC, TCW], BF16)
        self.emit_norm(tc, hT, 0, lambda kc: self.lc.ap[:, kc:kc + 1], lambda kc: self.modT.ap[:, kc:kc + 1],
                       gbufs=[self.lc, self.modT])
        yT = P.alloc("yT", [128, KC, TCW], BF16)
        win = io["w_in"].rearrange("(kc p) n -> p kc n", p=128)
        wsu = io["w_sb_up"].rearrange("(kc p) n -> p kc n", p=128)
        wmu = io["w_mla_up"].rearrange("(kc p) n -> p kc n", p=128)
        wo = io["w_o"].rearrange("(kc p) n -> p kc n", p=128)
        sg = [P.alloc(f"sg{i}", [128, TCW], F32) for i in range(4)]
        y1 = [P.alloc(f"y1{i}", [128, TCW], F32) for i in range(4)]
        for pair in range(8):
            s1, w1 = self.wtile(win[:, :, OFF_GSB + pair * 256:OFF_GSB + pair * 256 + 256], KC, 256)
            s2, w2 = self.wtile(win[:, :, OFF_GMLA + pair * 256:OFF_GMLA + pair * 256 + 256], KC, 256)
            s3, w3 = self.wtile(wsu[:, :, pair * 256:pair * 256 + 256], 8, 256)
            s4, w4 = self.wtile(wmu[:, :, pair * 256:pair * 256 + 256], 8, 256)
            for sub in range(2):
                dc = pair * 2 + sub
                b = (dc % 2) * 4
                pgs, pgm, pus, pum = self.ps[b], self.ps[b + 1], self.ps[b + 2], self.ps[b + 3]
                cs = slice(sub * 128, (sub + 1) * 128)
                for kc in range(KC):
                    self.mm(pgs.ap[:], w1[:, kc, cs], hT.ap[:, kc, :], kc == 0, kc == KC - 1, reads=[s1, hT], writes=[pgs])
                for kc in range(KC):
                    self.mm(pgm.ap[:], w2[:, kc, cs], hT.ap[:, kc, :], kc == 0, kc == KC - 1, reads=[s2, hT], writes=[pgm])
                for kc in range(8):
                    self.mm(pus.ap[:], w3[:, kc, cs], osb.ap[:, kc, :], kc == 0, kc == 7, reads=[s3, osb], writes=[pus])
                for kc in range(8):
                    self.mm(pum.ap[:], w4[:, kc, cs], omla.ap[:, kc, :], kc == 0, kc == 7, reads=[s4, omla], writes=[pum])
                ga, gb_ = sg[(dc % 2) * 2], sg[(dc % 2) * 2 + 1]
                ya, yb = y1[(dc % 2) * 2], y1[(dc % 2) * 2 + 1]
                P.op("act", lambda e, ga=ga, pgs=pgs: e.activation(ga.ap[:], pgs.ap[:], AF.Sigmoid), reads=[pgs], writes=[ga])
                P.op("act", lambda e, gb_=gb_, pgm=pgm: e.activation(gb_.ap[:], pgm.ap[:], AF.Sigmoid), reads=[pgm], writes=[gb_])
                P.op("dve", lambda e, ya=ya, ga=ga, pus=pus: e.tensor_tensor(ya.ap[:], ga.ap[:], pus.ap[:], ALU.mult), reads=[ga, pus], writes=[ya])
                P.op("dve", lambda e, yb=yb, gb_=gb_, pum=pum: e.tensor_tensor(yb.ap[:], gb_.ap[:], pum.ap[:], ALU.mult), reads=[gb_, pum], writes=[yb])
                P.op("dve", lambda e, dc=dc, ya=ya, yb=yb: e.tensor_tensor(yT.ap[:, dc, :], ya.ap[:], yb.ap[:], ALU.add),
                     reads=[ya, yb], writes=[(yT, dc)])
        for pair in range(8):
            s1, w1 = self.wtile(wo[:, :, pair * 256:pair * 256 + 256], KC, 256)
            for sub in range(2):
                dc = pair * 2 + sub
                pb = self.ps[dc % 4]
                for kc in range(KC):
                    self.mm(pb.ap[:], w1[:, kc, sub * 128:(sub + 1) * 128], yT.ap[:, kc, :], kc == 0, kc == KC - 1,
                            reads=[s1, (yT, kc)], writes=[pb])
                P.op("dve", lambda e, dc=dc, pb=pb: e.scalar_tensor_tensor(self.xT.ap[:, dc, t0:t0 + TCW], pb.ap[:],
                                                                          self.modT.ap[:, 32 + dc:33 + dc],
                                                                          self.xT.ap[:, dc, t0:t0 + TCW], ALU.mult, ALU.add),
                     reads=[pb, self.modT, (self.xT, (dc, tc))], writes=[(self.xT, (dc, tc))])

    def emit_ffn(self, io, moe):
        P = self.P
        m0 = P.mark()
        h2 = P.alloc("h2", [128, KC, TL], BF16)
        gbt = None
        g2cols = [self.lc, self.modT]
        if moe:
            gbt = P.alloc("gbt", [128, NE, TL], BF16)
        m1 = P.mark()
        if moe:
            lgT = P.alloc("lgT", [8, TL], F32)
            gT = P.alloc("gT", [8, TL], F32)
            wr = P.alloc("wr", [128, KC, 8], F32)
            ident = P.alloc("ident", [128, 128], F32)
            sel = P.alloc("sel", [8, 8, 128], F32)
            onesf = P.alloc("onesf", [128, 1024], F32)
            P.op("dve", lambda e: e.memset(onesf.ap[:], 1.0), writes=[onesf])
            P.op("pool", lambda e: e.affine_select(ident.ap[:], onesf.ap[:, 0:128], [[-1, 128]], ALU.is_equal, 0.0,
                                                   base=0, channel_multiplier=1), reads=[onesf], writes=[ident])
            P.op("pool", lambda e: e.affine_select(sel.ap[:], onesf.ap[0:8, :].rearrange("p (a b) -> p a b", a=8),
                                                   [[1, 8], [0, 128]], ALU.is_equal, 0.0, base=0, channel_multiplier=-1),
                 reads=[onesf], writes=[sel])
            P.dma("sp", wr, wr.ap[:], None, io["w_routerT"])
        for tc in range(NTC):
            hook = None
            if moe:
                pbr = self.ps[tc]

                def hook(kc, hb, pbr=pbr):
                    self.mm(pbr.ap[0:8, :], wr.ap[:, kc, :], hb.ap[:], kc == 0, kc == KC - 1, reads=[wr, hb], writes=[pbr], inc=True)
            self.emit_norm(tc, h2, tc * TCW, lambda kc: self.lc.ap[:, 16 + kc:17 + kc], lambda kc: self.modT.ap[:, 48 + kc:49 + kc],
                           gbufs=g2cols, hook=hook)
            if moe:
                P.op("act", lambda e, tc=tc, pbr=pbr: e.copy(lgT.ap[:, tc * TCW:(tc + 1) * TCW], pbr.ap[0:8, :]), reads=[pbr], writes=[(lgT, tc)])
        if moe:
            lg = P.alloc("lg", [128, 8, 8], F32)
            mx = P.alloc("mx", [128, 8, 8], F32)
            gts = P.alloc("gts", [128, 8, 8], F32)
            sc = P.alloc("sc", [128, 8, 4], F32)
            for blk in range(8):
                pb = self.ps[2 + blk % 2]
                P.op("pe", lambda e, pb=pb, blk=blk: e.transpose(pb.ap[:, 0:8], lgT.ap[0:8, blk * 128:(blk + 1) * 128], ident.ap[0:8, 0:8]),
                     reads=[lgT, ident], writes=[pb])
                P.op("dve", lambda e, pb=pb, blk=blk: e.tensor_copy(lg.ap[:, blk, :], pb.ap[:, 0:8]), reads=[pb], writes=[(lg, blk)])
                P.op("dve", lambda e, blk=blk: e.max(mx.ap[:, blk, :], lg.ap[:, blk, :]), reads=[(lg, blk)], writes=[(mx, blk)])
                P.op("dve", lambda e, blk=blk: e.tensor_scalar(sc.ap[:, blk, 0:1], mx.ap[:, blk, 0:1], -1.0, None, ALU.mult),
                     reads=[(mx, blk)], writes=[(sc, blk)])
                P.op("dve", lambda e, blk=blk: e.tensor_tensor(sc.ap[:, blk, 1:2], mx.ap[:, blk, 1:2], mx.ap[:, blk, 0:1], ALU.subtract),
                     reads=[(mx, blk), (sc, blk)], writes=[(sc, blk)])
                P.op("act", lambda e, blk=blk: e.activation(sc.ap[:, blk, 2:3], sc.ap[:, blk, 1:2], AF.Exp), reads=[(sc, blk)], writes=[(sc, blk)])
                P.op("dve", lambda e, blk=blk: e.tensor_scalar(sc.ap[:, blk, 2:3], sc.ap[:, blk, 2:3], 1.0, None, ALU.add),
                     reads=[(sc, blk)], writes=[(sc, blk)])
                P.op("dve", lambda e, blk=blk: e.reciprocal(sc.ap[:, blk, 3:4], sc.ap[:, blk, 2:3]), reads=[(sc, blk)], writes=[(sc, blk)])
                P.op("act", lambda e, blk=blk: e.activation(gts.ap[:, blk, :], lg.ap[:, blk, :], AF.Exp, bias=sc.ap[:, blk, 0:1], scale=1.0),
                     reads=[(lg, blk), (sc, blk)], writes=[(gts, blk)])
                P.op("dve", lambda e, blk=blk: e.scalar_tensor_tensor(gts.ap[:, blk, :], lg.ap[:, blk, :], mx.ap[:, blk, 1:2], gts.ap[:, blk, :],
                                                                      ALU.is_ge, ALU.mult),
                     reads=[(lg, blk), (mx, blk), (gts, blk)], writes=[(gts, blk)])
                P.op("dve", lambda e, blk=blk: e.tensor_scalar(gts.ap[:, blk, :], gts.ap[:, blk, :], sc.ap[:, blk, 3:4], None, ALU.mult),
                     reads=[(gts, blk), (sc, blk)], writes=[(gts, blk)])
                pt = self.ps[4 + blk % 2]
                P.op("pe", lambda e, pt=pt, blk=blk: e.transpose(pt.ap[0:8, 0:128], gts.ap[:, blk, :], ident.ap[:]),
                     reads=[(gts, blk), ident], writes=[pt])
                P.op("dve", lambda e, pt=pt, blk=blk: e.tensor_copy(gT.ap[:, blk * 128:(blk + 1) * 128], pt.ap[0:8, 0:128]),
                     reads=[pt], writes=[(gT, blk)])
            for ex in range(NE):
                for tc in range(NTC):
                    pb = self.ps[6 + (ex * 2 + tc) % 2]
                    self.mm(pb.ap[:], sel.ap[:, ex, :], gT.ap[:, tc * TCW:(tc + 1) * TCW], True, True, reads=[sel, gT], writes=[pb])
                    self.evac(lambda e, pb=pb, ex=ex, tc=tc: e.copy(gbt.ap[:, ex, tc * TCW:(tc + 1) * TCW], pb.ap[:]),
                              lambda e, pb=pb, ex=ex, tc=tc: e.tensor_copy(gbt.ap[:, ex, tc * TCW:(tc + 1) * TCW], pb.ap[:]),
                              reads=[pb], writes=[(gbt, (ex, tc))])
        P.release(m1)
        act = P.alloc("actT", [128, GMAX, TL], BF16)
        sil = [P.alloc(f"sil{i}", [128, TCW], BF16) for i in range(2)]
        tmpa = [P.alloc(f"ftmp{i}", [128, TCW], BF16) for i in range(2)]
        nexp = NE if moe else 1
        n = 0
        for ex in range(nexp):
            if moe:
                wg = io["w_exp_gate"][ex].rearrange("(kc p) n -> p kc n", p=128)
                wu = io["w_exp_up"][ex].rearrange("(kc p) n -> p kc n", p=128)
                wd = io["w_exp_down"][ex].rearrange("(fc p) n -> p fc n", p=128)
            else:
                wg = io["w_ffn_gate"].rearrange("(kc p) n -> p kc n", p=128)
                wu = io["w_ffn_up"].rearrange("(kc p) n -> p kc n", p=128)
                wd = io["w_ffn_down"].rearrange("(fc p) n -> p fc n", p=128)
            fbase = 0
            for gsz in GROUPS:
                for pr in range(gsz // 2):
                    f0 = (fbase + 2 * pr) * 128
                    sG, wG = self.wtile(wg[:, :, f0:f0 + 256], KC, 256)
                    sU, wU = self.wtile(wu[:, :, f0:f0 + 256], KC, 256)
                    for sub in range(2):
                        fc = 2 * pr + sub
                        cs = slice(sub * 128, (sub + 1) * 128)
                        for tc in range(NTC):
                            n += 1
                            pg, pu = self.ps[(n % 2) * 2], self.ps[(n % 2) * 2 + 1]
                            for kc in range(KC):
                                self.mm(pg.ap[:], wG[:, kc, cs], h2.ap[:, kc, tc * TCW:(tc + 1) * TCW], kc == 0, kc == KC - 1,
                                        reads=[sG, (h2, (kc, tc))], writes=[pg])
                            for kc in range(KC):
                                self.mm(pu.ap[:], wU[:, kc, cs], h2.ap[:, kc, tc * TCW:(tc + 1) * TCW], kc == 0, kc == KC - 1,
                                        reads=[sU, (h2, (kc, tc))], writes=[pu])
                            sl = sil[n % 2]
                            P.op("act", lambda e, sl=sl, pg=pg: e.activation(sl.ap[:], pg.ap[:], AF.Silu), reads=[pg], writes=[sl])
                            dst = act.ap[:, fc, tc * TCW:(tc + 1) * TCW]
                            if moe:
                                tt = tmpa[n % 2]
                                P.op("dve", lambda e, tt=tt, sl=sl, pu=pu: e.tensor_tensor(tt.ap[:], sl.ap[:], pu.ap[:], ALU.mult),
                                     reads=[sl, pu], writes=[tt])
                                P.op("dve", lambda e, tt=tt, dst=dst, ex=ex, tc=tc: e.tensor_tensor(dst, tt.ap[:], gbt.ap[:, ex, tc * TCW:(tc + 1) * TCW], ALU.mult),
                                     reads=[tt, (gbt, (ex, tc))], writes=[(act, (fc, tc))])
                            else:
                                P.op("dve", lambda e, dst=dst, sl=sl, pu=pu: e.tensor_tensor(dst, sl.ap[:], pu.ap[:], ALU.mult),
                                     reads=[sl, pu], writes=[(act, (fc, tc))])
                for pair in range(8):
                    s, wv = self.wtile(wd[:, fbase:fbase + gsz, pair * 256:pair * 256 + 256], gsz, 256)
                    for sub in range(2):
                        dc = pair * 2 + sub
                        for tc in range(NTC):
                            pb = self.ps[4 + (dc * 2 + tc) % 4]
                            for fc in range(gsz):
                                self.mm(pb.ap[:], wv[:, fc, sub * 128:(sub + 1) * 128], act.ap[:, fc, tc * TCW:(tc + 1) * TCW],
                                        fc == 0, fc == gsz - 1, reads=[s, (act, (fc, tc))], writes=[pb])
                            P.op("dve", lambda e, dc=dc, tc=tc, pb=pb: e.scalar_tensor_tensor(
                                self.xT.ap[:, dc, tc * TCW:(tc + 1) * TCW], pb.ap[:], self.modT.ap[:, 80 + dc:81 + dc],
                                self.xT.ap[:, dc, tc * TCW:(tc + 1) * TCW], ALU.mult, ALU.add),
                                reads=[pb, self.modT, (self.xT, (dc, tc))], writes=[(self.xT, (dc, tc))])
                fbase += gsz
        P.release(m0)

    def emit_final(self, io, out_ap):
        P = self.P
        m0 = P.mark()
        fg = P.alloc("fg", [128, KC], F32)
        P.dma("sp", fg, fg.ap[:], None, io["final_gT"])
        refs = []
        for tc in range(NTC):
            m1 = P.mark()
            o32 = P.alloc("o32", [128, KC, TCW], F32)
            self.emit_norm(tc, o32, 0, lambda kc: fg.ap[:, kc:kc + 1], None, gbufs=[fg])
            refs.append(P.dma("sp", None, out_ap[:, :, tc * TCW:(tc + 1) * TCW], o32, o32.ap[:]))
            P.release(m1)
        P.release(m0)
        return refs


def _dram_in(nc, name, shape, dt):
    return nc.dram_tensor(name, list(shape), dt, kind="ExternalInput").ap()


def _dram_out(nc, name, shape, dt):
    return nc.dram_tensor(name, list(shape), dt, kind="ExternalOutput").ap()


def build_A():
    nc = bass.Bass("TRN2", target_bir_lowering=False)
    io = {
        "xT": _dram_in(nc, "xT", [128, KC, TL], F32),
        "cT": _dram_in(nc, "cT", [128, KC], F32),
        "pos": _dram_in(nc, "pos", [64, TL], I32),
        "qpos": _dram_in(nc, "qpos", [128, TL], I32),
        "w_ada": _dram_in(nc, "w_ada", [D, 6 * D], F32),
        "b_adaT": _dram_in(nc, "b_adaT", [128, 96], F32),
        "g_mixT": _dram_in(nc, "g_mixT", [128, KC], F32),
        "g_ffnT": _dram_in(nc, "g_ffnT", [128, KC], F32),
        "q_gT": _dram_in(nc, "q_gT", [128, 4], F32),
        "kv_gT": _dram_in(nc, "kv_gT", [128, 4], F32),
        "w_in": _dram_in(nc, "w_in", [D, IN_COLS], F32),
        "w_ukv": _dram_in(nc, "w_ukv", [512, 2048], F32),
    }
    kvloc = _dram_out(nc, "kvloc", [128, XW], BF16)
    qsb_o = _dram_out(nc, "qsb", [128, NH, TL], BF16)
    cqn_o = _dram_out(nc, "cqn", [128, 4, TL], BF16)
    mod_o = _dram_out(nc, "modo", [128, 128], F32)
    k = K(nc)
    P = k.P
    P.dma("sp", k.xT, k.xT.ap[:], None, io["xT"])
    k.emit_setup(io)
    k.emit_mod(io)
    qsb = P.alloc("qsb", [128, NH, TL], BF16, phase=False)
    cqn = P.alloc("cqn", [128, 4, TL], BF16, phase=False)
    kvb = Buf("kvloc")
    kvx = KVX([kvloc], [kvb], XW)
    for tc in range(NTC):
        m = P.mark()
        k.emit_A(io, tc, qsb, cqn, kvx)
        P.release(m)
    refs = []
    refs.append(P.dma("sp", None, qsb_o, qsb, qsb.ap[:]))
    refs.append(P.dma("sp", None, cqn_o, cqn, cqn.ap[:]))
    refs.append(P.dma("sp", None, mod_o[:, 0:96], k.modT, k.modT.ap[:]))
    refs.append(P.dma("sp", None, mod_o[:, 96:128], k.lc, k.lc.ap[:]))
    for d in P.state.get(kvb.id, {}).values():
        if d[0] is not None:
            refs.append(d[0])
    P.finish(refs)
    P.emit()
    return nc, P


def build_B(moe, last):
    nc = bass.Bass("TRN2", target_bir_lowering=False)
    io = {
        "xT": _dram_in(nc, "xT", [128, KC, TL], F32),
        "cT": _dram_in(nc, "cT", [128, KC], F32),
        "pos": _dram_in(nc, "pos", [64, TL], I32),
        "qpos": _dram_in(nc, "qpos", [128, TL], I32),
        "kvfull": _dram_in(nc, "kvfull", [512, XW], BF16),
        "qsb": _dram_in(nc, "qsb", [128, NH, TL], BF16),
        "cqn": _dram_in(nc, "cqn", [128, 4, TL], BF16),
        "modo": _dram_in(nc, "modo", [128, 128], F32),
        "w_in": _dram_in(nc, "w_in", [D, IN_COLS], F32),
        "w_uq": _dram_in(nc, "w_uq", [512, 1536], F32),
        "w_sb_up": _dram_in(nc, "w_sb_up", [1024, D], F32),
        "w_mla_up": _dram_in(nc, "w_mla_up", [1024, D], F32),
        "w_o": _dram_in(nc, "w_o", [D, D], F32),
    }
    if moe:
        io["w_routerT"] = _dram_in(nc, "w_routerT", [128, KC, 8], F32)
        io["w_exp_gate"] = _dram_in(nc, "w_exp_gate", [NE, D, DFF], F32)
        io["w_exp_up"] = _dram_in(nc, "w_exp_up", [NE, D, DFF], F32)
        io["w_exp_down"] = _dram_in(nc, "w_exp_down", [NE, DFF, D], F32)
    else:
        io["w_ffn_gate"] = _dram_in(nc, "w_ffn_gate", [D, DFF], F32)
        io["w_ffn_up"] = _dram_in(nc, "w_ffn_up", [D, DFF], F32)
        io["w_ffn_down"] = _dram_in(nc, "w_ffn_down", [DFF, D], F32)
    if last:
        io["final_gT"] = _dram_in(nc, "final_gT", [128, KC], F32)
    xo = _dram_out(nc, "xo", [128, KC, TL], F32)
    k = K(nc)
    P = k.P
    P.dma("sp", k.xT, k.xT.ap[:], None, io["xT"])
    k.emit_setup(io)
    P.dma("sp", k.modT, k.modT.ap[:], None, io["modo"][:, 0:96])
    P.dma("sp", k.lc, k.lc.ap[:], None, io["modo"][:, 96:128])
    base = P.mark()
    qsb = P.alloc("qsb", [128, NH, TL], BF16)
    cqn = P.alloc("cqn", [128, 4, TL], BF16)
    P.dma("sp", qsb, qsb.ap[:], None, io["qsb"])
    P.dma("sp", cqn, cqn.ap[:], None, io["cqn"])
    kvb = Buf("kvfull")
    kvx = KVX([io["kvfull"]], [kvb], XW)
    for tc in range(NTC):
        m = P.mark()
        osb = P.alloc("osb", [128, NH, TCW], BF16)
        omla = P.alloc("omla", [128, NH, TCW], BF16)
        m2 = P.mark()
        k.emit_attn(io, tc, qsb, cqn, kvx, osb, omla)
        P.release(m2)
        k.emit_C(io, tc, osb, omla)
        P.release(m)
    P.release(base)
    k.emit_ffn(io, moe)
    if last:
        refs = k.emit_final(io, xo)
    else:
        refs = [P.dma("sp", None, xo, k.xT, k.xT.ap[:])]
    P.finish(refs)
    P.emit()
    return nc, P


def _layer_io(io, l):
    jj = l // 2
    d = {"w_in": io["w_in"][l], "w_ukv": io["w_ukv"][l], "w_uq": io["w_uq"][l], "w_sb_up": io["w_sb_up"][l],
         "w_mla_up": io["w_mla_up"][l], "w_o": io["w_o"][l], "q_gT": io["q_gT"][l], "kv_gT": io["kv_gT"][l]}
    if l % 2 == 1:
        d["w_routerT"] = io["w_routerT"][jj]
        d["w_exp_gate"] = io["w_exp_gate"][jj]
        d["w_exp_up"] = io["w_exp_up"][jj]
        d["w_exp_down"] = io["w_exp_down"][jj]
    else:
        d["w_ffn_gate"] = io["w_ffn_gate"][jj]
        d["w_ffn_up"] = io["w_ffn_up"][jj]
        d["w_ffn_down"] = io["w_ffn_down"][jj]
    return d


GROUPS4 = [[0, 1, 2, 3], [4, 5, 6, 7]]


def build_fused(depth=DEPTH, final=True):
    nc = bass.Bass("TRN2", target_bir_lowering=False, num_devices=8)
    nd, nm = (depth + 1) // 2, depth // 2
    io = {
        "xT": _dram_in(nc, "xT", [128, KC, TL], F32),
        "cT": _dram_in(nc, "cT", [128, KC], F32),
        "pos": _dram_in(nc, "pos", [64, TL], I32),
        "qpos": _dram_in(nc, "qpos", [128, TL], I32),
        "w_ada_sh": _dram_in(nc, "w_ada_sh", [DEPTH, D, 3072], F32),
        "b_adaT": _dram_in(nc, "b_adaT", [DEPTH, 128, 96], F32),
        "g_mixT": _dram_in(nc, "g_mixT", [DEPTH, 128, KC], F32),
        "g_ffnT": _dram_in(nc, "g_ffnT", [DEPTH, 128, KC], F32),
        "q_gT": _dram_in(nc, "q_gT", [DEPTH, 128, 4], F32),
        "kv_gT": _dram_in(nc, "kv_gT", [DEPTH, 128, 4], F32),
        "w_in": _dram_in(nc, "w_in", [depth, D, IN_COLS], F32),
        "w_ukv": _dram_in(nc, "w_ukv", [depth, 512, 2048], F32),
        "w_uq": _dram_in(nc, "w_uq", [depth, 512, 1536], F32),
        "w_sb_up": _dram_in(nc, "w_sb_up", [depth, 1024, D], F32),
        "w_mla_up": _dram_in(nc, "w_mla_up", [depth, 1024, D], F32),
        "w_o": _dram_in(nc, "w_o", [depth, D, D], F32),
        "w_ffn_gate": _dram_in(nc, "w_ffn_gate", [nd, D, DFF], F32),
        "w_ffn_up": _dram_in(nc, "w_ffn_up", [nd, D, DFF], F32),
        "w_ffn_down": _dram_in(nc, "w_ffn_down", [nd, DFF, D], F32),
        "final_gT": _dram_in(nc, "final_gT", [128, KC], F32),
    }
    if nm > 0:
        io["w_routerT"] = _dram_in(nc, "w_routerT", [nm, 128, KC, 8], F32)
        io["w_exp_gate"] = _dram_in(nc, "w_exp_gate", [nm, NE, D, DFF], F32)
        io["w_exp_up"] = _dram_in(nc, "w_exp_up", [nm, NE, D, DFF], F32)
        io["w_exp_down"] = _dram_in(nc, "w_exp_down", [nm, NE, DFF, D], F32)
    xo = _dram_out(nc, "xo", [128, KC, TL], F32)
    modloc = nc.dram_tensor("modloc", [1, DEPTH * 3072], F32, kind="Internal").ap()
    modfull = nc.dram_tensor("modfull", [4, DEPTH * 3072], F32, kind="Internal").ap()
    PW = 2048
    widths = [PW] * 16 + [1024]
    kvlx, kvfx = [], []
    for par in range(2):
        la = [nc.dram_tensor(f"kvloc{par}_{i}", [128, w], BF16, kind="Internal").ap() for i, w in enumerate(widths)]
        fa = [nc.dram_tensor(f"kvfull{par}_{i}", [512, w], BF16, kind="Internal").ap() for i, w in enumerate(widths)]
        kvlx.append(KVX(la, [Buf(f"kvl{par}_{i}") for i in range(17)], PW))
        kvfx.append(KVX(fa, [Buf(f"kvf{par}_{i}") for i in range(17)], PW))
    k = K(nc)
    P = k.P
    P.dma("sp", k.xT, k.xT.ap[:], None, io["xT"])
    k.emit_setup(io)
    mlb, mfb = Buf("modloc"), Buf("modfull")
    k.emit_mod_shard(io, mlb, modloc)
    P.collective_allgather(mlb, modloc, mfb, modfull, GROUPS4)
    base = P.mark()
    for l in range(depth):
        lio = _layer_io(io, l)
        k.emit_mod_load(io, l, mfb, modfull)
        qsb = P.alloc("qsb", [128, NH, TL], BF16)
        cqn = P.alloc("cqn", [128, 4, TL], BF16)
        par = l % 2
        for tc in range(NTC):
            m = P.mark()
            k.emit_A(lio, tc, qsb, cqn, kvlx[par])
            P.release(m)
        order = []
        for hp in range(4):
            order += [X_K // PW + hp, X_V // PW + hp]
        order.append(16)
        for hp in range(4):
            order += [X_KN // PW + hp, X_VM // PW + hp]
        for i in order:
            P.collective_allgather(kvlx[par].bufs[i], kvlx[par].aps[i], kvfx[par].bufs[i], kvfx[par].aps[i], GROUPS4)
        for tc in range(NTC):
            m = P.mark()
            osb = P.alloc("osb", [128, NH, TCW], BF16)
            omla = P.alloc("omla", [128, NH, TCW], BF16)
            m2 = P.mark()
            k.emit_attn(lio, tc, qsb, cqn, kvfx[par], osb, omla)
            P.release(m2)
            k.emit_C(lio, tc, osb, omla)
            P.release(m)
        P.release(base)
        k.emit_ffn(lio, l % 2 == 1)
    if final:
        refs = k.emit_final(io, xo)
    else:
        refs = [P.dma("sp", None, xo, k.xT, k.xT.ap[:])]
    P.finish(refs)
    P.emit()
    return nc, P


_CACHE = {}


def _get(name, fn):
    if name not in _CACHE:
        _CACHE[name] = fn()
    return _CACHE[name]


def _colT(v, n):
    return np.ascontiguousarray(np.asarray(v, np.float32).reshape(n, 128).T)


def _tok_index(j):
    m = np.arange(8)[:, None]
    i = np.arange(128)[None, :]
    return ((4 * m + j) * 128 + i).reshape(-1)


def _host_inputs(x, c, positions, w_ada, b_ada, norm_mix_g, norm_ffn_g, w_in, q_norm_g, kv_norm_g,
                 w_uq, w_ukv, w_sb_up, w_mla_up, w_o, w_ffn_gate, w_ffn_up, w_ffn_down,
                 w_router, w_exp_gate, w_exp_up, w_exp_down, final_norm_g, depth=DEPTH):
    f32 = lambda a: np.ascontiguousarray(np.asarray(a, np.float32))
    x = np.asarray(x, np.float32)
    nd, nm = (depth + 1) // 2, depth // 2
    shared = {
        "b_adaT": np.stack([_colT(np.asarray(b_ada)[l], 96) for l in range(DEPTH)]),
        "g_mixT": np.stack([_colT(np.asarray(norm_mix_g)[l], KC) for l in range(DEPTH)]),
        "g_ffnT": np.stack([_colT(np.asarray(norm_ffn_g)[l], KC) for l in range(DEPTH)]),
        "q_gT": np.stack([_colT(np.asarray(q_norm_g)[l], 4) for l in range(DEPTH)]),
        "kv_gT": np.stack([_colT(np.asarray(kv_norm_g)[l], 4) for l in range(DEPTH)]),
        "w_in": f32(np.asarray(w_in)[:depth]), "w_ukv": f32(np.asarray(w_ukv)[:depth]), "w_uq": f32(np.asarray(w_uq)[:depth]),
        "w_sb_up": f32(np.asarray(w_sb_up)[:depth]), "w_mla_up": f32(np.asarray(w_mla_up)[:depth]), "w_o": f32(np.asarray(w_o)[:depth]),
        "w_ffn_gate": f32(np.asarray(w_ffn_gate)[:nd]), "w_ffn_up": f32(np.asarray(w_ffn_up)[:nd]),
        "w_ffn_down": f32(np.asarray(w_ffn_down)[:nd]),
        "final_gT": _colT(final_norm_g, KC),
    }
    if nm > 0:
        shared["w_routerT"] = np.ascontiguousarray(
            np.asarray(w_router, np.float32)[:nm].reshape(nm, KC, 128, 8).transpose(0, 2, 1, 3))
        shared["w_exp_gate"] = f32(np.asarray(w_exp_gate)[:nm])
        shared["w_exp_up"] = f32(np.asarray(w_exp_up)[:nm])
        shared["w_exp_down"] = f32(np.asarray(w_exp_down)[:nm])
    w_ada = np.asarray(w_ada, np.float32)
    ada_sh = [np.ascontiguousarray(w_ada[:, :, j * 3072:(j + 1) * 3072]) for j in range(4)]
    in_maps, toks = [], []
    for cid in range(8):
        b, j = cid // 4, cid % 4
        tok = _tok_index(j)
        toks.append(tok)
        xs = x[b, tok, :]
        d = dict(shared)
        d["xT"] = np.ascontiguousarray(xs.T.reshape(KC, 128, TL).transpose(1, 0, 2))
        d["cT"] = _colT(np.asarray(c)[b], KC)
        d["pos"] = np.ascontiguousarray(np.broadcast_to(np.asarray(positions)[b, tok].astype(np.int32)[None, :], (64, TL)))
        d["qpos"] = np.ascontiguousarray(np.broadcast_to(tok.astype(np.int32)[None, :], (128, TL)))
        d["w_ada_sh"] = ada_sh[j]
        in_maps.append(d)
    return in_maps, toks


def kernel(x, c, positions, w_ada, b_ada, norm_mix_g, norm_ffn_g, w_in, q_norm_g, kv_norm_g,
           w_uq, w_ukv, w_sb_up, w_mla_up, w_o, w_ffn_gate, w_ffn_up, w_ffn_down,
           w_router, w_exp_gate, w_exp_up, w_exp_down, final_norm_g):
    in_maps, toks = _host_inputs(x, c, positions, w_ada, b_ada, norm_mix_g, norm_ffn_g, w_in, q_norm_g, kv_norm_g,
                                 w_uq, w_ukv, w_sb_up, w_mla_up, w_o, w_ffn_gate, w_ffn_up, w_ffn_down,
                                 w_router, w_exp_gate, w_exp_up, w_exp_down, final_norm_g)
    nc = _get("fused", build_fused)[0]
    res = run_bass_kernel_spmd(nc, in_maps, core_ids=list(range(8))).results
    out = np.zeros((NB, SEQ, D), np.float32)
    for cid in range(8):
        xs = np.asarray(res[cid]["xo"]).transpose(1, 0, 2).reshape(D, TL).T
        out[cid // 4, toks[cid], :] = xs
    return out
```

```python
import contextlib
import math
import numpy as np
import ml_dtypes
import concourse.bass as bass
import concourse.mybir as mybir
from concourse.bass_utils import run_bass_kernel_spmd

F32 = mybir.dt.float32
BF16 = mybir.dt.bfloat16
I32 = mybir.dt.int32
AF = mybir.ActivationFunctionType
ALU = mybir.AluOpType

ENGS = ["pe", "act", "dve", "pool", "sp"]

D = 2048
KC = 16
SEQ = 4096
NB = 2
DEPTH = 4
TL = 1024
NTC = 2
TCW = 512
NH = 8
DFF = 5632
NFC = 44
GROUPS = [12, 12, 10, 10]
GMAX = 12
NE = 8
IN_COLS = 8256
OFF_Q, OFF_K, OFF_V, OFF_CQ, OFF_CKV, OFF_KPE, OFF_GSB, OFF_GMLA = 0, 1024, 2048, 3072, 3584, 4096, 4160, 6208
X_K, X_V, X_KN, X_VM, X_KPE = 0, 8192, 16384, 24576, 32768
XW = 33792
EPS = 1e-6
SB_SCALE = 128 ** -0.5
MLA_SCALE = 192 ** -0.5
SLOT_EL = 4096
NSLOT = 5


class Buf:
    _n = 0

    def __init__(self, name, ap=None, phase=False):
        Buf._n += 1
        self.id = Buf._n
        self.name = name
        self.ap = ap
        self.phase = phase
        self.dsem = None
        self.dcnt = 0
        self.rsem = None
        self.rcnt = 0


class Prog:
    def __init__(self, nc, arena_kb=206):
        self.nc = nc
        self.eng = {"pe": nc.tensor, "act": nc.scalar, "dve": nc.vector,
                    "pool": nc.gpsimd, "sp": nc.sync}
        self.q = {e: [] for e in ENGS}
        self.cnt = {e: 0 for e in ENGS}
        self.known = {e: {} for e in ENGS}
        self.stack = contextlib.ExitStack()
        self.esem = {e: self.stack.enter_context(nc.semaphore("s_" + e)) for e in ENGS}
        self.state = {}
        self.nsem = 0
        self.ninstr = 0
        self.open_dma = []
        self.arena_words = arena_kb * 256
        self.arena = self.stack.enter_context(nc.sbuf_tensor("arena", [128, self.arena_words], F32))
        self.top = 0
        self.peak = 0
        self.sem_pool = {}
        self.ccsem = None
        self.cccnt = 0
        self.live = []
        self.free_sems = []
        self.pool_pending = []

    def alloc(self, name, shape, dtype, phase=True):
        esz = 2 if dtype == BF16 else 4
        nparts = shape[0]
        nel = 1
        for s in shape[1:]:
            nel *= s
        nbytes = (nel * esz + 31) // 32 * 32
        off = self.top
        self.top += nbytes
        self.peak = max(self.peak, self.top)
        assert self.top <= self.arena_words * 4, f"SBUF arena overflow at {name}: {self.top}"
        v = self.arena[0:nparts, off // 4:(off + nbytes) // 4]
        if esz == 2:
            v = v.bitcast(BF16)
        elif dtype != F32:
            v = v.bitcast(dtype)
        v = v[:, 0:nel]
        if len(shape) == 3:
            v = v.rearrange("p (a b) -> p a b", a=shape[1])
        elif len(shape) == 4:
            v = v.rearrange("p (a b c) -> p a b c", a=shape[1], b=shape[2])
        b = Buf(name, v, phase=phase)
        self.live.append((off, b))
        return b

    def mark(self):
        return self.top

    def release(self, mark):
        self.barrier()
        self.top = mark
        keep = []
        for (off, b) in self.live:
            if off >= mark:
                if b.dsem is not None:
                    self.free_sems.append((b.dsem, b.dcnt))
                    b.dsem = None
                if b.rsem is not None:
                    self.free_sems.append((b.rsem, b.rcnt))
                    b.rsem = None
            else:
                keep.append((off, b))
        self.live = keep

    def sem_for(self, buf, kind):
        if self.free_sems:
            sem, cnt = self.free_sems.pop()
        else:
            sem, cnt = self.new_sem(kind), 0
        if kind == "d":
            buf.dsem, buf.dcnt = sem, cnt
        else:
            buf.rsem, buf.rcnt = sem, cnt

    def collective_allgather(self, in_buf, in_ap, out_buf, out_ap, groups):
        e = "pool"
        reads = [(in_buf, None)]
        writes = [(out_buf, None)]
        self._flush_pool_pending()
        self._emit_waits(e, self._collect(reads, writes))
        if self.ccsem is None:
            self.ccsem = self.new_sem("cc")
        self.cccnt += 1
        sem = self.ccsem
        self.ninstr += 1
        self.q[e].append(lambda eng=self.eng[e], sem=sem, i=in_ap, o=out_ap, groups=groups:
                         eng.collective_compute("AllGather", ALU.bypass, replica_groups=groups, ins=[i], outs=[o]).then_inc(sem, 1))
        ref = ("sem", sem, ("cc",), self.cccnt)
        self._record(ref, reads, writes)
        return ref

    def _flush_pool_pending(self):
        if self.pool_pending:
            refs = self.pool_pending
            self.pool_pending = []
            self._emit_waits("pool", refs)

    def psum(self, name):
        t = self.stack.enter_context(self.nc.psum_tensor(name, [128, 512], F32))
        return Buf(name, t)

    def new_sem(self, name):
        self.nsem += 1
        return self.stack.enter_context(self.nc.semaphore(f"{name}_{self.nsem}"))

    def _conf(self, b, key):
        d = self.state.get(b.id)
        if not d:
            return []
        if key is None:
            return list(d.values())
        out = []
        s = d.get(key)
        if s is not None:
            out.append(s)
        s = d.get(None)
        if s is not None:
            out.append(s)
        return out

    def _collect(self, reads, writes):
        refs = []
        for (b, key) in reads:
            for s in self._conf(b, key):
                if s[0] is not None:
                    refs.append(s[0])
        for (b, key) in writes:
            for s in self._conf(b, key):
                if s[0] is not None:
                    refs.append(s[0])
                refs.extend(s[1].values())
        return refs

    @staticmethod
    def _rk(ref):
        return ("eng", ref[1]) if ref[0] == "eng" else ("sem", ref[2])

    @staticmethod
    def _rv(ref):
        return ref[2] if ref[0] == "eng" else ref[3]

    def _record(self, ref, reads, writes):
        rk = self._rk(ref)
        for (b, key) in reads:
            d = self.state.setdefault(b.id, {})
            s = d.get(key)
            if s is None:
                s = [None, {}]
                d[key] = s
            old = s[1].get(rk)
            if old is None or self._rv(old) < self._rv(ref):
                s[1][rk] = ref
        for (b, key) in writes:
            d = self.state.setdefault(b.id, {})
            if key is None:
                d.clear()
            d[key] = [ref, {}]

    def _emit_waits(self, e, refs):
        need = {}
        for r in refs:
            if r[0] == "eng" and r[1] == e and e == "pe":
                continue
            k = self._rk(r)
            v = self._rv(r)
            sem = self.esem[r[1]] if r[0] == "eng" else r[1]
            if k not in need or need[k][1] < v:
                need[k] = (sem, v)
        kn = self.known[e]
        for k, (sem, v) in need.items():
            if kn.get(k, 0) >= v:
                continue
            kn[k] = v
            self.q[e].append(lambda eng=self.eng[e], sem=sem, v=v: eng.wait_ge(sem, v))

    @staticmethod
    def _norm(lst):
        out = []
        for x in lst or []:
            out.append((x, None) if isinstance(x, Buf) else x)
        return out

    def op(self, e, fn, reads=None, writes=None, inc=True):
        reads = self._norm(reads)
        writes = self._norm(writes)
        if e == "pool":
            self._flush_pool_pending()
        self._emit_waits(e, self._collect(reads, writes))
        self.ninstr += 1
        if inc:
            self.cnt[e] += 1
            idx = self.cnt[e]
            self.q[e].append(lambda eng=self.eng[e], fn=fn, sem=self.esem[e]: fn(eng).then_inc(sem, 1))
        else:
            idx = self.cnt[e] + 1
            self.q[e].append(lambda eng=self.eng[e], fn=fn: fn(eng))
        self._record(("eng", e, idx), reads, writes)

    def dma(self, e, out_buf, out_ap, in_buf, in_ap, out_key=None, in_key=None, **kw):
        reads = [(in_buf, in_key)] if in_buf is not None else []
        writes = [(out_buf, out_key)] if out_buf is not None else []
        if e == "pool" and ((out_buf is not None and out_buf.phase) or (in_buf is not None and in_buf.phase)):
            self._flush_pool_pending()
        self._emit_waits(e, self._collect(reads, writes))
        self.ninstr += 1
        tb = out_buf if (out_buf is not None and out_buf.ap is not None) else (in_buf if in_buf is not None else out_buf)
        if tb is out_buf:
            if tb.dsem is None:
                self.sem_for(tb, "d")
            tb.dcnt += 16
            ref = ("sem", tb.dsem, ("d", tb.id), tb.dcnt)
            sem = tb.dsem
        else:
            if tb.rsem is None:
                self.sem_for(tb, "r")
            tb.rcnt += 16
            ref = ("sem", tb.rsem, ("r", tb.id), tb.rcnt)
            sem = tb.rsem
        self.q[e].append(lambda eng=self.eng[e], o=out_ap, i=in_ap, sem=sem, kw=kw:
                         eng.dma_start(out=o, in_=i, **kw).then_inc(sem, 16))
        self._record(ref, reads, writes)
        if (out_buf is not None and out_buf.phase) or (in_buf is not None and in_buf.phase):
            self.open_dma.append(ref)
        return ref

    def dma_multi(self, e, out_buf, parts, **kw):
        writes = [(out_buf, None)]
        if e == "pool" and out_buf.phase:
            self._flush_pool_pending()
        self._emit_waits(e, self._collect([], writes))
        if out_buf.dsem is None:
            self.sem_for(out_buf, "d")
        sem = out_buf.dsem
        for (o, i) in parts:
            self.ninstr += 1
            out_buf.dcnt += 16
            self.q[e].append(lambda eng=self.eng[e], o=o, i=i, sem=sem, kw=kw:
                             eng.dma_start(out=o, in_=i, **kw).then_inc(sem, 16))
        ref = ("sem", sem, ("d", out_buf.id), out_buf.dcnt)
        self._record(ref, [], writes)
        if out_buf.phase:
            self.open_dma.append(ref)
        return ref

    def barrier(self, engines=ENGS, lazy_pool=True):
        for e in engines:
            refs = [("eng", o, self.cnt[o]) for o in ENGS if o != e and self.cnt[o] > 0]
            refs += self.open_dma
            if e == "pool" and lazy_pool:
                self.pool_pending = self.pool_pending + refs
                continue
            self._emit_waits(e, refs)
        self.open_dma = []

    def finish(self, final_refs):
        self._emit_waits("sp", list(final_refs))
        self.barrier(["sp"])

    def emit(self):
        with self.nc.Block() as block:
            @block.tensor
            def _(eng):
                for f in self.q["pe"]:
                    f()

            @block.scalar
            def _(eng):
                for f in self.q["act"]:
                    f()

            @block.vector
            def _(eng):
                for f in self.q["dve"]:
                    f()

            @block.gpsimd
            def _(eng):
                for f in self.q["pool"]:
                    f()

            @block.sync
            def _(eng):
                for f in self.q["sp"]:
                    f()
        self.stack.close()


class KVX:
    def __init__(self, aps, bufs, pw):
        self.aps, self.bufs, self.pw = aps, bufs, pw

    def loc(self, x0, w, rows=None):
        i, off = x0 // self.pw, x0 % self.pw
        assert off + w <= self.pw
        ap = self.aps[i]
        ap = ap[:, off:off + w] if rows is None else ap[rows[0]:rows[1], off:off + w]
        return self.bufs[i], ap

    def full(self, x0, w, rows=None):
        i, off = x0 // self.pw, x0 % self.pw
        assert off + w <= self.pw
        ap = self.aps[i].rearrange("(r p) x -> p r x", p=128)
        ap = ap[:, :, off:off + w] if rows is None else ap[rows[0]:rows[1], :, off:off + w]
        return self.bufs[i], ap


class K:
    def __init__(self, nc):
        self.nc = nc
        P = self.P = Prog(nc)
        self.ps = [P.psum(f"ps{i}") for i in range(8)]
        self.xT = P.alloc("xT", [128, KC, TL], F32, phase=False)
        self.slots = [P.alloc(f"slot{i}", [128, SLOT_EL], BF16, phase=False) for i in range(NSLOT)]
        self.slot_i = 0
        self.ones = P.alloc("ones", [128, 128], BF16, phase=False)
        self.tincl = P.alloc("tincl", [128, 128], BF16, phase=False)
        self.kposc = P.alloc("kposc", [128, 32], F32, phase=False)
        self.qposb = P.alloc("qposb", [128, TL], F32, phase=False)
        self.cc = P.alloc("cc", [64, TL], F32, phase=False)
        self.ss = P.alloc("ss", [64, TL], F32, phase=False)
        self.modT = P.alloc("modT", [128, 96], F32, phase=False)
        self.lc = P.alloc("lc", [128, 32], F32, phase=False)
        self.cact = P.alloc("cact", [128, KC], BF16, phase=False)
        self.one1 = P.alloc("one1", [1, 8], F32, phase=False)
        self.rr = 0

    def mm(self, out, lhsT, rhs, start, stop, reads, writes, inc=None):
        if inc is None:
            inc = stop
        self.P.op("pe", lambda e: e.matmul(out, lhsT, rhs, start=start, stop=stop),
                  reads=reads, writes=writes, inc=inc)

    def wslot(self):
        s = self.slots[self.slot_i % NSLOT]
        self.slot_i += 1
        return s

    def wload(self, parts):
        P = self.P
        s = self.wslot()
        for (view, src) in parts(s):
            P.dma("pool", s, view, None, src)
        return s

    def wtile(self, src, a, b):
        s = self.wslot()
        v = s.ap[:, 0:a * b].rearrange("p (a b) -> p a b", a=a)
        self.P.dma("pool", s, v, None, src)
        return s, v

    def evac(self, fn_act, fn_dve, reads, writes):
        self.rr += 1
        if self.rr % 2 == 0:
            self.P.op("act", fn_act, reads=reads, writes=writes)
        else:
            self.P.op("dve", fn_dve, reads=reads, writes=writes)

    def emit_setup(self, io):
        P = self.P
        nc = self.nc
        ones, tincl = self.ones, self.tincl
        P.op("dve", lambda e: e.memset(ones.ap[:], 1.0), writes=[ones])
        P.op("pool", lambda e: e.affine_select(tincl.ap[:], ones.ap[:], [[-1, 128]], ALU.is_ge, 0.0,
                                               base=0, channel_multiplier=1), reads=[ones], writes=[tincl])
        m0 = P.mark()
        ti = P.alloc("ti", [128, 32], I32)
        P.op("dve", lambda e: e.memset(self.one1.ap[:], 1.0), writes=[self.one1])
        P.op("pool", lambda e: e.iota(ti.ap[:], [[128, 32]], base=0, channel_multiplier=1), writes=[ti])
        P.op("dve", lambda e: e.tensor_copy(self.kposc.ap[:], ti.ap[:]), reads=[ti], writes=[self.kposc])
        qi = P.alloc("qi", [128, TL], I32)
        P.dma("sp", qi, qi.ap[:], None, io["qpos"])
        P.op("dve", lambda e: e.tensor_copy(self.qposb.ap[:], qi.ap[:]), reads=[qi], writes=[self.qposb])
        pi = P.alloc("pi", [64, TL], I32)
        P.dma("sp", pi, pi.ap[:], None, io["pos"])
        posf = P.alloc("posf", [64, TL], F32)
        P.op("dve", lambda e: e.tensor_copy(posf.ap[:], pi.ap[:]), reads=[pi], writes=[posf])
        fi = P.alloc("fi", [64, 2], I32)
        ff = P.alloc("ff", [64, 4], F32)
        P.op("pool", lambda e: e.iota(fi.ap[0:32, 0:1], [[0, 1]], base=0, channel_multiplier=1), writes=[fi])
        P.op("pool", lambda e: e.iota(fi.ap[32:64, 0:1], [[0, 1]], base=0, channel_multiplier=1), writes=[fi])
        P.op("dve", lambda e: e.tensor_copy(ff.ap[:, 0:1], fi.ap[:, 0:1]), reads=[fi], writes=[ff])
        P.op("act", lambda e: e.activation(ff.ap[:, 1:2], ff.ap[:, 0:1], AF.Exp, scale=-math.log(10000.0) / 32.0),
             reads=[ff], writes=[ff])
        P.op("dve", lambda e: e.memset(ff.ap[0:32, 2:3], -1.0), reads=[ff], writes=[ff])
        P.op("dve", lambda e: e.memset(ff.ap[32:64, 2:3], 1.0), reads=[ff], writes=[ff])
        ang = P.alloc("ang", [64, TL], F32)
        P.op("dve", lambda e: e.tensor_scalar(ang.ap[:], posf.ap[:], ff.ap[:, 1:2], None, ALU.mult),
             reads=[posf, ff], writes=[ang])
        kf = P.alloc("kf", [64, TL], F32)
        ki = P.alloc("ki", [64, TL], I32)
        r = P.alloc("r", [64, TL], F32)
        g = P.alloc("g", [64, TL], F32)
        TWO_PI = 2.0 * math.pi
        C1 = 6.28125
        C2 = TWO_PI - C1

        def reduce_sin(dst, shift):
            P.op("dve", lambda e: e.tensor_scalar(kf.ap[:], ang.ap[:], shift, 1.0 / TWO_PI, ALU.add, ALU.mult),
                 reads=[ang], writes=[kf])
            P.op("dve", lambda e: e.tensor_copy(ki.ap[:], kf.ap[:]), reads=[kf], writes=[ki])
            P.op("dve", lambda e: e.tensor_copy(kf.ap[:], ki.ap[:]), reads=[ki], writes=[kf])
            P.op("dve", lambda e: e.scalar_tensor_tensor(r.ap[:], kf.ap[:], -C1, ang.ap[:], ALU.mult, ALU.add),
                 reads=[kf, ang], writes=[r])
            P.op("dve", lambda e: e.scalar_tensor_tensor(r.ap[:], kf.ap[:], -C2, r.ap[:], ALU.mult, ALU.add),
                 reads=[kf, r], writes=[r])
            if shift != 0.0:
                P.op("dve", lambda e: e.tensor_scalar(r.ap[:], r.ap[:], shift, None, ALU.add), reads=[r], writes=[r])
            P.op("dve", lambda e: e.tensor_scalar(g.ap[:], r.ap[:], math.pi, -TWO_PI, ALU.is_gt, ALU.mult),
                 reads=[r], writes=[g])
            P.op("dve", lambda e: e.tensor_tensor(r.ap[:], r.ap[:], g.ap[:], ALU.add), reads=[r, g], writes=[r])
            P.op("dve", lambda e: e.tensor_scalar(g.ap[:], r.ap[:], -math.pi, TWO_PI, ALU.is_lt, ALU.mult),
                 reads=[r], writes=[g])
            P.op("dve", lambda e: e.tensor_tensor(r.ap[:], r.ap[:], g.ap[:], ALU.add), reads=[r, g], writes=[r])
            P.op("dve", lambda e: e.tensor_scalar(r.ap[:], r.ap[:], 3.1415925, -3.1415925, ALU.min, ALU.max),
                 reads=[r], writes=[r])
            P.op("act", lambda e: e.activation(dst, r.ap[:], AF.Sin), reads=[r], writes=[self.cc, self.ss])

        reduce_sin(self.cc.ap[:], math.pi / 2.0)
        reduce_sin(self.ss.ap[:], 0.0)
        P.op("dve", lambda e: e.tensor_scalar(self.ss.ap[:], self.ss.ap[:], ff.ap[:, 2:3], None, ALU.mult),
             reads=[self.ss, ff], writes=[self.ss])
        ct = P.alloc("ct", [128, KC], F32)
        P.dma("sp", ct, ct.ap[:], None, io["cT"])
        P.op("act", lambda e: e.activation(self.cact.ap[:], ct.ap[:], AF.Silu), reads=[ct], writes=[self.cact])
        P.release(m0)

    def emit_mod(self, io):
        P = self.P
        m0 = P.mark()
        wada = io["w_ada"].rearrange("(kc p) n -> p kc n", p=128)
        rowt = [P.alloc(f"modrow{i}", [1, 512], F32) for i in range(2)]
        bT = P.alloc("bT", [128, 96], F32)
        P.dma("sp", bT, bT.ap[:], None, io["b_adaT"])
        gm = P.alloc("gm", [128, 32], F32)
        P.dma_multi("sp", gm, [(gm.ap[:, 0:16], io["g_mixT"]), (gm.ap[:, 16:32], io["g_ffnT"])])
        mps = self.ps[7]
        for blk in range(24):
            pb = self.ps[blk % 2]
            for half in range(2):
                s, wv = self.wtile(wada[:, :, blk * 512 + half * 256: blk * 512 + half * 256 + 256], KC, 256)
                for kc in range(KC):
                    self.mm(pb.ap[0:1, half * 256:(half + 1) * 256], self.cact.ap[:, kc:kc + 1], wv[:, kc, :],
                            kc == 0, kc == KC - 1, reads=[s, self.cact], writes=[(pb, half)])
            row = rowt[blk % 2]
            P.op("act", lambda e, row=row, pb=pb: e.copy(row.ap[:], pb.ap[0:1, :]), reads=[pb], writes=[row])
            for s4 in range(4):
                j = blk * 4 + s4
                self.mm(mps.ap[:, j:j + 1], row.ap[0:1, s4 * 128:(s4 + 1) * 128], self.one1.ap[0:1, 0:1],
                        True, True, reads=[row, self.one1], writes=[(mps, j)], inc=True)
        P.op("dve", lambda e: e.tensor_tensor(self.modT.ap[:], mps.ap[:, 0:96], bT.ap[:], ALU.add),
             reads=[mps, bT], writes=[self.modT])
        P.op("dve", lambda e: e.scalar_tensor_tensor(self.lc.ap[:, 0:16], self.modT.ap[:, 16:32], 1.0, gm.ap[:, 0:16],
                                                     ALU.add, ALU.mult), reads=[self.modT, gm], writes=[self.lc])
        P.op("dve", lambda e: e.scalar_tensor_tensor(self.lc.ap[:, 16:32], self.modT.ap[:, 64:80], 1.0, gm.ap[:, 16:32],
                                                     ALU.add, ALU.mult), reads=[self.modT, gm, self.lc], writes=[self.lc])
        P.release(m0)

    def emit_mod_shard(self, io, modloc_buf, modloc):
        P = self.P
        m0 = P.mark()
        rowt = [P.alloc(f"modrow{i}", [1, 512], F32) for i in range(2)]
        n = 0
        for l in range(DEPTH):
            wada = io["w_ada_sh"][l].rearrange("(kc p) n -> p kc n", p=128)
            for blk in range(6):
                pb = self.ps[n % 2]
                for half in range(2):
                    c0 = blk * 512 + half * 256
                    s, wv = self.wtile(wada[:, :, c0:c0 + 256], KC, 256)
                    for kc in range(KC):
                        self.mm(pb.ap[0:1, half * 256:(half + 1) * 256], self.cact.ap[:, kc:kc + 1], wv[:, kc, :],
                                kc == 0, kc == KC - 1, reads=[s, self.cact], writes=[(pb, half)])
                row = rowt[n % 2]
                P.op("act", lambda e, row=row, pb=pb: e.copy(row.ap[:], pb.ap[0:1, :]), reads=[pb], writes=[row])
                P.dma("sp", modloc_buf, modloc[0:1, l * 3072 + blk * 512: l * 3072 + blk * 512 + 512], row, row.ap[:], out_key=(l, blk))
                n += 1
        P.release(m0)

    def emit_mod_load(self, io, l, modfull_buf, modfull):
        P = self.P
        m0 = P.mark()
        bT = P.alloc("bT", [128, 96], F32)
        P.dma("sp", bT, bT.ap[:], None, io["b_adaT"][l])
        gm = P.alloc("gm", [128, 32], F32)
        P.dma_multi("sp", gm, [(gm.ap[:, 0:16], io["g_mixT"][l]), (gm.ap[:, 16:32], io["g_ffnT"][l])])
        mrow = P.alloc("mrow", [1, 4 * 3072], F32)
        P._emit_waits("sp", P._collect([(modfull_buf, None)], []))
        P.dma_multi("sp", mrow, [(mrow.ap[0:1, r * 3072:(r + 1) * 3072], modfull[r:r + 1, l * 3072:(l + 1) * 3072]) for r in range(4)])
        mraw = self.ps[7]
        for j in range(96):
            self.mm(mraw.ap[:, j:j + 1], mrow.ap[0:1, j * 128:(j + 1) * 128], self.one1.ap[0:1, 0:1],
                    True, True, reads=[mrow, self.one1], writes=[(mraw, j)], inc=True)
        P.op("dve", lambda e: e.tensor_tensor(self.modT.ap[:], mraw.ap[:, 0:96], bT.ap[:], ALU.add), reads=[mraw, bT], writes=[self.modT])
        P.op("dve", lambda e: e.scalar_tensor_tensor(self.lc.ap[:, 0:16], self.modT.ap[:, 16:32], 1.0, gm.ap[:, 0:16],
                                                     ALU.add, ALU.mult), reads=[self.modT, gm], writes=[self.lc])
        P.op("dve", lambda e: e.scalar_tensor_tensor(self.lc.ap[:, 16:32], self.modT.ap[:, 64:80], 1.0, gm.ap[:, 16:32],
                                                     ALU.add, ALU.mult), reads=[self.modT, gm, self.lc], writes=[self.lc])
        P.release(m0)

    def emit_norm(self, tc, hT, hoff, gcol, shcol, gbufs=(), hook=None):
        P = self.P
        t0 = tc * TCW
        m0 = P.mark()
        sq = [P.alloc(f"sq{i}", [128, TCW], BF16) for i in range(3)]
        rstd = P.alloc("rstd", [128, TCW], F32)
        tmp = [P.alloc(f"ntmp{i}", [128, TCW], F32) for i in range(2)]
        h32 = [P.alloc(f"h32_{i}", [128, TCW], F32) for i in range(3)] if hook is not None else None
        pb = self.ps[6]
        gb = list(gbufs)
        for kc in range(KC):
            s = sq[kc % 3]
            P.op("act", lambda e, s=s, kc=kc: e.activation(s.ap[:], self.xT.ap[:, kc, t0:t0 + TCW], AF.Square),
                 reads=[(self.xT, (kc, tc))], writes=[s])
            self.mm(pb.ap[:], self.ones.ap[:], s.ap[:], kc == 0, kc == KC - 1, reads=[s, self.ones], writes=[pb], inc=True)
        P.op("act", lambda e: e.activation(rstd.ap[:], pb.ap[:], AF.Ln, bias=EPS, scale=1.0 / D), reads=[pb], writes=[rstd])
        P.op("act", lambda e: e.activation(rstd.ap[:], rstd.ap[:], AF.Exp, scale=-0.5), reads=[rstd], writes=[rstd])
        for kc in range(KC):
            t = tmp[kc % 2]
            dst = hT.ap[:, kc, hoff:hoff + TCW]
            if shcol is None:
                P.op("dve", lambda e, kc=kc, dst=dst: e.scalar_tensor_tensor(dst, self.xT.ap[:, kc, t0:t0 + TCW], gcol(kc), rstd.ap[:],
                                                                            ALU.mult, ALU.mult),
                     reads=[(self.xT, (kc, tc)), rstd] + gb, writes=[(hT, (kc, tc))])
                continue
            P.op("dve", lambda e, kc=kc, t=t: e.scalar_tensor_tensor(t.ap[:], self.xT.ap[:, kc, t0:t0 + TCW], gcol(kc), rstd.ap[:],
                                                                      ALU.mult, ALU.mult),
                 reads=[(self.xT, (kc, tc)), rstd] + gb, writes=[t])
            if hook is not None:
                hb = h32[kc % 3]
                P.op("act", lambda e, kc=kc, t=t, hb=hb: e.activation(hb.ap[:], t.ap[:], AF.Identity, bias=shcol(kc), scale=1.0),
                     reads=[t] + gb, writes=[hb])
                P.op("dve", lambda e, dst=dst, hb=hb: e.tensor_copy(dst, hb.ap[:]), reads=[hb], writes=[(hT, (kc, tc))])
                hook(kc, hb)
            else:
                P.op("act", lambda e, kc=kc, t=t, dst=dst: e.activation(dst, t.ap[:], AF.Identity, bias=shcol(kc), scale=1.0),
                     reads=[t] + gb, writes=[(hT, (kc, tc))])
        P.release(m0)

    def emit_norm512(self, raw, rbase, out, ooff, gT, goff):
        P = self.P
        m0 = P.mark()
        sq = [P.alloc(f"sqb{i}", [128, TCW], BF16) for i in range(2)]
        rstd = P.alloc("rstdb", [128, TCW], F32)
        pb = self.ps[6]
        for kc in range(4):
            s = sq[kc % 2]
            P.op("act", lambda e, s=s, kc=kc: e.activation(s.ap[:], raw.ap[:, rbase + kc, :], AF.Square), reads=[(raw, rbase + kc)], writes=[s])
            self.mm(pb.ap[:], self.ones.ap[:], s.ap[:], kc == 0, kc == 3, reads=[s, self.ones], writes=[pb], inc=True)
        P.op("act", lambda e: e.activation(rstd.ap[:], pb.ap[:], AF.Ln, bias=EPS, scale=1.0 / 512.0), reads=[pb], writes=[rstd])
        P.op("act", lambda e: e.activation(rstd.ap[:], rstd.ap[:], AF.Exp, scale=-0.5), reads=[rstd], writes=[rstd])
        for kc in range(4):
            P.op("dve", lambda e, kc=kc: e.scalar_tensor_tensor(out.ap[:, kc, ooff:ooff + TCW], raw.ap[:, rbase + kc, :],
                                                                gT.ap[:, goff + kc:goff + kc + 1], rstd.ap[:], ALU.mult, ALU.mult),
                 reads=[(raw, rbase + kc), rstd, gT], writes=[(out, (kc, ooff))])
        P.release(m0)

    def emit_rope(self, p_n, p_sw, t0, dst_buf, dst_ap, tmpa, tmpb):
        P = self.P
        P.op("dve", lambda e: e.tensor_tensor(tmpa.ap[:], p_n.ap[0:64, :], self.cc.ap[:, t0:t0 + TCW], ALU.mult),
             reads=[p_n, self.cc], writes=[tmpa])
        P.op("dve", lambda e: e.tensor_tensor(tmpb.ap[:], p_sw.ap[0:64, :], self.ss.ap[:, t0:t0 + TCW], ALU.mult),
             reads=[p_sw, self.ss], writes=[tmpb])
        P.op("pool", lambda e: e.tensor_tensor(dst_ap, tmpa.ap[:], tmpb.ap[:], ALU.add), reads=[tmpa, tmpb], writes=[dst_buf])

    def emit_A(self, io, tc, qsb, cqn, kvx):
        P = self.P
        t0 = tc * TCW
        gq = P.alloc("gq", [128, 8], F32)
        P.dma_multi("sp", gq, [(gq.ap[:, 0:4], io["q_gT"]), (gq.ap[:, 4:8], io["kv_gT"])])
        hT = P.alloc("hT", [128, KC, TCW], BF16)
        self.emit_norm(tc, hT, 0, lambda kc: self.lc.ap[:, kc:kc + 1], lambda kc: self.modT.ap[:, kc:kc + 1],
                       gbufs=[self.lc, self.modT])
        win = io["w_in"].rearrange("(kc p) n -> p kc n", p=128)
        craw = P.alloc("craw", [128, 8, TCW], F32)
        kst = [P.alloc(f"kst{i}", [128, TCW], BF16) for i in range(2)]
        nst = 0
        for c0 in list(range(0, 2048, 256)) + list(range(OFF_CQ, OFF_KPE, 256)):
            s, wv = self.wtile(win[:, :, c0:c0 + 256], KC, 256)
            for sub in range(2):
                col = c0 + sub * 128
                pb = self.ps[(col // 128) % 4]
                for kc in range(KC):
                    self.mm(pb.ap[:], wv[:, kc, sub * 128:(sub + 1) * 128], hT.ap[:, kc, :], kc == 0, kc == KC - 1,
                            reads=[s, (hT, (kc, tc))], writes=[pb])
                if col < OFF_K:
                    h = col // 128
                    self.evac(lambda e, h=h, pb=pb: e.copy(qsb.ap[:, h, t0:t0 + TCW], pb.ap[:]),
                              lambda e, h=h, pb=pb: e.tensor_copy(qsb.ap[:, h, t0:t0 + TCW], pb.ap[:]),
                              reads=[pb], writes=[(qsb, (h, tc))])
                elif col < OFF_V:
                    h = (col - OFF_K) // 128
                    st = kst[nst % 2]
                    nst += 1
                    self.evac(lambda e, st=st, pb=pb: e.copy(st.ap[:], pb.ap[:]),
                              lambda e, st=st, pb=pb: e.tensor_copy(st.ap[:], pb.ap[:]), reads=[pb], writes=[st])
                    kb_, kap_ = kvx.loc(X_K + h * 1024 + t0, TCW)
                    P.dma("sp", kb_, kap_, st, st.ap[:], out_key=("k", h, tc))
                else:
                    ci = (col - OFF_CQ) // 128
                    self.evac(lambda e, ci=ci, pb=pb: e.copy(craw.ap[:, ci, :], pb.ap[:]),
                              lambda e, ci=ci, pb=pb: e.tensor_copy(craw.ap[:, ci, :], pb.ap[:]),
                              reads=[pb], writes=[(craw, ci)])
        s = self.wslot()
        wv = s.ap[:, 0:KC * 128].rearrange("p (a b) -> p a b", a=KC)
        P.dma_multi("pool", s, [(wv[:, :, 0:64], win[:, :, OFF_KPE:OFF_KPE + 64]),
                                (wv[:, :, 64:96], win[:, :, OFF_KPE + 32:OFF_KPE + 64]),
                                (wv[:, :, 96:128], win[:, :, OFF_KPE:OFF_KPE + 32])])
        pn, psw = self.ps[4], self.ps[5]
        for kc in range(KC):
            self.mm(pn.ap[0:64, :], wv[:, kc, 0:64], hT.ap[:, kc, :], kc == 0, kc == KC - 1, reads=[s, (hT, (kc, tc))], writes=[pn])
        for kc in range(KC):
            self.mm(psw.ap[0:64, :], wv[:, kc, 64:128], hT.ap[:, kc, :], kc == 0, kc == KC - 1, reads=[s, (hT, (kc, tc))], writes=[psw])
        ra = P.alloc("ra", [64, TCW], F32)
        rb = P.alloc("rb", [64, TCW], F32)
        kpst = P.alloc("kpst", [64, TCW], BF16)
        self.emit_rope(pn, psw, t0, kpst, kpst.ap[:], ra, rb)
        kb_, kap_ = kvx.loc(X_KPE + t0, TCW, rows=(0, 64))
        P.dma("sp", kb_, kap_, kpst, kpst.ap[:], out_key=("kpe", tc))
        kb_, kap_ = kvx.loc(X_KPE + t0, TCW, rows=(64, 128))
        P.dma("sp", kb_, kap_, kpst, kpst.ap[:], out_key=("kpe2", tc))
        vst = [P.alloc(f"vst{i}", [128, 2, 4, 128], BF16) for i in range(2)]
        for wi in range(4):
            s, wv = self.wtile(win[:, :, OFF_V + wi * 256:OFF_V + wi * 256 + 256], KC, 256)
            st = vst[wi % 2]
            for mm_ in range(4):
                pb = self.ps[mm_ % 4]
                for kc in range(KC):
                    self.mm(pb.ap[:, 0:256], hT.ap[:, kc, mm_ * 128:(mm_ + 1) * 128], wv[:, kc, :], kc == 0, kc == KC - 1,
                            reads=[s, (hT, (kc, tc))], writes=[pb])
                self.evac(lambda e, st=st, pb=pb, mm_=mm_: e.copy(st.ap[:, :, mm_, :], pb.ap[:, 0:256].rearrange("p (a b) -> p a b", a=2)),
                          lambda e, st=st, pb=pb, mm_=mm_: e.tensor_copy(st.ap[:, :, mm_, :], pb.ap[:, 0:256].rearrange("p (a b) -> p a b", a=2)),
                          reads=[pb], writes=[(st, mm_)])
            kb_, kap_ = kvx.loc(X_V + (2 * wi) * 1024, 2048)
            dst = kap_.rearrange("p (a b) -> p a b", a=2)[:, :, t0:t0 + TCW]
            P.dma("sp", kb_, dst, st, st.ap[:].rearrange("p a m d -> p a (m d)"), out_key=("v", wi, tc))
        ckvn = P.alloc("ckvn", [128, 4, TCW], BF16)
        self.emit_norm512(craw, 0, cqn, t0, gq, 0)
        self.emit_norm512(craw, 4, ckvn, 0, gq, 4)
        wukv = io["w_ukv"].rearrange("(kc p) n -> p kc n", p=128)
        vmst = [P.alloc(f"vmst{i}", [128, 4, 128], BF16) for i in range(2)]
        for h in range(NH):
            s, wv = self.wtile(wukv[:, :, h * 256:(h + 1) * 256], 4, 256)
            pb = self.ps[h % 2]
            for kc in range(4):
                self.mm(pb.ap[:], wv[:, kc, 0:128], ckvn.ap[:, kc, :], kc == 0, kc == 3, reads=[s, ckvn], writes=[pb])
            st = kst[nst % 2]
            nst += 1
            self.evac(lambda e, st=st, pb=pb: e.copy(st.ap[:], pb.ap[:]),
                      lambda e, st=st, pb=pb: e.tensor_copy(st.ap[:], pb.ap[:]), reads=[pb], writes=[st])
            kb_, kap_ = kvx.loc(X_KN + h * 1024 + t0, TCW)
            P.dma("sp", kb_, kap_, st, st.ap[:], out_key=("kn", h, tc))
            pv = self.ps[2 + h % 2]
            for mm_ in range(4):
                for kc in range(4):
                    self.mm(pv.ap[:, mm_ * 128:(mm_ + 1) * 128], ckvn.ap[:, kc, mm_ * 128:(mm_ + 1) * 128], wv[:, kc, 128:256],
                            kc == 0, kc == 3, reads=[s, ckvn], writes=[(pv, mm_)], inc=(kc == 3))
            vs = vmst[h % 2]
            self.evac(lambda e, vs=vs, pv=pv: e.copy(vs.ap[:].rearrange("p a b -> p (a b)"), pv.ap[:]),
                      lambda e, vs=vs, pv=pv: e.tensor_copy(vs.ap[:].rearrange("p a b -> p (a b)"), pv.ap[:]),
                      reads=[pv], writes=[vs])
            kb_, kap_ = kvx.loc(X_VM + h * 1024 + t0, TCW)
            P.dma("sp", kb_, kap_, vs, vs.ap[:].rearrange("p a b -> p (a b)"), out_key=("vm", h, tc))

    def emit_attn(self, io, tc, qsb, cqn, kvx, osb, omla):
        P = self.P
        t0 = tc * TCW
        nchunk = tc + 1
        nblk = 16 * nchunk
        kbuf = [P.alloc(f"kbuf{i}", [128, 4, 512], BF16) for i in range(2)]
        vbuf = [P.alloc(f"vbuf{i}", [128, 4, 512], BF16) for i in range(2)]
        qpos = self.qposb.ap[:, t0:t0 + TCW]
        nld = [0]

        def load_chunk(xk, xv, h, ch):
            kb, vb = kbuf[nld[0] % 2], vbuf[nld[0] % 2]
            nld[0] += 1
            c0 = h * 1024 + ch * 512
            fb_, fap_ = kvx.full(xk + c0, 512)
            P.dma("sp", kb, kb.ap[:], fb_, fap_)
            fb_, fap_ = kvx.full(xv + c0, 512)
            P.dma("sp", vb, vb.ap[:], fb_, fap_)
            return kb, vb

        m1 = P.mark()
        NE_ = 7
        ebuf = [P.alloc(f"ebuf{i}", [128, TCW], F32) for i in range(NE_)]
        spb = [P.alloc(f"spb{i}", [128, TCW], BF16) for i in range(3)]
        wps = [self.ps[6], self.ps[7]]
        qps = self.ps[5]
        P.op("dve", lambda e: e.tensor_copy(qps.ap[:], qpos), reads=[self.qposb], writes=[qps])
        abuf3 = [P.alloc(f"abuf3_{i}", [128, TCW], BF16) for i in range(3)]
        cacc = [P.alloc(f"cacc{i}", [128, TCW], BF16) for i in range(2)]
        units = []
        for h in range(NH):
            n = 0
            for ch in range(nchunk - 1, -1, -1):
                for ml in range(3, -1, -1):
                    for r in range(3, -1, -1):
                        units.append(dict(h=h, n=n, ch=ch, ml=ml, r=r, i=4 * (4 * ch + ml) + r))
                        n += 1
        chunk_bufs = {}
        NU = len(units)

        def st0(u):
            d = units[u]
            key = (d["h"], d["ch"])
            if key not in chunk_bufs:
                chunk_bufs[key] = load_chunk(X_K, X_V, d["h"], d["ch"])
            kb, vb = chunk_bufs[key]
            zps = self.ps[u % 2]
            ks = slice(d["ml"] * 128, (d["ml"] + 1) * 128)
            self.mm(zps.ap[:], kb.ap[:, d["r"], ks], qsb.ap[:, d["h"], t0:t0 + TCW], True, True,
                    reads=[kb, (qsb, (d["h"], tc))], writes=[zps])

        def st1(u):
            zps, eb = self.ps[u % 2], ebuf[u % NE_]
            P.op("act", lambda e: e.activation(eb.ap[:], zps.ap[:], AF.Exp, scale=SB_SCALE), reads=[zps], writes=[eb])

        def st2(u):
            i = units[u]["i"]
            eb = ebuf[u % NE_]
            if i >= 16 * tc:
                P.op("dve", lambda e: e.scalar_tensor_tensor(eb.ap[:], qps.ap[:], self.kposc.ap[:, i:i + 1], eb.ap[:], ALU.is_gt, ALU.mult),
                     reads=[eb, qps, self.kposc], writes=[eb])

        def st3(u):
            eb, sp_ = ebuf[u % NE_], spb[u % 3]
            P.op("act", lambda e: e.activation(sp_.ap[:], eb.ap[:], AF.Ln, bias=1.0, scale=1.0), reads=[eb], writes=[sp_])

        def st4(u):
            n = units[u]["n"]
            sps, sp_ = self.ps[2 + u % 2], spb[u % 3]
            cprev, cnew = cacc[(n + 1) % 2], cacc[n % 2]
            self.mm(sps.ap[:], self.tincl.ap[:], sp_.ap[:], True, n == 0, reads=[sp_, self.tincl], writes=[sps], inc=(n == 0))
            if n > 0:
                self.mm(sps.ap[:], self.ones.ap[:], cprev.ap[:], False, True, reads=[cprev, self.ones], writes=[sps])
            if n == 0:
                P.op("pool", lambda e: e.tensor_copy(cnew.ap[:], sp_.ap[:]), reads=[sp_], writes=[cnew])
            elif n < nblk - 1:
                P.op("pool", lambda e: e.tensor_tensor(cnew.ap[:], cprev.ap[:], sp_.ap[:], ALU.add), reads=[sp_, cprev], writes=[cnew])

        def st5(u):
            sps, wb = self.ps[2 + u % 2], wps[u % 2]
            P.op("act", lambda e: e.activation(wb.ap[:], sps.ap[:], AF.Exp, scale=-1.0), reads=[sps], writes=[wb])

        def st6(u):
            eb, wb, ab = ebuf[u % NE_], wps[u % 2], abuf3[u % 3]
            P.op("dve", lambda e: e.tensor_tensor(ab.ap[:], eb.ap[:], wb.ap[:], ALU.mult), reads=[eb, wb], writes=[ab])

        def st7(u):
            d = units[u]
            kb, vb = chunk_bufs[(d["h"], d["ch"])]
            ab = abuf3[u % 3]
            h, n = d["h"], d["n"]
            ops_ = self.ps[4]
            ks = slice(d["ml"] * 128, (d["ml"] + 1) * 128)
            self.mm(ops_.ap[:], vb.ap[:, d["r"], ks], ab.ap[:], n == 0, n == nblk - 1, reads=[vb, ab], writes=[ops_], inc=True)
            if n == nblk - 1:
                self.evac(lambda e, h=h, ops_=ops_: e.copy(osb.ap[:, h, :], ops_.ap[:]),
                          lambda e, h=h, ops_=ops_: e.tensor_copy(osb.ap[:, h, :], ops_.ap[:]), reads=[ops_], writes=[(osb, h)])

        stages = [st0, st1, st2, st3, st4, st5, st6, st7]
        for step in range(NU + len(stages) - 1):
            for k_, st in enumerate(stages):
                u = step - k_
                if 0 <= u < NU:
                    st(u)
        P.release(m1)
        kpe = P.alloc("kpe", [64, 4, 1024], BF16)
        nk = nchunk * 512
        fb_, fap_ = kvx.full(X_KPE, nk, rows=(0, 64))
        P.dma("sp", kpe, kpe.ap[:, :, 0:nk], fb_, fap_)
        wuq = io["w_uq"].rearrange("(kc p) n -> p kc n", p=128)
        qn = [P.alloc(f"qn{i}", [128, TCW], BF16) for i in range(2)]
        qp = [P.alloc(f"qp{i}", [64, TCW], BF16) for i in range(2)]
        ra = P.alloc("ra2", [64, TCW], F32)
        rb = P.alloc("rb2", [64, TCW], F32)
        lsum = P.alloc("lsum", [128, TCW], F32)
        abuf3m = [P.alloc(f"abuf3m_{i}", [128, TCW], BF16) for i in range(4)]
        for h in range(NH):
            s = self.wslot()
            wv = s.ap[:, 0:4 * 256].rearrange("p (a b) -> p a b", a=4)
            P.dma_multi("pool", s, [(wv[:, :, 0:192], wuq[:, :, h * 192:h * 192 + 192]),
                                    (wv[:, :, 192:224], wuq[:, :, h * 192 + 160:h * 192 + 192]),
                                    (wv[:, :, 224:256], wuq[:, :, h * 192 + 128:h * 192 + 160])])
            p0, p1, p2 = self.ps[0], self.ps[1], self.ps[2]
            for kc in range(4):
                self.mm(p0.ap[:], wv[:, kc, 0:128], cqn.ap[:, kc, t0:t0 + TCW], kc == 0, kc == 3, reads=[s, cqn], writes=[p0])
            for kc in range(4):
                self.mm(p1.ap[0:64, :], wv[:, kc, 128:192], cqn.ap[:, kc, t0:t0 + TCW], kc == 0, kc == 3, reads=[s, cqn], writes=[p1])
            for kc in range(4):
                self.mm(p2.ap[0:64, :], wv[:, kc, 192:256], cqn.ap[:, kc, t0:t0 + TCW], kc == 0, kc == 3, reads=[s, cqn], writes=[p2])
            qn_, qp_ = qn[h % 2], qp[h % 2]
            P.op("act", lambda e, qn_=qn_, p0=p0: e.copy(qn_.ap[:], p0.ap[:]), reads=[p0], writes=[qn_])
            self.emit_rope(p1, p2, t0, qp_, qp_.ap[:], ra, rb)
            ops_ = self.ps[4 + h % 2]
            sums = self.ps[6 + h % 2]
            units = [(ch, ml, r, 4 * (4 * ch + ml) + r) for ch in range(nchunk) for ml in range(4) for r in range(4)]
            chunk_bufs = {}

            def ma(n):
                ch, ml, r, i = units[n]
                if ch not in chunk_bufs:
                    chunk_bufs[ch] = load_chunk(X_KN, X_VM, h, ch)
                kb, vb = chunk_bufs[ch]
                sps = self.ps[[0, 1, 3][n % 3]]
                ks = slice(ml * 128, (ml + 1) * 128)
                kg = slice((4 * ch + ml) * 128, (4 * ch + ml + 1) * 128)
                self.mm(sps.ap[:], kb.ap[:, r, ks], qn_.ap[:], True, False, reads=[kb, qn_], writes=[sps], inc=False)
                self.mm(sps.ap[:], kpe.ap[:, r, kg], qp_.ap[:], False, True, reads=[kpe, qp_], writes=[sps])

            def mb(n):
                sps, ab = self.ps[[0, 1, 3][n % 3]], abuf3m[n % 4]
                P.op("act", lambda e: e.activation(ab.ap[:], sps.ap[:], AF.Exp, scale=MLA_SCALE), reads=[sps], writes=[ab])

            def mc(n):
                i = units[n][3]
                ab = abuf3m[n % 4]
                if i >= 16 * tc:
                    P.op("dve", lambda e: e.scalar_tensor_tensor(ab.ap[:], qpos, self.kposc.ap[:, i:i + 1], ab.ap[:], ALU.is_ge, ALU.mult),
                         reads=[ab, self.qposb, self.kposc], writes=[ab])

            def md(n):
                ch, ml, r, i = units[n]
                kb, vb = chunk_bufs[ch]
                ab = abuf3m[n % 4]
                ks = slice(ml * 128, (ml + 1) * 128)
                self.mm(ops_.ap[:], vb.ap[:, r, ks], ab.ap[:], n == 0, n == nblk - 1, reads=[vb, ab], writes=[ops_], inc=False)
                self.mm(sums.ap[:], self.ones.ap[:], ab.ap[:], n == 0, n == nblk - 1, reads=[self.ones, ab], writes=[sums], inc=True)

            mst = [ma, mb, mc, md]
            for step in range(nblk + len(mst) - 1):
                for k_, st in enumerate(mst):
                    n_ = step - k_
                    if 0 <= n_ < nblk:
                        st(n_)
            P.op("act", lambda e, sums=sums: e.activation(lsum.ap[:], sums.ap[:], AF.Ln), reads=[sums], writes=[lsum])
            P.op("act", lambda e: e.activation(lsum.ap[:], lsum.ap[:], AF.Exp, scale=-1.0), reads=[lsum], writes=[lsum])
            P.op("dve", lambda e, h=h, ops_=ops_: e.tensor_tensor(omla.ap[:, h, :], ops_.ap[:], lsum.ap[:], ALU.mult),
                 reads=[ops_, lsum], writes=[(omla, h)])

    def emit_C(self, io, tc, osb, omla):
        P = self.P
        t0 = tc * TCW
        hT = P.alloc("hTc", [128, KC, TCW], BF16)
        self.emit_norm(tc, hT, 0, lambda kc: self.lc.ap[:, kc:kc + 1], lambda kc: self.modT.ap[:, kc:kc + 1],
                       gbufs=[self.lc, self.modT])
        yT = P.alloc("yT", [128, KC, TCW], BF16)
        win = io["w_in"].rearrange("(kc p) n -> p kc n", p=128)
        wsu = io["w_sb_up"].rearrange("(kc p) n -> p kc n", p=128)
        wmu = io["w_mla_up"].rearrange("(kc p) n -> p kc n", p=128)
        wo = io["w_o"].rearrange("(kc p) n -> p kc n", p=128)
        sg = [P.alloc(f"sg{i}", [128, TCW], F32) for i in range(4)]
        y1 = [P.alloc(f"y1{i}", [128, TCW], F32) for i in range(4)]
        for pair in range(8):
            s1, w1 = self.wtile(win[:, :, OFF_GSB + pair * 256:OFF_GSB + pair * 256 + 256], KC, 256)
            s2, w2 = self.wtile(win[:, :, OFF_GMLA + pair * 256:OFF_GMLA + pair * 256 + 256], KC, 256)
            s3, w3 = self.wtile(wsu[:, :, pair * 256:pair * 256 + 256], 8, 256)
            s4, w4 = self.wtile(wmu[:, :, pair * 256:pair * 256 + 256], 8, 256)
            for sub in range(2):
                dc = pair * 2 + sub
                b = (dc % 2) * 4
                pgs, pgm, pus, pum = self.ps[b], self.ps[b + 1], self.ps[b + 2], self.ps[b + 3]
                cs = slice(sub * 128, (sub + 1) * 128)
                for kc in range(KC):
                    self.mm(pgs.ap[:], w1[:, kc, cs], hT.ap[:, kc, :], kc == 0, kc == KC - 1, reads=[s1, hT], writes=[pgs])
                for kc in range(KC):
                    self.mm(pgm.ap[:], w2[:, kc, cs], hT.ap[:, kc, :], kc == 0, kc == KC - 1, reads=[s2, hT], writes=[pgm])
                for kc in range(8):
                    self.mm(pus.ap[:], w3[:, kc, cs], osb.ap[:, kc, :], kc == 0, kc == 7, reads=[s3, osb], writes=[pus])
                for kc in range(8):
                    self.mm(pum.ap[:], w4[:, kc, cs], omla.ap[:, kc, :], kc == 0, kc == 7, reads=[s4, omla], writes=[pum])
                ga, gb_ = sg[(dc % 2) * 2], sg[(dc % 2) * 2 + 1]
                ya, yb = y1[(dc % 2) * 2], y1[(dc % 2) * 2 + 1]
                P.op("act", lambda e, ga=ga, pgs=pgs: e.activation(ga.ap[:], pgs.ap[:], AF.Sigmoid), reads=[pgs], writes=[ga])
                P.op("act", lambda e, gb_=gb_, pgm=pgm: e.activation(gb_.ap[:], pgm.ap[:], AF.Sigmoid), reads=[pgm], writes=[gb_])
                P.op("dve", lambda e, ya=ya, ga=ga, pus=pus: e.tensor_tensor(ya.ap[:], ga.ap[:], pus.ap[:], ALU.mult), reads=[ga, pus], writes=[ya])
                P.op("dve", lambda e, yb=yb, gb_=gb_, pum=pum: e.tensor_tensor(yb.ap[:], gb_.ap[:], pum.ap[:], ALU.mult), reads=[gb_, pum], writes=[yb])
                P.op("dve", lambda e, dc=dc, ya=ya, yb=yb: e.tensor_tensor(yT.ap[:, dc, :], ya.ap[:], yb.ap[:], ALU.add),
                     reads=[ya, yb], writes=[(yT, dc)])
        for pair in range(8):
            s1, w1 = self.wtile(wo[:, :, pair * 256:pair * 256 + 256], KC, 256)
            for sub in range(2):
                dc = pair * 2 + sub
                pb = self.ps[dc % 4]
                for kc in range(KC):
                    self.mm(pb.ap[:], w1[:, kc, sub * 128:(sub + 1) * 128], yT.ap[:, kc, :], kc == 0, kc == KC - 1,
                            reads=[s1, (yT, kc)], writes=[pb])
                P.op("dve", lambda e, dc=dc, pb=pb: e.scalar_tensor_tensor(self.xT.ap[:, dc, t0:t0 + TCW], pb.ap[:],
                                                                          self.modT.ap[:, 32 + dc:33 + dc],
                                                                          self.xT.ap[:, dc, t0:t0 + TCW], ALU.mult, ALU.add),
                     reads=[pb, self.modT, (self.xT, (dc, tc))], writes=[(self.xT, (dc, tc))])

    def emit_ffn(self, io, moe):
        P = self.P
        m0 = P.mark()
        h2 = P.alloc("h2", [128, KC, TL], BF16)
        gbt = None
        g2cols = [self.lc, self.modT]
        if moe:
            gbt = P.alloc("gbt", [128, NE, TL], BF16)
        m1 = P.mark()
        if moe:
            lgT = P.alloc("lgT", [8, TL], F32)
            gT = P.alloc("gT", [8, TL], F32)
            wr = P.alloc("wr", [128, KC, 8], F32)
            ident = P.alloc("ident", [128, 128], F32)
            sel = P.alloc("sel", [8, 8, 128], F32)
            onesf = P.alloc("onesf", [128, 1024], F32)
            P.op("dve", lambda e: e.memset(onesf.ap[:], 1.0), writes=[onesf])
            P.op("pool", lambda e: e.affine_select(ident.ap[:], onesf.ap[:, 0:128], [[-1, 128]], ALU.is_equal, 0.0,
                                                   base=0, channel_multiplier=1), reads=[onesf], writes=[ident])
            P.op("pool", lambda e: e.affine_select(sel.ap[:], onesf.ap[0:8, :].rearrange("p (a b) -> p a b", a=8),
                                                   [[1, 8], [0, 128]], ALU.is_equal, 0.0, base=0, channel_multiplier=-1),
                 reads=[onesf], writes=[sel])
            P.dma("sp", wr, wr.ap[:], None, io["w_routerT"])
        for tc in range(NTC):
            hook = None
            if moe:
                pbr = self.ps[tc]

                def hook(kc, hb, pbr=pbr):
                    self.mm(pbr.ap[0:8, :], wr.ap[:, kc, :], hb.ap[:], kc == 0, kc == KC - 1, reads=[wr, hb], writes=[pbr], inc=True)
            self.emit_norm(tc, h2, tc * TCW, lambda kc: self.lc.ap[:, 16 + kc:17 + kc], lambda kc: self.modT.ap[:, 48 + kc:49 + kc],
                           gbufs=g2cols, hook=hook)
            if moe:
                P.op("act", lambda e, tc=tc, pbr=pbr: e.copy(lgT.ap[:, tc * TCW:(tc + 1) * TCW], pbr.ap[0:8, :]), reads=[pbr], writes=[(lgT, tc)])
        if moe:
            lg = P.alloc("lg", [128, 8, 8], F32)
            mx = P.alloc("mx", [128, 8, 8], F32)
            gts = P.alloc("gts", [128, 8, 8], F32)
            sc = P.alloc("sc", [128, 8, 4], F32)
            for blk in range(8):
                pb = self.ps[2 + blk % 2]
                P.op("pe", lambda e, pb=pb, blk=blk: e.transpose(pb.ap[:, 0:8], lgT.ap[0:8, blk * 128:(blk + 1) * 128], ident.ap[0:8, 0:8]),
                     reads=[lgT, ident], writes=[pb])
                P.op("dve", lambda e, pb=pb, blk=blk: e.tensor_copy(lg.ap[:, blk, :], pb.ap[:, 0:8]), reads=[pb], writes=[(lg, blk)])
                P.op("dve", lambda e, blk=blk: e.max(mx.ap[:, blk, :], lg.ap[:, blk, :]), reads=[(lg, blk)], writes=[(mx, blk)])
                P.op("dve", lambda e, blk=blk: e.tensor_scalar(sc.ap[:, blk, 0:1], mx.ap[:, blk, 0:1], -1.0, None, ALU.mult),
                     reads=[(mx, blk)], writes=[(sc, blk)])
                P.op("dve", lambda e, blk=blk: e.tensor_tensor(sc.ap[:, blk, 1:2], mx.ap[:, blk, 1:2], mx.ap[:, blk, 0:1], ALU.subtract),
                     reads=[(mx, blk), (sc, blk)], writes=[(sc, blk)])
                P.op("act", lambda e, blk=blk: e.activation(sc.ap[:, blk, 2:3], sc.ap[:, blk, 1:2], AF.Exp), reads=[(sc, blk)], writes=[(sc, blk)])
                P.op("dve", lambda e, blk=blk: e.tensor_scalar(sc.ap[:, blk, 2:3], sc.ap[:, blk, 2:3], 1.0, None, ALU.add),
                     reads=[(sc, blk)], writes=[(sc, blk)])
                P.op("dve", lambda e, blk=blk: e.reciprocal(sc.ap[:, blk, 3:4], sc.ap[:, blk, 2:3]), reads=[(sc, blk)], writes=[(sc, blk)])
                P.op("act", lambda e, blk=blk: e.activation(gts.ap[:, blk, :], lg.ap[:, blk, :], AF.Exp, bias=sc.ap[:, blk, 0:1], scale=1.0),
                     reads=[(lg, blk), (sc, blk)], writes=[(gts, blk)])
                P.op("dve", lambda e, blk=blk: e.scalar_tensor_tensor(gts.ap[:, blk, :], lg.ap[:, blk, :], mx.ap[:, blk, 1:2], gts.ap[:, blk, :],
                                                                      ALU.is_ge, ALU.mult),
                     reads=[(lg, blk), (mx, blk), (gts, blk)], writes=[(gts, blk)])
                P.op("dve", lambda e, blk=blk: e.tensor_scalar(gts.ap[:, blk, :], gts.ap[:, blk, :], sc.ap[:, blk, 3:4], None, ALU.mult),
                     reads=[(gts, blk), (sc, blk)], writes=[(gts, blk)])
                pt = self.ps[4 + blk % 2]
                P.op("pe", lambda e, pt=pt, blk=blk: e.transpose(pt.ap[0:8, 0:128], gts.ap[:, blk, :], ident.ap[:]),
                     reads=[(gts, blk), ident], writes=[pt])
                P.op("dve", lambda e, pt=pt, blk=blk: e.tensor_copy(gT.ap[:, blk * 128:(blk + 1) * 128], pt.ap[0:8, 0:128]),
                     reads=[pt], writes=[(gT, blk)])
            for ex in range(NE):
                for tc in range(NTC):
                    pb = self.ps[6 + (ex * 2 + tc) % 2]
                    self.mm(pb.ap[:], sel.ap[:, ex, :], gT.ap[:, tc * TCW:(tc + 1) * TCW], True, True, reads=[sel, gT], writes=[pb])
                    self.evac(lambda e, pb=pb, ex=ex, tc=tc: e.copy(gbt.ap[:, ex, tc * TCW:(tc + 1) * TCW], pb.ap[:]),
                              lambda e, pb=pb, ex=ex, tc=tc: e.tensor_copy(gbt.ap[:, ex, tc * TCW:(tc + 1) * TCW], pb.ap[:]),
                              reads=[pb], writes=[(gbt, (ex, tc))])
        P.release(m1)
        act = P.alloc("actT", [128, GMAX, TL], BF16)
        sil = [P.alloc(f"sil{i}", [128, TCW], BF16) for i in range(2)]
        tmpa = [P.alloc(f"ftmp{i}", [128, TCW], BF16) for i in range(2)]
        nexp = NE if moe else 1
        n = 0
        for ex in range(nexp):
            if moe:
                wg = io["w_exp_gate"][ex].rearrange("(kc p) n -> p kc n", p=128)
                wu = io["w_exp_up"][ex].rearrange("(kc p) n -> p kc n", p=128)
                wd = io["w_exp_down"][ex].rearrange("(fc p) n -> p fc n", p=128)
            else:
                wg = io["w_ffn_gate"].rearrange("(kc p) n -> p kc n", p=128)
                wu = io["w_ffn_up"].rearrange("(kc p) n -> p kc n", p=128)
                wd = io["w_ffn_down"].rearrange("(fc p) n -> p fc n", p=128)
            fbase = 0
            for gsz in GROUPS:
                for pr in range(gsz // 2):
                    f0 = (fbase + 2 * pr) * 128
                    sG, wG = self.wtile(wg[:, :, f0:f0 + 256], KC, 256)
                    sU, wU = self.wtile(wu[:, :, f0:f0 + 256], KC, 256)
                    for sub in range(2):
                        fc = 2 * pr + sub
                        cs = slice(sub * 128, (sub + 1) * 128)
                        for tc in range(NTC):
                            n += 1
                            pg, pu = self.ps[(n % 2) * 2], self.ps[(n % 2) * 2 + 1]
                            for kc in range(KC):
                                self.mm(pg.ap[:], wG[:, kc, cs], h2.ap[:, kc, tc * TCW:(tc + 1) * TCW], kc == 0, kc == KC - 1,
                                        reads=[sG, (h2, (kc, tc))], writes=[pg])
                            for kc in range(KC):
                                self.mm(pu.ap[:], wU[:, kc, cs], h2.ap[:, kc, tc * TCW:(tc + 1) * TCW], kc == 0, kc == KC - 1,
                                        reads=[sU, (h2, (kc, tc))], writes=[pu])
                            sl = sil[n % 2]
                            P.op("act", lambda e, sl=sl, pg=pg: e.activation(sl.ap[:], pg.ap[:], AF.Silu), reads=[pg], writes=[sl])
                            dst = act.ap[:, fc, tc * TCW:(tc + 1) * TCW]
                            if moe:
                                tt = tmpa[n % 2]
                                P.op("dve", lambda e, tt=tt, sl=sl, pu=pu: e.tensor_tensor(tt.ap[:], sl.ap[:], pu.ap[:], ALU.mult),
                                     reads=[sl, pu], writes=[tt])
                                P.op("dve", lambda e, tt=tt, dst=dst, ex=ex, tc=tc: e.tensor_tensor(dst, tt.ap[:], gbt.ap[:, ex, tc * TCW:(tc + 1) * TCW], ALU.mult),
                                     reads=[tt, (gbt, (ex, tc))], writes=[(act, (fc, tc))])
                            else:
                                P.op("dve", lambda e, dst=dst, sl=sl, pu=pu: e.tensor_tensor(dst, sl.ap[:], pu.ap[:], ALU.mult),
                                     reads=[sl, pu], writes=[(act, (fc, tc))])
                for pair in range(8):
                    s, wv = self.wtile(wd[:, fbase:fbase + gsz, pair * 256:pair * 256 + 256], gsz, 256)
                    for sub in range(2):
                        dc = pair * 2 + sub
                        for tc in range(NTC):
                            pb = self.ps[4 + (dc * 2 + tc) % 4]
                            for fc in range(gsz):
                                self.mm(pb.ap[:], wv[:, fc, sub * 128:(sub + 1) * 128], act.ap[:, fc, tc * TCW:(tc + 1) * TCW],
                                        fc == 0, fc == gsz - 1, reads=[s, (act, (fc, tc))], writes=[pb])
                            P.op("dve", lambda e, dc=dc, tc=tc, pb=pb: e.scalar_tensor_tensor(
                                self.xT.ap[:, dc, tc * TCW:(tc + 1) * TCW], pb.ap[:], self.modT.ap[:, 80 + dc:81 + dc],
                                self.xT.ap[:, dc, tc * TCW:(tc + 1) * TCW], ALU.mult, ALU.add),
                                reads=[pb, self.modT, (self.xT, (dc, tc))], writes=[(self.xT, (dc, tc))])
                fbase += gsz
        P.release(m0)

    def emit_final(self, io, out_ap):
        P = self.P
        m0 = P.mark()
        fg = P.alloc("fg", [128, KC], F32)
        P.dma("sp", fg, fg.ap[:], None, io["final_gT"])
        refs = []
        for tc in range(NTC):
            m1 = P.mark()
            o32 = P.alloc("o32", [128, KC, TCW], F32)
            self.emit_norm(tc, o32, 0, lambda kc: fg.ap[:, kc:kc + 1], None, gbufs=[fg])
            refs.append(P.dma("sp", None, out_ap[:, :, tc * TCW:(tc + 1) * TCW], o32, o32.ap[:]))
            P.release(m1)
        P.release(m0)
        return refs


def _dram_in(nc, name, shape, dt):
    return nc.dram_tensor(name, list(shape), dt, kind="ExternalInput").ap()


def _dram_out(nc, name, shape, dt):
    return nc.dram_tensor(name, list(shape), dt, kind="ExternalOutput").ap()


def build_A():
    nc = bass.Bass("TRN2", target_bir_lowering=False)
    io = {
        "xT": _dram_in(nc, "xT", [128, KC, TL], F32),
        "cT": _dram_in(nc, "cT", [128, KC], F32),
        "pos": _dram_in(nc, "pos", [64, TL], I32),
        "qpos": _dram_in(nc, "qpos", [128, TL], I32),
        "w_ada": _dram_in(nc, "w_ada", [D, 6 * D], F32),
        "b_adaT": _dram_in(nc, "b_adaT", [128, 96], F32),
        "g_mixT": _dram_in(nc, "g_mixT", [128, KC], F32),
        "g_ffnT": _dram_in(nc, "g_ffnT", [128, KC], F32),
        "q_gT": _dram_in(nc, "q_gT", [128, 4], F32),
        "kv_gT": _dram_in(nc, "kv_gT", [128, 4], F32),
        "w_in": _dram_in(nc, "w_in", [D, IN_COLS], F32),
        "w_ukv": _dram_in(nc, "w_ukv", [512, 2048], F32),
    }
    kvloc = _dram_out(nc, "kvloc", [128, XW], BF16)
    qsb_o = _dram_out(nc, "qsb", [128, NH, TL], BF16)
    cqn_o = _dram_out(nc, "cqn", [128, 4, TL], BF16)
    mod_o = _dram_out(nc, "modo", [128, 128], F32)
    k = K(nc)
    P = k.P
    P.dma("sp", k.xT, k.xT.ap[:], None, io["xT"])
    k.emit_setup(io)
    k.emit_mod(io)
    qsb = P.alloc("qsb", [128, NH, TL], BF16, phase=False)
    cqn = P.alloc("cqn", [128, 4, TL], BF16, phase=False)
    kvb = Buf("kvloc")
    kvx = KVX([kvloc], [kvb], XW)
    for tc in range(NTC):
        m = P.mark()
        k.emit_A(io, tc, qsb, cqn, kvx)
        P.release(m)
    refs = []
    refs.append(P.dma("sp", None, qsb_o, qsb, qsb.ap[:]))
    refs.append(P.dma("sp", None, cqn_o, cqn, cqn.ap[:]))
    refs.append(P.dma("sp", None, mod_o[:, 0:96], k.modT, k.modT.ap[:]))
    refs.append(P.dma("sp", None, mod_o[:, 96:128], k.lc, k.lc.ap[:]))
    for d in P.state.get(kvb.id, {}).values():
        if d[0] is not None:
            refs.append(d[0])
    P.finish(refs)
    P.emit()
    return nc, P


def build_B(moe, last):
    nc = bass.Bass("TRN2", target_bir_lowering=False)
    io = {
        "xT": _dram_in(nc, "xT", [128, KC, TL], F32),
        "cT": _dram_in(nc, "cT", [128, KC], F32),
        "pos": _dram_in(nc, "pos", [64, TL], I32),
        "qpos": _dram_in(nc, "qpos", [128, TL], I32),
        "kvfull": _dram_in(nc, "kvfull", [512, XW], BF16),
        "qsb": _dram_in(nc, "qsb", [128, NH, TL], BF16),
        "cqn": _dram_in(nc, "cqn", [128, 4, TL], BF16),
        "modo": _dram_in(nc, "modo", [128, 128], F32),
        "w_in": _dram_in(nc, "w_in", [D, IN_COLS], F32),
        "w_uq": _dram_in(nc, "w_uq", [512, 1536], F32),
        "w_sb_up": _dram_in(nc, "w_sb_up", [1024, D], F32),
        "w_mla_up": _dram_in(nc, "w_mla_up", [1024, D], F32),
        "w_o": _dram_in(nc, "w_o", [D, D], F32),
    }
    if moe:
        io["w_routerT"] = _dram_in(nc, "w_routerT", [128, KC, 8], F32)
        io["w_exp_gate"] = _dram_in(nc, "w_exp_gate", [NE, D, DFF], F32)
        io["w_exp_up"] = _dram_in(nc, "w_exp_up", [NE, D, DFF], F32)
        io["w_exp_down"] = _dram_in(nc, "w_exp_down", [NE, DFF, D], F32)
    else:
        io["w_ffn_gate"] = _dram_in(nc, "w_ffn_gate", [D, DFF], F32)
        io["w_ffn_up"] = _dram_in(nc, "w_ffn_up", [D, DFF], F32)
        io["w_ffn_down"] = _dram_in(nc, "w_ffn_down", [DFF, D], F32)
    if last:
        io["final_gT"] = _dram_in(nc, "final_gT", [128, KC], F32)
    xo = _dram_out(nc, "xo", [128, KC, TL], F32)
    k = K(nc)
    P = k.P
    P.dma("sp", k.xT, k.xT.ap[:], None, io["xT"])
    k.emit_setup(io)
    P.dma("sp", k.modT, k.modT.ap[:], None, io["modo"][:, 0:96])
    P.dma("sp", k.lc, k.lc.ap[:], None, io["modo"][:, 96:128])
    base = P.mark()
    qsb = P.alloc("qsb", [128, NH, TL], BF16)
    cqn = P.alloc("cqn", [128, 4, TL], BF16)
    P.dma("sp", qsb, qsb.ap[:], None, io["qsb"])
    P.dma("sp", cqn, cqn.ap[:], None, io["cqn"])
    kvb = Buf("kvfull")
    kvx = KVX([io["kvfull"]], [kvb], XW)
    for tc in range(NTC):
        m = P.mark()
        osb = P.alloc("osb", [128, NH, TCW], BF16)
        omla = P.alloc("omla", [128, NH, TCW], BF16)
        m2 = P.mark()
        k.emit_attn(io, tc, qsb, cqn, kvx, osb, omla)
        P.release(m2)
        k.emit_C(io, tc, osb, omla)
        P.release(m)
    P.release(base)
    k.emit_ffn(io, moe)
    if last:
        refs = k.emit_final(io, xo)
    else:
        refs = [P.dma("sp", None, xo, k.xT, k.xT.ap[:])]
    P.finish(refs)
    P.emit()
    return nc, P


def _layer_io(io, l):
    jj = l // 2
    d = {"w_in": io["w_in"][l], "w_ukv": io["w_ukv"][l], "w_uq": io["w_uq"][l], "w_sb_up": io["w_sb_up"][l],
         "w_mla_up": io["w_mla_up"][l], "w_o": io["w_o"][l], "q_gT": io["q_gT"][l], "kv_gT": io["kv_gT"][l]}
    if l % 2 == 1:
        d["w_routerT"] = io["w_routerT"][jj]
        d["w_exp_gate"] = io["w_exp_gate"][jj]
        d["w_exp_up"] = io["w_exp_up"][jj]
        d["w_exp_down"] = io["w_exp_down"][jj]
    else:
        d["w_ffn_gate"] = io["w_ffn_gate"][jj]
        d["w_ffn_up"] = io["w_ffn_up"][jj]
        d["w_ffn_down"] = io["w_ffn_down"][jj]
    return d


GROUPS4 = [[0, 1, 2, 3], [4, 5, 6, 7]]


def build_fused(depth=DEPTH, final=True):
    nc = bass.Bass("TRN2", target_bir_lowering=False, num_devices=8)
    nd, nm = (depth + 1) // 2, depth // 2
    io = {
        "xT": _dram_in(nc, "xT", [128, KC, TL], F32),
        "cT": _dram_in(nc, "cT", [128, KC], F32),
        "pos": _dram_in(nc, "pos", [64, TL], I32),
        "qpos": _dram_in(nc, "qpos", [128, TL], I32),
        "w_ada_sh": _dram_in(nc, "w_ada_sh", [DEPTH, D, 3072], F32),
        "b_adaT": _dram_in(nc, "b_adaT", [DEPTH, 128, 96], F32),
        "g_mixT": _dram_in(nc, "g_mixT", [DEPTH, 128, KC], F32),
        "g_ffnT": _dram_in(nc, "g_ffnT", [DEPTH, 128, KC], F32),
        "q_gT": _dram_in(nc, "q_gT", [DEPTH, 128, 4], F32),
        "kv_gT": _dram_in(nc, "kv_gT", [DEPTH, 128, 4], F32),
        "w_in": _dram_in(nc, "w_in", [depth, D, IN_COLS], F32),
        "w_ukv": _dram_in(nc, "w_ukv", [depth, 512, 2048], F32),
        "w_uq": _dram_in(nc, "w_uq", [depth, 512, 1536], F32),
        "w_sb_up": _dram_in(nc, "w_sb_up", [depth, 1024, D], F32),
        "w_mla_up": _dram_in(nc, "w_mla_up", [depth, 1024, D], F32),
        "w_o": _dram_in(nc, "w_o", [depth, D, D], F32),
        "w_ffn_gate": _dram_in(nc, "w_ffn_gate", [nd, D, DFF], F32),
        "w_ffn_up": _dram_in(nc, "w_ffn_up", [nd, D, DFF], F32),
        "w_ffn_down": _dram_in(nc, "w_ffn_down", [nd, DFF, D], F32),
        "final_gT": _dram_in(nc, "final_gT", [128, KC], F32),
    }
    if nm > 0:
        io["w_routerT"] = _dram_in(nc, "w_routerT", [nm, 128, KC, 8], F32)
        io["w_exp_gate"] = _dram_in(nc, "w_exp_gate", [nm, NE, D, DFF], F32)
        io["w_exp_up"] = _dram_in(nc, "w_exp_up", [nm, NE, D, DFF], F32)
        io["w_exp_down"] = _dram_in(nc, "w_exp_down", [nm, NE, DFF, D], F32)
    xo = _dram_out(nc, "xo", [128, KC, TL], F32)
    modloc = nc.dram_tensor("modloc", [1, DEPTH * 3072], F32, kind="Internal").ap()
    modfull = nc.dram_tensor("modfull", [4, DEPTH * 3072], F32, kind="Internal").ap()
    PW = 2048
    widths = [PW] * 16 + [1024]
    kvlx, kvfx = [], []
    for par in range(2):
        la = [nc.dram_tensor(f"kvloc{par}_{i}", [128, w], BF16, kind="Internal").ap() for i, w in enumerate(widths)]
        fa = [nc.dram_tensor(f"kvfull{par}_{i}", [512, w], BF16, kind="Internal").ap() for i, w in enumerate(widths)]
        kvlx.append(KVX(la, [Buf(f"kvl{par}_{i}") for i in range(17)], PW))
        kvfx.append(KVX(fa, [Buf(f"kvf{par}_{i}") for i in range(17)], PW))
    k = K(nc)
    P = k.P
    P.dma("sp", k.xT, k.xT.ap[:], None, io["xT"])
    k.emit_setup(io)
    mlb, mfb = Buf("modloc"), Buf("modfull")
    k.emit_mod_shard(io, mlb, modloc)
    P.collective_allgather(mlb, modloc, mfb, modfull, GROUPS4)
    base = P.mark()
    for l in range(depth):
        lio = _layer_io(io, l)
        k.emit_mod_load(io, l, mfb, modfull)
        qsb = P.alloc("qsb", [128, NH, TL], BF16)
        cqn = P.alloc("cqn", [128, 4, TL], BF16)
        par = l % 2
        for tc in range(NTC):
            m = P.mark()
            k.emit_A(lio, tc, qsb, cqn, kvlx[par])
            P.release(m)
        order = []
        for hp in range(4):
            order += [X_K // PW + hp, X_V // PW + hp]
        order.append(16)
        for hp in range(4):
            order += [X_KN // PW + hp, X_VM // PW + hp]
        for i in order:
            P.collective_allgather(kvlx[par].bufs[i], kvlx[par].aps[i], kvfx[par].bufs[i], kvfx[par].aps[i], GROUPS4)
        for tc in range(NTC):
            m = P.mark()
            osb = P.alloc("osb", [128, NH, TCW], BF16)
            omla = P.alloc("omla", [128, NH, TCW], BF16)
            m2 = P.mark()
            k.emit_attn(lio, tc, qsb, cqn, kvfx[par], osb, omla)
            P.release(m2)
            k.emit_C(lio, tc, osb, omla)
            P.release(m)
        P.release(base)
        k.emit_ffn(lio, l % 2 == 1)
    if final:
        refs = k.emit_final(io, xo)
    else:
        refs = [P.dma("sp", None, xo, k.xT, k.xT.ap[:])]
    P.finish(refs)
    P.emit()
    return nc, P


_CACHE = {}


def _get(name, fn):
    if name not in _CACHE:
        _CACHE[name] = fn()
    return _CACHE[name]


def _colT(v, n):
    return np.ascontiguousarray(np.asarray(v, np.float32).reshape(n, 128).T)


def _tok_index(j):
    m = np.arange(8)[:, None]
    i = np.arange(128)[None, :]
    return ((4 * m + j) * 128 + i).reshape(-1)


def _host_inputs(x, c, positions, w_ada, b_ada, norm_mix_g, norm_ffn_g, w_in, q_norm_g, kv_norm_g,
                 w_uq, w_ukv, w_sb_up, w_mla_up, w_o, w_ffn_gate, w_ffn_up, w_ffn_down,
                 w_router, w_exp_gate, w_exp_up, w_exp_down, final_norm_g, depth=DEPTH):
    f32 = lambda a: np.ascontiguousarray(np.asarray(a, np.float32))
    x = np.asarray(x, np.float32)
    nd, nm = (depth + 1) // 2, depth // 2
    shared = {
        "b_adaT": np.stack([_colT(np.asarray(b_ada)[l], 96) for l in range(DEPTH)]),
        "g_mixT": np.stack([_colT(np.asarray(norm_mix_g)[l], KC) for l in range(DEPTH)]),
        "g_ffnT": np.stack([_colT(np.asarray(norm_ffn_g)[l], KC) for l in range(DEPTH)]),
        "q_gT": np.stack([_colT(np.asarray(q_norm_g)[l], 4) for l in range(DEPTH)]),
        "kv_gT": np.stack([_colT(np.asarray(kv_norm_g)[l], 4) for l in range(DEPTH)]),
        "w_in": f32(np.asarray(w_in)[:depth]), "w_ukv": f32(np.asarray(w_ukv)[:depth]), "w_uq": f32(np.asarray(w_uq)[:depth]),
        "w_sb_up": f32(np.asarray(w_sb_up)[:depth]), "w_mla_up": f32(np.asarray(w_mla_up)[:depth]), "w_o": f32(np.asarray(w_o)[:depth]),
        "w_ffn_gate": f32(np.asarray(w_ffn_gate)[:nd]), "w_ffn_up": f32(np.asarray(w_ffn_up)[:nd]),
        "w_ffn_down": f32(np.asarray(w_ffn_down)[:nd]),
        "final_gT": _colT(final_norm_g, KC),
    }
    if nm > 0:
        shared["w_routerT"] = np.ascontiguousarray(
            np.asarray(w_router, np.float32)[:nm].reshape(nm, KC, 128, 8).transpose(0, 2, 1, 3))
        shared["w_exp_gate"] = f32(np.asarray(w_exp_gate)[:nm])
        shared["w_exp_up"] = f32(np.asarray(w_exp_up)[:nm])
        shared["w_exp_down"] = f32(np.asarray(w_exp_down)[:nm])
    w_ada = np.asarray(w_ada, np.float32)
    ada_sh = [np.ascontiguousarray(w_ada[:, :, j * 3072:(j + 1) * 3072]) for j in range(4)]
    in_maps, toks = [], []
    for cid in range(8):
        b, j = cid // 4, cid % 4
        tok = _tok_index(j)
        toks.append(tok)
        xs = x[b, tok, :]
        d = dict(shared)
        d["xT"] = np.ascontiguousarray(xs.T.reshape(KC, 128, TL).transpose(1, 0, 2))
        d["cT"] = _colT(np.asarray(c)[b], KC)
        d["pos"] = np.ascontiguousarray(np.broadcast_to(np.asarray(positions)[b, tok].astype(np.int32)[None, :], (64, TL)))
        d["qpos"] = np.ascontiguousarray(np.broadcast_to(tok.astype(np.int32)[None, :], (128, TL)))
        d["w_ada_sh"] = ada_sh[j]
        in_maps.append(d)
    return in_maps, toks


def kernel(x, c, positions, w_ada, b_ada, norm_mix_g, norm_ffn_g, w_in, q_norm_g, kv_norm_g,
           w_uq, w_ukv, w_sb_up, w_mla_up, w_o, w_ffn_gate, w_ffn_up, w_ffn_down,
           w_router, w_exp_gate, w_exp_up, w_exp_down, final_norm_g):
    in_maps, toks = _host_inputs(x, c, positions, w_ada, b_ada, norm_mix_g, norm_ffn_g, w_in, q_norm_g, kv_norm_g,
                                 w_uq, w_ukv, w_sb_up, w_mla_up, w_o, w_ffn_gate, w_ffn_up, w_ffn_down,
                                 w_router, w_exp_gate, w_exp_up, w_exp_down, final_norm_g)
    nc = _get("fused", build_fused)[0]
    res = run_bass_kernel_spmd(nc, in_maps, core_ids=list(range(8))).results
    out = np.zeros((NB, SEQ, D), np.float32)
    for cid in range(8):
        xs = np.asarray(res[cid]["xo"]).transpose(1, 0, 2).reshape(D, TL).T
        out[cid // 4, toks[cid], :] = xs
    return out
```
